# Optimizing a Trainium2 kernel written in Bass

```python
import jax, jax.numpy as jnp
from jax import lax
import numpy as np

D_MODEL = 2048
BATCH = 4
SEQ = 8192
DEPTH = 1

HEAD_DIM = 128
NSA_HEADS = D_MODEL // (2 * HEAD_DIM)
NSA_KV_HEADS = 2
NSA_GROUP = NSA_HEADS // NSA_KV_HEADS
NSA_WIDTH = NSA_HEADS * HEAD_DIM
KV_WIDTH = NSA_KV_HEADS * HEAD_DIM
CMP_BLOCK = 32
CMP_STRIDE = 16
CMP_HIDDEN = 256
SEL_BLOCK = 64
SEL_TOPK = 16
SEL_LOCAL = 2
WINDOW = 512
Q_BLOCK = 128
GLA_HEADS = 4
GLA_DV = D_MODEL // (2 * GLA_HEADS)
GLA_DK = GLA_DV // 2
GLA_WIDTH = GLA_HEADS * GLA_DV
GLA_GATE_RANK = 16
GLA_GATE_TAU = 16.0
GLA_CHUNK = 64
MIX_WIDTH = NSA_WIDTH + GLA_WIDTH
N_EXPERTS = 32
TOP_K = 4
D_FF = D_MODEL
SWIGLU_LIMIT = 7.0
SWIGLU_ALPHA = 1.702
MOE_BLOCK = 256
LN_EPS = 1e-5
RMS_EPS = 1e-6
NEG_INF = -1e30
FORCE_SCORE = 1e6
DN_ALPHA = (2 * DEPTH) ** 0.25
DN_BETA = (8 * DEPTH) ** -0.25
PROJ_WIDTHS = (NSA_WIDTH, KV_WIDTH, KV_WIDTH, KV_WIDTH, KV_WIDTH, KV_WIDTH, KV_WIDTH, 3 * NSA_HEADS,
               GLA_HEADS * GLA_DK, GLA_HEADS * GLA_DK, GLA_WIDTH, GLA_GATE_RANK, GLA_WIDTH)
IN_WIDTH = sum(PROJ_WIDTHS)
VALUE_SEGMENTS = (2, 4, 6, 10)

kernel_name = "hybrid_nsa_gla_moe_block"


def split_cols(p):
    outs = []
    off = 0
    for w in PROJ_WIDTHS:
        outs.append(p[..., off:off + w])
        off += w
    return outs


def layer_norm(x, g, b):
    xf = x.astype(jnp.float32)
    mu = jnp.mean(xf, axis=-1, keepdims=True)
    var = jnp.mean(jnp.square(xf - mu), axis=-1, keepdims=True)
    return ((xf - mu) * lax.rsqrt(var + LN_EPS) * g + b).astype(x.dtype)


def alibi_slopes(n):
    return 2.0 ** (-8.0 * jnp.arange(1, n + 1, dtype=jnp.float32) / n)


def compress_tokens(kv, pe, w1, b1, w2):
    bsz, seq = kv.shape[:2]
    n_cmp = (seq - CMP_BLOCK) // CMP_STRIDE + 1
    idx = jnp.arange(n_cmp)[:, None] * CMP_STRIDE + jnp.arange(CMP_BLOCK)[None, :]
    blocks = kv[:, idx] + pe[None, None, :, None, :]
    flat = blocks.transpose(0, 1, 3, 2, 4).reshape(bsz, n_cmp, NSA_KV_HEADS, CMP_BLOCK * HEAD_DIM)
    return jax.nn.gelu(flat @ w1 + b1) @ w2


def nsa_attention(q, k_cmp, v_cmp, k_sel, v_sel, k_win, v_win, gates,
                  pe_k, w1_k, b1_k, w2_k, pe_v, w1_v, b1_v, w2_v):
    bsz, seq = q.shape[:2]
    G, R, Dh = NSA_KV_HEADS, NSA_GROUP, HEAD_DIM
    scale = Dh ** -0.5
    slopes = alibi_slopes(NSA_HEADS).reshape(G, R)
    kc = compress_tokens(k_cmp, pe_k, w1_k, b1_k, w2_k)
    vc = compress_tokens(v_cmp, pe_v, w1_v, b1_v, w2_v)
    n_cmp = kc.shape[1]
    cmp_start = jnp.arange(n_cmp) * CMP_STRIDE
    cmp_end = cmp_start + CMP_BLOCK - 1
    cmp_mid = cmp_start.astype(jnp.float32) + (CMP_BLOCK - 1) / 2.0
    n_sel = seq // SEL_BLOCK
    n_topk = min(SEL_TOPK, n_sel)
    sel_start = jnp.arange(n_sel) * SEL_BLOCK
    overlap = ((cmp_start[:, None] < sel_start[None, :] + SEL_BLOCK) &
               (cmp_start[:, None] + CMP_BLOCK > sel_start[None, :])).astype(jnp.float32)
    kb = k_sel.reshape(bsz, n_sel, SEL_BLOCK, G, Dh).transpose(0, 3, 1, 2, 4)
    vb = v_sel.reshape(bsz, n_sel, SEL_BLOCK, G, Dh).transpose(0, 3, 1, 2, 4)
    kw = jnp.pad(k_win, ((0, 0), (WINDOW, 0), (0, 0), (0, 0)))
    vw = jnp.pad(v_win, ((0, 0), (WINDOW, 0), (0, 0), (0, 0)))
    qg = q.reshape(bsz, seq, G, R, Dh)
    gg = gates.reshape(bsz, seq, G, R, 3)
    gather_blocks = jax.vmap(jax.vmap(lambda tbl, ix: tbl[ix]))

    def query_block(i):
        q0 = i * Q_BLOCK
        qb = lax.dynamic_slice_in_dim(qg, q0, Q_BLOCK, axis=1) * scale
        gb = lax.dynamic_slice_in_dim(gg, q0, Q_BLOCK, axis=1)
        pos = q0 + jnp.arange(Q_BLOCK)
        posf = pos.astype(jnp.float32)
        s = jnp.einsum('bqgrd,bcgd->bgrqc', qb, kc).astype(jnp.float32)
        s = s - slopes[None, :, :, None, None] * jnp.abs(posf[:, None] - cmp_mid[None, :])
        ok = cmp_end[None, :] <= pos[:, None]
        p_cmp = jax.nn.softmax(jnp.where(ok, s, NEG_INF), axis=-1) * jnp.any(ok, axis=-1)[:, None].astype(jnp.float32)
        o_cmp = jnp.einsum('bgrqc,bcgd->bqgrd', p_cmp.astype(vc.dtype), vc)
        imp = jnp.einsum('bgrqc,cn->bgqn', p_cmp, overlap)
        blk = jnp.arange(n_sel)[None, :]
        cur = (pos // SEL_BLOCK)[:, None]
        forced = (blk == 0) | ((blk <= cur) & (blk > cur - SEL_LOCAL))
        imp = jnp.where(forced, FORCE_SCORE, jnp.where(blk > cur, -1.0, imp))
        _, idx = lax.top_k(imp, n_topk)
        ks = gather_blocks(kb, idx)
        vs = gather_blocks(vb, idx)
        s = jnp.einsum('bqgrd,bgqkld->bgrqkl', qb, ks).astype(jnp.float32)
        kpos = idx[..., None] * SEL_BLOCK + jnp.arange(SEL_BLOCK)
        rel = pos[None, None, :, None, None] - kpos
        s = s - slopes[None, :, :, None, None, None] * jnp.abs(rel).astype(jnp.float32)[:, :, None]
        s = jnp.where((rel >= 0)[:, :, None], s, NEG_INF)
        p_sel = jax.nn.softmax(s.reshape(bsz, G, R, Q_BLOCK, -1), axis=-1).reshape(s.shape)
        o_sel = jnp.einsum('bgrqkl,bgqkld->bqgrd', p_sel.astype(vs.dtype), vs)
        kwb = lax.dynamic_slice_in_dim(kw, q0, Q_BLOCK + WINDOW, axis=1)
        vwb = lax.dynamic_slice_in_dim(vw, q0, Q_BLOCK + WINDOW, axis=1)
        kwpos = q0 - WINDOW + jnp.arange(Q_BLOCK + WINDOW)
        relw = pos[:, None] - kwpos[None, :]
        okw = (relw >= 0) & (relw < WINDOW) & (kwpos[None, :] >= 0)
        s = jnp.einsum('bqgrd,bkgd->bgrqk', qb, kwb).astype(jnp.float32)
        s = s - slopes[None, :, :, None, None] * jnp.abs(relw).astype(jnp.float32)
        p_win = jax.nn.softmax(jnp.where(okw, s, NEG_INF), axis=-1)
        o_win = jnp.einsum('bgrqk,bkgd->bqgrd', p_win.astype(vwb.dtype), vwb)
        o = gb[..., 0:1] * o_cmp + gb[..., 1:2] * o_sel + gb[..., 2:3] * o_win
        return o.reshape(bsz, Q_BLOCK, NSA_WIDTH)

    out = lax.map(query_block, jnp.arange(seq // Q_BLOCK))
    return out.transpose(1, 0, 2, 3).reshape(bsz, seq, NSA_WIDTH)


def gla_attention(q, k, v, log_a, r, norm_w):
    bsz, seq = q.shape[:2]
    H, C = GLA_HEADS, GLA_CHUNK
    nc = seq // C

    def chunks(t, d):
        return t.astype(jnp.float32).reshape(bsz, nc, C, H, d).transpose(1, 0, 3, 2, 4)

    qc = chunks(q, GLA_DK) * GLA_DK ** -0.5
    kc = chunks(k, GLA_DK)
    vc = chunks(v, GLA_DV)
    ac = chunks(log_a, GLA_DK)
    causal = jnp.tril(jnp.ones((C, C), dtype=bool))[:, :, None]

    def step(state, inp):
        qi, ki, vi, ai = inp
        b = jnp.cumsum(ai, axis=2)
        diff = b[:, :, :, None, :] - b[:, :, None, :, :]
        decay = jnp.exp(jnp.where(causal, diff, -jnp.inf))
        scores = jnp.einsum('bhtd,bhsd,bhtsd->bhts', qi, ki, decay)
        o = scores @ vi + (qi * jnp.exp(b)) @ state
        b_last = b[:, :, -1:, :]
        state = (jnp.exp(b_last[:, :, 0, :, None]) * state +
                 jnp.einsum('bhsd,bhsv->bhdv', ki * jnp.exp(b_last - b), vi))
        return state, o

    s0 = jnp.zeros((bsz, H, GLA_DK, GLA_DV), jnp.float32)
    _, o = lax.scan(step, s0, (qc, kc, vc, ac))
    o = o.transpose(1, 0, 3, 2, 4).reshape(bsz, seq, H, GLA_DV)
    o = o * lax.rsqrt(jnp.mean(jnp.square(o), axis=-1, keepdims=True) + RMS_EPS) * norm_w
    return (o.reshape(bsz, seq, GLA_WIDTH) * jax.nn.silu(r.astype(jnp.float32))).astype(r.dtype)


def moe_ffn(h, w_router, b_router, w_gate_up, b_gate_up, w_down, b_down):
    bsz, seq, d = h.shape
    n_tok = bsz * seq
    n_asg = n_tok * TOP_K
    hf = h.reshape(n_tok, d)
    logits = (hf @ w_router + b_router).astype(jnp.float32)
    top_logits, top_idx = lax.top_k(logits, TOP_K)
    top_w = jax.nn.softmax(top_logits, axis=-1)
    flat_e = top_idx.reshape(n_asg)
    order = jnp.argsort(flat_e)
    sorted_e = flat_e[order]
    counts = jnp.bincount(flat_e, length=N_EXPERTS)
    padded = (counts + MOE_BLOCK - 1) // MOE_BLOCK * MOE_BLOCK
    start = jnp.cumsum(counts) - counts
    ends_p = jnp.cumsum(padded)
    start_p = ends_p - padded
    dest = start_p[sorted_e] + jnp.arange(n_asg, dtype=jnp.int32) - start[sorted_e]
    slot_of = jnp.zeros(n_asg, jnp.int32).at[order].set(dest.astype(jnp.int32))
    n_blocks = -(-n_asg // MOE_BLOCK) + N_EXPERTS
    n_slots = n_blocks * MOE_BLOCK
    slot_token = jnp.zeros(n_slots, jnp.int32).at[slot_of].set(jnp.arange(n_asg, dtype=jnp.int32) // TOP_K)
    slot_w = jnp.zeros(n_slots, jnp.float32).at[slot_of].set(top_w.reshape(n_asg))
    block_expert = jnp.minimum(jnp.searchsorted(ends_p, jnp.arange(n_blocks) * MOE_BLOCK, side='right'),
                               N_EXPERTS - 1)
    xbuf = hf[slot_token].reshape(n_blocks, MOE_BLOCK, d)

    def expert_block(args):
        xb, e = args
        gu = xb @ w_gate_up[e] + b_gate_up[e]
        gate = jnp.minimum(gu[:, :D_FF], SWIGLU_LIMIT)
        up = jnp.clip(gu[:, D_FF:], -SWIGLU_LIMIT, SWIGLU_LIMIT)
        act = (up + 1.0) * gate * jax.nn.sigmoid(SWIGLU_ALPHA * gate)
        return act @ w_down[e] + b_down[e]

    ybuf = lax.map(expert_block, (xbuf, block_expert)).reshape(n_slots, d)
    y = jax.ops.segment_sum(ybuf * slot_w[:, None].astype(ybuf.dtype), slot_token, num_segments=n_tok)
    return y.reshape(bsz, seq, d)


def setup_inputs(seed: int = 0) -> dict:
    key = jax.random.key(seed)
    ks = jax.random.split(key, 27)
    L = DEPTH

    def nrm(k, shape, s):
        return s * jax.random.normal(k, shape, jnp.float32)

    col_scale = jnp.concatenate([jnp.full((w,), DN_BETA if i in VALUE_SEGMENTS else 1.0, jnp.float32)
                                 for i, w in enumerate(PROJ_WIDTHS)])
    return {
        "x": nrm(ks[0], (BATCH, SEQ, D_MODEL), 1.0),
        "c": nrm(ks[1], (BATCH, D_MODEL), 1.0),
        "w_ada": nrm(ks[2], (L, D_MODEL, 6 * D_MODEL), 0.5 * D_MODEL ** -0.5),
        "b_ada": nrm(ks[3], (L, 6 * D_MODEL), 0.02),
        "w_in": nrm(ks[4], (L, D_MODEL, IN_WIDTH), D_MODEL ** -0.5) * col_scale,
        "cmp_pe_k": nrm(ks[5], (L, CMP_BLOCK, HEAD_DIM), 0.02),
        "cmp_w1_k": nrm(ks[6], (L, CMP_BLOCK * HEAD_DIM, CMP_HIDDEN), (CMP_BLOCK * HEAD_DIM) ** -0.5),
        "cmp_b1_k": nrm(ks[7], (L, CMP_HIDDEN), 0.02),
        "cmp_w2_k": nrm(ks[8], (L, CMP_HIDDEN, HEAD_DIM), CMP_HIDDEN ** -0.5),
        "cmp_pe_v": nrm(ks[9], (L, CMP_BLOCK, HEAD_DIM), 0.02),
        "cmp_w1_v": nrm(ks[10], (L, CMP_BLOCK * HEAD_DIM, CMP_HIDDEN), (CMP_BLOCK * HEAD_DIM) ** -0.5),
        "cmp_b1_v": nrm(ks[11], (L, CMP_HIDDEN), 0.02),
        "cmp_w2_v": nrm(ks[12], (L, CMP_HIDDEN, HEAD_DIM), CMP_HIDDEN ** -0.5),
        "gla_w_gate": nrm(ks[13], (L, GLA_GATE_RANK, GLA_HEADS * GLA_DK), GLA_GATE_RANK ** -0.5),
        "gla_b_gate": nrm(ks[14], (L, GLA_HEADS * GLA_DK), 0.1),
        "gla_norm_w": 1.0 + nrm(ks[15], (L, GLA_DV), 0.02),
        "w_out": nrm(ks[16], (L, MIX_WIDTH, D_MODEL), DN_BETA * MIX_WIDTH ** -0.5),
        "ln1_g": 1.0 + nrm(ks[17], (L, D_MODEL), 0.02),
        "ln1_b": nrm(ks[18], (L, D_MODEL), 0.02),
        "w_router": nrm(ks[19], (L, D_MODEL, N_EXPERTS), D_MODEL ** -0.5),
        "b_router": nrm(ks[20], (L, N_EXPERTS), 0.01),
        "w_gate_up": nrm(ks[21], (L, N_EXPERTS, D_MODEL, 2 * D_FF), D_MODEL ** -0.5),
        "b_gate_up": nrm(ks[22], (L, N_EXPERTS, 2 * D_FF), 0.02),
        "w_down": nrm(ks[23], (L, N_EXPERTS, D_FF, D_MODEL), DN_BETA * D_FF ** -0.5),
        "b_down": nrm(ks[24], (L, N_EXPERTS, D_MODEL), 0.02),
        "ln2_g": 1.0 + nrm(ks[25], (L, D_MODEL), 0.02),
        "ln2_b": nrm(ks[26], (L, D_MODEL), 0.02),
    }


def reference(x, c, w_ada, b_ada, w_in, cmp_pe_k, cmp_w1_k, cmp_b1_k, cmp_w2_k,
              cmp_pe_v, cmp_w1_v, cmp_b1_v, cmp_w2_v, gla_w_gate, gla_b_gate, gla_norm_w,
              w_out, ln1_g, ln1_b, w_router, b_router, w_gate_up, b_gate_up, w_down, b_down,
              ln2_g, ln2_b):
    bsz, seq, _ = x.shape
    for l in range(DEPTH):
        mod = jax.nn.silu(c) @ w_ada[l] + b_ada[l]
        sh1, sc1, g1, sh2, sc2, g2 = [m[:, None, :] for m in jnp.split(mod, 6, axis=-1)]
        h = x * (1.0 + sc1) + sh1
        (q_nsa, k_cmp, v_cmp, k_sel, v_sel, k_win, v_win, g_nsa,
         q_gla, k_gla, v_gla, a_gla, r_gla) = split_cols(h @ w_in[l])
        kv_heads = lambda t: t.reshape(bsz, seq, NSA_KV_HEADS, HEAD_DIM)
        y_nsa = nsa_attention(q_nsa.reshape(bsz, seq, NSA_HEADS, HEAD_DIM),
                              kv_heads(k_cmp), kv_heads(v_cmp), kv_heads(k_sel), kv_heads(v_sel),
                              kv_heads(k_win), kv_heads(v_win),
                              jax.nn.sigmoid(g_nsa.reshape(bsz, seq, NSA_HEADS, 3)),
                              cmp_pe_k[l], cmp_w1_k[l], cmp_b1_k[l], cmp_w2_k[l],
                              cmp_pe_v[l], cmp_w1_v[l], cmp_b1_v[l], cmp_w2_v[l])
        log_a = jax.nn.log_sigmoid((a_gla @ gla_w_gate[l] + gla_b_gate[l]).astype(jnp.float32)) / GLA_GATE_TAU
        y_gla = gla_attention(q_gla, k_gla, v_gla, log_a, r_gla, gla_norm_w[l])
        mix = jnp.concatenate([y_nsa, y_gla], axis=-1) @ w_out[l]
        x = layer_norm(DN_ALPHA * x + g1 * mix, ln1_g[l], ln1_b[l])
        h = x * (1.0 + sc2) + sh2
        ffn = moe_ffn(h, w_router[l], b_router[l], w_gate_up[l], b_gate_up[l], w_down[l], b_down[l])
        x = layer_norm(DN_ALPHA * x + g2 * ffn, ln2_g[l], ln2_b[l])
    return x
```

```python
import numpy as np
import ml_dtypes
import concourse.bass as bass
import concourse.mybir as mybir
from concourse.bass_utils import run_bass_kernel_spmd

F32 = mybir.dt.float32
BF16 = mybir.dt.bfloat16
I32 = mybir.dt.int32
U32 = mybir.dt.uint32
AF = mybir.ActivationFunctionType
ALU = mybir.AluOpType
AX = mybir.AxisListType

D = 2048
T = 8192
TO = 4096
NQB = 32
IN_W = 5672
NEG = -30000.0
CAP = 2048
NE = 32
LN_EPS = 1e-5
RMS_EPS = 1e-6
DN_ALPHA = 2 ** 0.25


class Res:
    __slots__ = ("name", "w", "sw", "r")

    def __init__(self, name=""):
        self.name = name
        self.w = {}
        self.sw = {}
        self.r = {}


class Prog:
    ENGS = ("pe", "act", "dve", "pool", "sp")

    def __init__(self, nc, n_dsem=40):
        self.nc = nc
        self.ops = {e: [] for e in self.ENGS}
        self.cnt = {e: 0 for e in self.ENGS}
        self.known = {e: {} for e in self.ENGS}
        self.n_dsem = n_dsem
        self.dsem_use = [0] * n_dsem
        self.dsem_next = 0
        self.final_toks = []
        self.pending = {e: [] for e in self.ENGS}

    def barrier(self):
        toks = [("E", e, self.cnt[e]) for e in self.ENGS if self.cnt[e] > 0]
        toks += [("D", s, u * 16) for s, u in enumerate(self.dsem_use) if u > 0]
        for e in self.ENGS:
            self.pending[e] = list(toks)

    def _deps(self, eng, reads, writes, ws, extra=()):
        toks = list(extra) + self.pending[eng]
        self.pending[eng] = []
        for r in reads:
            for dd in (r.w, r.sw):
                for k, v in dd.items():
                    toks.append((k[0], k[1], v))
        for w in writes:
            for dd in (w.w, w.sw, w.r):
                for k, v in dd.items():
                    toks.append((k[0], k[1], v))
        for w in ws:
            for dd in (w.w, w.r):
                for k, v in dd.items():
                    toks.append((k[0], k[1], v))
        waits = {}
        kn = self.known[eng]
        for kind, key, val in toks:
            if kind == "E" and key == eng and eng == "pe":
                continue
            if kn.get((kind, key), 0) >= val:
                continue
            if waits.get((kind, key), 0) < val:
                waits[(kind, key)] = val
        for k, v in waits.items():
            kn[k] = v
        return list(waits.items())

    def _mark(self, tok, reads, writes, ws):
        k = (tok[0], tok[1])
        for r in reads:
            if r.r.get(k, 0) < tok[2]:
                r.r[k] = tok[2]
        for w in writes:
            w.w = {k: tok[2]}
            w.sw = {}
            w.r = {}
        for w in ws:
            if w.sw.get(k, 0) < tok[2]:
                w.sw[k] = tok[2]

    def op(self, eng, fn, reads=(), writes=(), ws=()):
        waits = self._deps(eng, reads, writes, ws)
        self.cnt[eng] += 1
        tok = ("E", eng, self.cnt[eng])
        self.ops[eng].append((waits, fn, ("E", eng, 1)))
        self._mark(tok, reads, writes, ws)
        return tok

    def dma(self, eng, fn, reads=(), writes=(), ws=(), final=False):
        s = self.dsem_next
        self.dsem_next = (s + 1) % self.n_dsem
        prev = self.dsem_use[s]
        extra = [("D", s, prev * 16)] if prev > 0 else []
        waits = self._deps(eng, reads, writes, ws, extra)
        self.dsem_use[s] += 1
        tok = ("D", s, self.dsem_use[s] * 16)
        self.ops[eng].append((waits, fn, ("D", s, 16)))
        self._mark(tok, reads, writes, ws)
        if final:
            self.final_toks.append(tok)
        return tok

    def emit(self):
        nc = self.nc
        esem = {e: nc.alloc_semaphore(name="es_" + e) for e in self.ENGS}
        dsem = [nc.alloc_semaphore(name="ds_%d" % i) for i in range(self.n_dsem)]
        fw = self._deps("sp", (), (), (), self.final_toks)

        def sem_of(kind, key):
            return esem[key] if kind == "E" else dsem[key]

        def run(eng_name, e):
            for waits, fn, inc in self.ops[eng_name]:
                for (kind, key), val in waits:
                    e.wait_ge(sem_of(kind, key), val)
                ins = fn(e)
                ins.then_inc(sem_of(inc[0], inc[1]), inc[2])
            if eng_name == "sp":
                for (kind, key), val in fw:
                    e.wait_ge(sem_of(kind, key), val)

        with nc.Block() as block:
            @block.tensor
            def _(e):
                run("pe", e)

            @block.scalar
            def _(e):
                run("act", e)

            @block.vector
            def _(e):
                run("dve", e)

            @block.gpsimd
            def _(e):
                run("pool", e)

            @block.sync
            def _(e):
                run("sp", e)


def _cmp_partial_tiles():
    idx = {}
    for i in range(NQB):
        for j in range(4):
            o = 4065 + 128 * i - 2048 * j
            if o + 127 < 0:
                continue
            if o < 2032:
                idx[(i, j)] = len(idx)
    return idx


CMP_PART = _cmp_partial_tiles()


def _bf(a):
    return np.ascontiguousarray(a.astype(ml_dtypes.bfloat16))


def _pack(tb, dtype):
    off = {}
    cols = []
    o = 0
    for k, v in tb.items():
        off[k] = (o, v.shape[1])
        o += v.shape[1]
        cols.append(v)
    arr = np.concatenate(cols, axis=1)
    if dtype == "bf16":
        return _bf(arr), off
    return np.ascontiguousarray(arr.astype(np.float32)), off


def make_tables(h):
    p = np.arange(128)
    tb = {}
    tb["identb"] = np.eye(128)
    tb["onesb"] = np.ones((128, 128))
    tb["tri"] = np.where(p[:, None] <= p[None, :], 0.0, NEG)
    tb["band"] = np.where(p[:, None] > p[None, :], 0.0, NEG)
    tb["cmask"] = (p[:, None] <= p[None, :]).astype(np.float64)
    tb["ltri"] = (p[:, None] < p[None, :]).astype(np.float64)
    tbs, tbs_off = _pack(tb, "bf16")

    ta = {}
    E = np.zeros((128, 64, 128))
    for kt in range(64):
        for key in range(128):
            E[2 * kt + key // 64, kt, key] = 1.0
    ta["E"] = E.reshape(128, 64 * 128)
    ov = np.zeros((128, 4, 128))
    for j in range(4):
        cs = 16 * (128 * j + p)
        for blk in range(128):
            ov[:, j, blk] = ((cs < 64 * blk + 64) & (cs + 32 > 64 * blk)).astype(np.float64)
    ta["ovl"] = ov.reshape(128, 512)
    cm = np.zeros((128, len(CMP_PART), 128))
    for (i, j), t in CMP_PART.items():
        c = 128 * j + p
        q = 4096 + 128 * i + p
        cm[:, t, :] = np.where((16 * c + 31)[:, None] <= q[None, :], 0.0, NEG)
    ta["cmpm"] = cm.reshape(128, -1)
    tba, tba_off = _pack(ta, "bf16")

    tf = {}
    tf["identf"] = np.eye(128)
    tf["triI"] = np.where(p[:, None] <= p[None, :], -1.0 / 16, 0.0)
    tf["triR"] = np.where(p[:, None] > p[None, :], -1.0 / 16, 0.0)
    tf["onesf"] = np.ones((128, 128))
    r = np.arange(256) - 128
    curq = (p >= 64).astype(np.int64)
    future = r[None, :] > curq[:, None]
    forced = (r[None, :] <= curq[:, None]) & (r[None, :] > curq[:, None] - 2)
    tf["tblA"] = np.where(future | forced, 0.0, 1.0)
    tf["tblB"] = np.where(forced, 1e6, np.where(future, -1.0, 0.0))
    blk0 = 64 * (1 - h)
    f0 = np.full((128, 128), -1e9)
    f0[:, blk0] = 1e6
    tf["F0"] = f0
    tf["iotaE"] = np.tile(np.arange(32)[None, :], (128, 1)).astype(np.float64)
    tf["iotaEC1"] = tf["iotaE"] * CAP + 1.0
    tf["tokid"] = (np.arange(32)[None, :] * 128 + p[:, None]).astype(np.float64)
    tf["flag"] = np.full((128, 1), float(h))
    tf128, tf_off = _pack(tf, "f32")

    pos = np.arange(T)
    invalid = (pos < 4096).astype(np.float64) * (1 - h)
    posrows = np.stack([np.ones(T), pos // 64, pos % 64, invalid, np.ones(T)])
    c = np.arange(512)
    m2 = 32 * c + 31
    inv_c = ((c < 256).astype(np.float64) * (1 - h))
    inv_c[511] = 1.0
    posrows_c = np.stack([np.ones(512), m2 // 64, m2 % 64, inv_c, np.ones(512)])
    slopes = 2.0 ** (-(np.arange(8) + 1.0))
    src = np.zeros((5, NQB, 8))
    srcc = np.zeros((5, NQB, 8))
    for i in range(NQB):
        a_i = 64 + 2 * i
        src[1, i] = 64 * slopes
        src[2, i] = slopes
        src[3, i] = NEG
        src[4, i] = -64 * slopes * a_i
        srcc[1, i] = 32 * slopes
        srcc[2, i] = slopes / 2
        srcc[3, i] = NEG
        srcc[4, i] = -64 * slopes * a_i
    qrow = np.zeros((5, 8, 128))
    qrow[0] = -slopes[:, None] * np.arange(128)[None, :]
    t5 = {"posrows": posrows, "posrows_c": posrows_c, "src": src.reshape(5, -1),
          "srcc": srcc.reshape(5, -1), "qrow": qrow.reshape(5, -1)}
    for k, v in t5.items():
        ok = (v == NEG) | (v.astype(ml_dtypes.bfloat16).astype(np.float64) == v)
        assert ok.all(), k
    tb5, t5_off = _pack(t5, "bf16")
    return dict(tbs=tbs, tbs_off=tbs_off, tba=tba, tba_off=tba_off, tf=tf128, tf_off=tf_off,
                tb5=tb5, t5_off=t5_off)


_TBL_CACHE = {}


def tables(h):
    if h not in _TBL_CACHE:
        _TBL_CACHE[h] = make_tables(h)
    return _TBL_CACHE[h]


def build(debug=False, stages=99):
    nc = bass.Bass("TRN2", target_bir_lowering=False)
    P = Prog(nc)
    TBL = tables(0)
    tbo, tao, tfo, t5o = TBL["tbs_off"], TBL["tba_off"], TBL["tf_off"], TBL["t5_off"]

    def din(name, shape, dt=F32):
        return nc.dram_tensor(name, list(shape), dt, kind="ExternalInput").ap()

    skind = "ExternalOutput" if debug else "Internal"

    def dscr(name, shape, dt):
        return nc.dram_tensor(name, list(shape), dt, kind=skind).ap()

    def sb(name, shape, dt=F32):
        return nc.alloc_sbuf_tensor(name, list(shape), dt)

    ARENA = 196 * 1024
    arena = sb("arena", [128, ARENA // 2], BF16)
    ast = {"off": 0}
    DSZ = {F32: 4, BF16: 2, I32: 4, U32: 4}

    def areset():
        P.barrier()
        ast["off"] = 0

    def al(name, shape, dt=F32):
        shape = list(shape)
        nfree = 1
        for d_ in shape[1:]:
            nfree *= d_
        nbytes = (nfree * DSZ[dt] + 31) // 32 * 32
        o = ast["off"]
        assert o + nbytes <= ARENA, (name, o, nbytes)
        ast["off"] = o + nbytes
        v = arena[0:shape[0], o // 2:(o + nbytes) // 2]
        if dt != BF16:
            v = v.bitcast(dt)
        v = v[:, 0:nfree]
        if len(shape) == 3:
            v = v.rearrange("p (a b) -> p a b", a=shape[1])
        elif len(shape) == 4:
            v = v.rearrange("p (a b c) -> p a b c", a=shape[1], b=shape[2])
        return v

    RS = {}

    def R(name):
        if name not in RS:
            RS[name] = Res(name)
        return RS[name]

    def mm(out, lhsT, rhs, start, stop, reads, writes, ws=()):
        P.op("pe", lambda e: e.matmul(out, lhsT, rhs, start=start, stop=stop), reads, writes, ws)

    def trp(out, in_, ident, reads, writes, ws=()):
        P.op("pe", lambda e: e.transpose(out, in_, ident), reads, writes, ws)

    def act(out, in_, func, reads, writes, bias=None, scale=None, ws=()):
        kw = {}
        if bias is not None:
            kw["bias"] = bias
        if scale is not None:
            kw["scale"] = scale
        P.op("act", lambda e: e.activation(out=out, in_=in_, func=func, **kw), reads, writes, ws)

    def tt(eng, out, in0, in1, op, reads, writes, ws=()):
        P.op(eng, lambda e: e.tensor_tensor(out=out, in0=in0, in1=in1, op=op), reads, writes, ws)

    def ts(eng, out, in0, s1, s2, op0, op1, reads, writes, ws=()):
        if op1 is None:
            P.op(eng, lambda e: e.tensor_scalar(out=out, in0=in0, scalar1=s1, scalar2=None, op0=op0),
                 reads, writes, ws)
        else:
            P.op(eng, lambda e: e.tensor_scalar(out=out, in0=in0, scalar1=s1, scalar2=s2, op0=op0, op1=op1),
                 reads, writes, ws)

    def stt(eng, out, in0, scalar, in1, op0, op1, reads, writes, ws=()):
        P.op(eng, lambda e: e.scalar_tensor_tensor(out=out, in0=in0, scalar=scalar, in1=in1, op0=op0, op1=op1),
             reads, writes, ws)

    def cp(eng, out, in_, reads, writes, ws=()):
        if eng == "act":
            P.op("act", lambda e: e.activation(out=out, in_=in_, func=AF.Copy), reads, writes, ws)
        else:
            P.op(eng, lambda e: e.tensor_copy(out, in_), reads, writes, ws)

    def recip(out, in_, reads, writes, ws=()):
        P.op("dve", lambda e: e.reciprocal(out, in_), reads, writes, ws)

    def rsum(out, in_, reads, writes):
        P.op("dve", lambda e: e.reduce_sum(out, in_, AX.X), reads, writes)

    def max8(out, in_, reads, writes):
        P.op("dve", lambda e: e.max(out=out, in_=in_), reads, writes)

    def mrepl(out, m8, in_, val, reads, writes):
        P.op("dve", lambda e: e.match_replace(out=out, in_to_replace=m8, in_values=in_, imm_value=val), reads, writes)

    def memset(eng, ap, val, writes, ws=()):
        P.op(eng, lambda e: e.memset(ap, val), (), writes, ws)

    def dma(eng, out, in_, reads, writes, ws=(), final=False):
        P.dma(eng, lambda e: e.dma_start(out=out, in_=in_), reads, writes, ws, final=final)

    def gather(out, src, idx_ap, reads, writes, ws=()):
        P.dma("pool", lambda e: e.indirect_dma_start(
            out=out, out_offset=None, in_=src,
            in_offset=bass.IndirectOffsetOnAxis(ap=idx_ap, axis=0)), reads, writes, ws)

    def scatter(dst, idx_ap, in_, reads, writes, ws=()):
        P.dma("pool", lambda e: e.indirect_dma_start(
            out=dst, out_offset=bass.IndirectOffsetOnAxis(ap=idx_ap, axis=0),
            in_=in_, in_offset=None), reads, writes, ws)

    x_ctx = din("x_ctx", [T, D])
    c_col = din("c_col", [128, 16])
    w_ada = din("w_ada", [D, 6 * D])
    b_ada_col = din("b_ada_col", [128, 96])
    b_ada_row = din("b_ada_row", [1, 6 * D])
    w_in = din("w_in", [D, IN_W])
    cmp_in = {}
    for kv in ("k", "v"):
        cmp_in[kv] = dict(pe=din("cmp_pe_" + kv, [32, 128]), w1=din("cmp_w1_" + kv, [4096, 256]),
                          b1=din("cmp_b1c_" + kv, [128, 2]), w2=din("cmp_w2_" + kv, [256, 128]))
    gla_w_gate = din("gla_w_gate", [16, 512])
    gla_b_gate = din("gla_b_gate", [1, 512])
    gla_nw_col = din("gla_nw_col", [128, 2])
    w_out = din("w_out", [D, D])
    ln1_g = din("ln1_g", [1, D])
    ln1_b = din("ln1_b", [1, D])
    w_router = din("w_router", [D, NE])
    b_router = din("b_router", [1, NE])
    w_gate_up = din("w_gate_up", [NE, D, 2 * D])
    b_gu_col = din("b_gu_col", [128, NE * 32])
    w_down = din("w_down", [NE, D, D])
    b_down = din("b_down", [NE, D])
    ln2_g = din("ln2_g", [1, D])
    ln2_b = din("ln2_b", [1, D])
    tbs_d = din("tbs", TBL["tbs"].shape, BF16)
    tba_d = din("tba", TBL["tba"].shape, BF16)
    tf_d = din("tf", TBL["tf"].shape, F32)
    tb5_d = din("tb5", TBL["tb5"].shape, BF16)
    out_d = nc.dram_tensor("out", [TO, D], F32, kind="ExternalOutput").ap()

    modrow_d = dscr("modrow_d", [4, D], F32)
    qT_d = dscr("qT_d", [1024, TO], BF16)
    kcmpT_d = dscr("kcmpT_d", [256, T], BF16)
    vcmpT_d = dscr("vcmpT_d", [256, T], BF16)
    kselT_d = dscr("kselT_d", [256, T], BF16)
    vsel_d = dscr("vsel_d", [T, 256], BF16)
    kwinT_d = dscr("kwinT_d", [256, T], BF16)
    vwin_d = dscr("vwin_d", [T, 256], BF16)
    gT_d = dscr("gT_d", [24, TO], F32)
    qglaT_d = dscr("qglaT_d", [512, TO], BF16)
    kglaT_d = dscr("kglaT_d", [512, T], BF16)
    kgla_d = dscr("kgla_d", [T, 512], BF16)
    vgla_d = dscr("vgla_d", [T, 1024], BF16)
    aT_d = dscr("aT_d", [16, T], F32)
    rT_d = dscr("rT_d", [1024, TO], BF16)
    yT_d = dscr("yT_d", [D, TO], BF16)
    x1_d = dscr("x1_d", [TO, D], F32)
    h2_d = dscr("h2_d", [TO, D], BF16)
    slotinfo_d = dscr("slotinfo_d", [NE * CAP, 2], F32)
    ybuf_d = [nc.dram_tensor("ybuf_d%d" % q_, [NE * CAP, 512], F32, kind="Internal").ap() for q_ in range(4)]

    tbs = sb("tbs_s", TBL["tbs"].shape, BF16)
    tf128 = sb("tf_s", TBL["tf"].shape, F32)
    sc1p = sb("sc1p", [128, 32])
    sc1v = sb("sc1v", [128, 32])
    kcT = sb("kcT", [128, 2, 512], BF16)
    vcs = sb("vcs", [128, 4, 2, 128], BF16)
    slot_i = sb("slot_i", [128, NQB, 4], I32)

    def TB(name, a=None, b=None):
        o, n = tbo[name]
        return tbs[:, o:o + n] if a is None else tbs[:, o + a:o + b]

    def TF(name, a=None, b=None):
        o, n = tfo[name]
        return tf128[:, o:o + n] if a is None else tf128[:, o + a:o + b]

    ps = [nc.alloc_psum_tensor("ps%d" % i, [128, 512], F32) for i in range(8)]
    PSR = [R("ps%d" % i) for i in range(8)]
    psb = [p_[:].bitcast(BF16) for p_ in ps]

    def ps3(b_):
        return ps[b_][:].rearrange("p (h q) -> p h q", h=4)

    dma("sp", tbs[:], tbs_d[:, :], [], [R("tbs")])
    dma("sp", tf128[:], tf_d[:, :], [], [R("tf")])
    identf = TF("identf")
    identb = TB("identb")
    onesb = TB("onesb")
    flagc = TF("flag")
    RTB, RTF = R("tbs"), R("tf")

    def bc4(ap2d):
        return ap2d.unsqueeze(1).to_broadcast([128, 4, 128])

    cc = al("cc", [128, 16])
    scc = al("scc", [128, 16])
    screp = al("screp", [128, 16, 128])
    bcol = al("bcol", [128, 96])
    modcol = al("modcol", [128, 32])
    modrep = al("modrep", [128, 4, D])
    wa = [al("wa%d" % i, [128, 16, 512]) for i in range(2)]
    brow_ = [al("brow%d" % i, [128, 512]) for i in range(2)]
    dma("sp", cc, c_col[:, :], [], [R("cc")])
    dma("sp", bcol, b_ada_col[:, :], [], [R("bcol")])
    act(scc, cc, AF.Silu, [R("cc")], [R("scc")])
    cp("dve", screp, scc.unsqueeze(2).to_broadcast([128, 16, 128]), [R("scc")], [R("screp")])
    for blk in range(24):
        w = wa[blk % 2]
        wr = R("wa%d" % (blk % 2))
        dma("sp", w, w_ada[:, blk * 512:(blk + 1) * 512].rearrange("(dc p) n -> p dc n", p=128), [], [wr])
        if blk < 8:
            for fc in range(4):
                col = blk * 4 + fc
                for dc in range(16):
                    mm(ps[0][:, col:col + 1], w[:, dc, fc * 128:(fc + 1) * 128], scc[:, dc:dc + 1],
                       dc == 0, dc == 15, [wr, R("scc")], [PSR[0]])
        else:
            br = brow_[blk % 2]
            brr = R("brow%d" % (blk % 2))
            dma("sp", br, b_ada_row[0:1, blk * 512:(blk + 1) * 512].partition_broadcast(128), [], [brr])
            pb = 1 + blk % 2
            for dc in range(16):
                mm(ps[pb][:], screp[:, dc, :], w[:, dc, :], dc == 0, dc == 15, [wr, R("screp")], [PSR[pb]])
            seg = (blk - 8) // 4
            off = ((blk - 8) % 4) * 512
            tt("dve", modrep[:, seg, off:off + 512], ps[pb][:], br, ALU.add, [PSR[pb], brr], [], ws=[R("modrep")])
    tt("dve", modcol, ps[0][:, 0:32], bcol[:, 0:32], ALU.add, [PSR[0], R("bcol")], [R("modcol")])
    ts("dve", sc1p[:, 0:16], modcol[:, 16:32], 1.0, None, ALU.add, None, [R("modcol")], [R("sc1p")])
    cp("dve", sc1p[:, 16:32], modcol[:, 0:16], [R("modcol"), R("sc1p")], [R("sc1p")])
    ts("dve", sc1v[:], sc1p[:], flagc[:, 0:1], None, ALU.mult, None, [R("sc1p"), RTF], [R("sc1v")])
    for seg in range(4):
        dma("sp", modrow_d[seg:seg + 1, :], modrep[0:1, seg, :], [R("modrep")], [], ws=[R("modrow_d")])

    if stages >= 1:
        areset()
        xs = al("xs", [128, 4, D])
        hT = al("hT", [128, 16, 1024], BF16)
        wb = [al("wb%d" % i, [128, 16, 512], BF16) for i in range(2)]
        stg = [al("stg%d" % i, [128, 512], BF16) for i in range(4)]
        stgf = [al("stgf%d" % i, [128, 512], F32) for i in range(2)]
        SQ = 128 ** -0.5
        blocks = [
            (0, 512, [("F", 0, 512, qT_d, 0, "q")], True),
            (512, 512, [("F", 0, 512, qT_d, 512, "q")], True),
            (1024, 512, [("F", 0, 256, kcmpT_d, 0, "c"), ("F", 256, 256, vcmpT_d, 0, "c")], False),
            (1536, 512, [("F", 0, 256, kselT_d, 0, "c"), ("T", 256, 256, vsel_d, 0, "c")], False),
            (2048, 512, [("F", 0, 256, kwinT_d, 0, "c"), ("T", 256, 256, vwin_d, 0, "c")], False),
            (2560, 24, [("F", 0, 24, gT_d, 0, "sig")], True),
            (2584, 512, [("F", 0, 512, qglaT_d, 0, "q")], True),
            (3096, 512, [("F", 0, 512, kglaT_d, 0, "c"), ("T", 0, 512, kgla_d, 0, "c")], False),
            (3608, 512, [("T", 0, 512, vgla_d, 0, "c")], False),
            (4120, 512, [("T", 0, 512, vgla_d, 512, "c")], False),
            (4632, 16, [("F", 0, 16, aT_d, 0, "f32")], False),
            (4648, 512, [("F", 0, 512, rT_d, 0, "silu")], True),
            (5160, 512, [("F", 0, 512, rT_d, 512, "silu")], True),
        ]
        wcnt = 0
        pcnt = 0
        scnt = 0
        for g in range(8):
            own = g >= 4
            scl = sc1p if own else sc1v
            sclr = R("sc1p") if own else R("sc1v")
            for hh in range(2):
                for tt_ in range(4):
                    r0 = g * 1024 + hh * 512 + tt_ * 128
                    dma("sp", xs[:, tt_, :], x_ctx[r0:r0 + 128, :], [], [R("xs%d" % tt_)])
                for dc in range(16):
                    pb = pcnt % 4
                    pcnt += 1
                    for tt_ in range(4):
                        trp(ps[pb][:, tt_ * 128:(tt_ + 1) * 128], xs[:, tt_, dc * 128:(dc + 1) * 128], identf,
                            [R("xs%d" % tt_), RTF], [PSR[pb]])
                    act(hT[:, dc, hh * 512:(hh + 1) * 512], ps[pb][:], AF.Identity, [PSR[pb], sclr], [],
                        bias=scl[:, 16 + dc:17 + dc], scale=scl[:, dc:dc + 1], ws=[R("hT")])
            for (c0, ncol, subs, own_only) in blocks:
                if own_only and not own:
                    continue
                w = wb[wcnt % 2]
                wr = R("wb%d" % (wcnt % 2))
                wcnt += 1
                dma("pool", w[:, :, 0:ncol], w_in[:, c0:c0 + ncol].rearrange("(dc p) n -> p dc n", p=128), [], [wr])
                for (mode, so, sn, dst, doff, ev) in subs:
                    if mode == "F":
                        for fc in range((sn + 127) // 128):
                            nf = min(128, sn - fc * 128)
                            for hh in range(2):
                                pb = 4 + pcnt % 4
                                pcnt += 1
                                for dc in range(16):
                                    mm(ps[pb][0:nf, :], w[:, dc, so + fc * 128:so + fc * 128 + nf],
                                       hT[:, dc, hh * 512:(hh + 1) * 512], dc == 0, dc == 15, [wr, R("hT")], [PSR[pb]])
                                tcol = (g * 1024 + hh * 512) - (TO if own_only else 0)
                                frow = doff + fc * 128
                                if ev in ("sig", "f32"):
                                    s_ = stgf[scnt % 2]
                                    sr = R("stgf%d" % (scnt % 2))
                                else:
                                    s_ = stg[scnt % 4]
                                    sr = R("stg%d" % (scnt % 4))
                                scnt += 1
                                if ev == "q":
                                    act(s_[0:nf, :], ps[pb][0:nf, :], AF.Identity, [PSR[pb]], [sr], scale=SQ)
                                elif ev == "sig":
                                    act(s_[0:nf, :], ps[pb][0:nf, :], AF.Sigmoid, [PSR[pb]], [sr])
                                elif ev == "silu":
                                    act(s_[0:nf, :], ps[pb][0:nf, :], AF.Silu, [PSR[pb]], [sr])
                                elif scnt % 2 == 0:
                                    act(s_[0:nf, :], ps[pb][0:nf, :], AF.Copy, [PSR[pb]], [sr])
                                else:
                                    cp("dve", s_[0:nf, :], ps[pb][0:nf, :], [PSR[pb]], [sr])
                                dma("sp", dst[frow:frow + nf, tcol:tcol + 512], s_[0:nf, :], [sr], [],
                                    ws=[R(dst.name)])
                    else:
                        for tt_ in range(8):
                            pb = 4 + pcnt % 4
                            pcnt += 1
                            for dc in range(16):
                                mm(ps[pb][:, 0:sn], hT[:, dc, tt_ * 128:(tt_ + 1) * 128], w[:, dc, so:so + sn],
                                   dc == 0, dc == 15, [wr, R("hT")], [PSR[pb]])
                            s_ = stg[scnt % 4]
                            sr = R("stg%d" % (scnt % 4))
                            scnt += 1
                            if scnt % 2 == 0:
                                act(s_[:, 0:sn], ps[pb][:, 0:sn], AF.Copy, [PSR[pb]], [sr])
                            else:
                                cp("dve", s_[:, 0:sn], ps[pb][:, 0:sn], [PSR[pb]], [sr])
                            t0 = g * 1024 + tt_ * 128
                            dma("sp", dst[t0:t0 + 128, doff:doff + sn], s_[:, 0:sn], [sr], [], ws=[R(dst.name)])

    if stages >= 2:
        areset()
        kTc = al("kTc", [128, 2, T], BF16)
        w1 = al("w1", [128, 32, 256], BF16)
        pes = al("pes", [32, 128])
        peT = al("peT", [128, 32], BF16)
        b1c = al("b1c", [128, 2])
        w2 = al("w2", [128, 2, 128], BF16)
        biasc = al("biasc", [128, 2])
        u = [al("u%d" % i, [128, 512]) for i in range(2)]
        u2 = [al("u2%d" % i, [128, 512]) for i in range(2)]
        gel = al("gel", [128, 2, 512], BF16)
        memset("pool", gel[:, :, 511:512], 0.0, [R("gel")])
        memset("pool", kcT[:, :, 511:512], 0.0, [R("kcT")])
        for kv in ("k", "v"):
            ci = cmp_in[kv]
            src_d = kcmpT_d if kv == "k" else vcmpT_d
            dma("sp", kTc, src_d.rearrange("(g p) t -> p g t", p=128), [R(src_d.name)], [R("kTc")])
            dma("pool", w1, ci["w1"].rearrange("(l p) n -> p l n", p=128), [], [R("w1")])
            dma("pool", w2, ci["w2"].rearrange("(c p) n -> p c n", p=128), [], [R("w2")])
            dma("sp", pes, ci["pe"][:, :], [], [R("pes")])
            dma("sp", b1c, ci["b1"][:, :], [], [R("b1c")])
            trp(ps[0][:, 0:32], pes[0:32, :], identf[0:32, 0:32], [R("pes"), RTF], [PSR[0]])
            cp("dve", peT, ps[0][:, 0:32], [PSR[0]], [R("peT")])
            for hc in range(2):
                for l in range(32):
                    mm(ps[1][:, hc:hc + 1], w1[:, l, hc * 128:(hc + 1) * 128], peT[:, l:l + 1],
                       l == 0, l == 31, [R("w1"), R("peT")], [PSR[1]])
            tt("dve", biasc, ps[1][:, 0:2], b1c, ALU.add, [PSR[1], R("b1c")], [R("biasc")])
            for g in range(2):
                for hc in range(2):
                    pb = 2 + hc
                    for l in range(32):
                        mm(ps[pb][:, 0:511], w1[:, l, hc * 128:(hc + 1) * 128], kTc[:, g, l:l + 16 * 510 + 1:16],
                           l == 0, l == 31, [R("w1"), R("kTc")], [PSR[pb]])
                    uu, uu2 = u[hc], u2[hc]
                    ru, ru2 = R("u%d" % hc), R("u2%d" % hc)
                    act(uu[:, 0:511], ps[pb][:, 0:511], AF.Identity, [PSR[pb], R("biasc")], [ru],
                        bias=biasc[:, hc:hc + 1])
                    tt("dve", uu2[:, 0:511], uu[:, 0:511], uu[:, 0:511], ALU.mult, [ru], [ru2])
                    tt("dve", uu2[:, 0:511], uu2[:, 0:511], uu[:, 0:511], ALU.mult, [ru, ru2], [ru2])
                    stt("dve", uu2[:, 0:511], uu2[:, 0:511], 0.044715, uu[:, 0:511], ALU.mult, ALU.add, [ru, ru2], [ru2])
                    act(uu2[:, 0:511], uu2[:, 0:511], AF.Tanh, [ru2], [ru2], scale=0.7978845608028654)
                    ts("dve", uu2[:, 0:511], uu2[:, 0:511], 1.0, 0.5, ALU.add, ALU.mult, [ru2], [ru2])
                    tt("dve", gel[:, hc, 0:511], uu2[:, 0:511], uu[:, 0:511], ALU.mult, [ru, ru2], [], ws=[R("gel")])
                if kv == "k":
                    for hc in range(2):
                        mm(ps[4][:, 0:511], w2[:, hc, :], gel[:, hc, 0:511], hc == 0, hc == 1,
                           [R("w2"), R("gel")], [PSR[4]])
                    cp("act", kcT[:, g, 0:511], ps[4][:, 0:511], [PSR[4]], [], ws=[R("kcT")])
                else:
                    for ct in range(4):
                        for hc in range(2):
                            mm(ps[4][:, ct * 128:(ct + 1) * 128], gel[:, hc, ct * 128:(ct + 1) * 128], w2[:, hc, :],
                               hc == 0, hc == 1, [R("w2"), R("gel")], [PSR[4]])
                    cp("act", vcs[:, :, g, :], ps[4][:].rearrange("p (c d) -> p c d", c=4), [PSR[4]], [],
                       ws=[R("vcs")])
                memset("pool", gel[:, :, 511:512], 0.0, [R("gel")])

    if stages >= 3:
        areset()
        kselT = al("kselT", [128, 2, T], BF16)
        vsel = al("vsel", [128, 64, 256], BF16)
        tba = al("tba", TBL["tba"].shape, BF16)
        tb5 = al("tb5", TBL["tb5"].shape, BF16)
        dma("sp", kselT, kselT_d.rearrange("(g p) t -> p g t", p=128), [R("kselT_d")], [R("kselT")])
        dma("sp", vsel, vsel_d.rearrange("(kt p) c -> p kt c", p=128), [R("vsel_d")], [R("vsel")])
        dma("sp", tba, tba_d[:, :], [], [R("tba")])
        dma("sp", tb5, tb5_d[:, :], [], [R("tb5")])
        RKS, RVS, RTA, RT5 = R("kselT"), R("vsel"), R("tba"), R("tb5")

        def TA(name, a, b):
            o, n = tao[name]
            return tba[:, o + a:o + b]

        def T5(name, a, b, rows=5):
            o, n = t5o[name]
            return tb5[0:rows, o + a:o + b]

        qTb = [al("qTb%d" % i, [128, 8, 128], BF16) for i in range(2)]
        kwT = [al("kwT%d" % i, [128, 2, 640], BF16) for i in range(2)]
        vw = [al("vw%d" % i, [128, 5, 256], BF16) for i in range(2)]
        grep = [al("grep%d" % i, [128, 12, 128]) for i in range(2)]
        PTc = [al("PTc%d" % i, [128, 512], BF16) for i in range(4)]
        Pn = [al("Pn%d" % i, [128, 512], BF16) for i in range(4)]
        PTr = [al("PTr%d" % i, [128, 512], BF16) for i in range(3)]
        browS = [al("browS%d" % i, [5, 512], BF16) for i in range(2)]
        browC = [al("browC%d" % i, [5, 512], BF16) for i in range(2)]
        Rz = [al("Rz%d" % i, [128, 512]) for i in range(2)]
        Rg = [al("Rg%d" % i, [128, 512]) for i in range(2)]
        tmpo = [al("tmpo%d" % i, [128, 512]) for i in range(2)]
        acc = [al("acc%d" % i, [128, 512]) for i in range(2)]
        accb = [al("accb%d" % i, [128, 512], BF16) for i in range(2)]
        iw = al("iw", [128, 128])
        iw2 = al("iw2", [128, 128])
        m8a = al("m8a", [128, 8])
        m8b = al("m8b", [128, 8])
        mb = al("mb", [128, 128], BF16)
        mbT = [al("mbT%d" % i, [128, 128], BF16) for i in range(2)]
        cnt = {"pt": 0, "s": 0, "oz": 0, "ep": 0}

        def score_and_pv(mms, PT, PTres, vl, vreads, ob, zb, first, last):
            sbk = cnt["s"] % 2
            cnt["s"] += 1
            for n_, (l_, r_, rd_) in enumerate(mms):
                mm(ps3(sbk), l_, r_, n_ == 0, n_ == len(mms) - 1, rd_, [PSR[sbk]])
            act(PT, ps[sbk][:], AF.Exp, [PSR[sbk]], [PTres])
            mm(ps[ob][:], vl, PT, first, last, vreads + [PTres], [PSR[ob]])
            mm(ps[zb][:], onesb, PT, first, last, [RTB, PTres], [PSR[zb]])

        def epilogue(br, ob, zb, grp, grpr, ac, acr, first_branch, cmp_pts=None):
            k = cnt["ep"] % 2
            cnt["ep"] += 1
            rz, rg, tm = Rz[k], Rg[k], tmpo[k]
            rzr, rgr, tmr = R("Rz%d" % k), R("Rg%d" % k), R("tmpo%d" % k)
            ts("dve", rz, ps[zb][:], 1e-30, None, ALU.add, None, [PSR[zb]], [rzr])
            recip(rz, rz, [rzr], [rzr])
            if cmp_pts is not None:
                for (j, ptc, ptr, pn, pnr) in cmp_pts:
                    tt("pool", pn, ptc, rz, ALU.mult, [ptr, rzr], [pnr])
            tt("pool", rg.rearrange("p (h q) -> p h q", h=4), rz.rearrange("p (h q) -> p h q", h=4),
               grp[:, br::3, :], ALU.mult, [rzr, grpr], [rgr])
            if first_branch:
                tt("dve", ac, ps[ob][:], rg, ALU.mult, [PSR[ob], rgr], [acr])
            else:
                tt("dve", tm, ps[ob][:], rg, ALU.mult, [PSR[ob], rgr], [tmr])
                tt("pool", ac, ac, tm, ALU.add, [tmr, acr], [acr])

        for i in range(NQB):
            q0 = 4096 + 128 * i
            oc = 128 * i
            b2 = i % 2
            qt, kw_, vw_ = qTb[b2], kwT[b2], vw[b2]
            rq, rkw, rvw = R("qTb%d" % b2), R("kwT%d" % b2), R("vw%d" % b2)
            dma("sp", qt, qT_d[:, oc:oc + 128].rearrange("(h p) q -> p h q", p=128), [R("qT_d")], [rq])
            dma("sp", kw_, kwinT_d[:, q0 - 512:q0 + 128].rearrange("(g p) t -> p g t", p=128), [R("kwinT_d")], [rkw])
            dma("sp", vw_, vwin_d[q0 - 512:q0 + 128, :].rearrange("(kt p) c -> p kt c", p=128), [R("vwin_d")], [rvw])
            for g in range(2):
                ig = 2 * i + g
                k2 = ig % 2
                gp, gpr = grep[k2], R("grep%d" % k2)
                dma("sp", gp, gT_d[12 * g:12 * g + 12, oc:oc + 128].partition_broadcast(128), [R("gT_d")], [gpr])
                bS, bSr = browS[k2], R("browS%d" % k2)
                bC, bCr = browC[k2], R("browC%d" % k2)
                cp("pool", bS.rearrange("p (h q) -> p h q", h=4),
                   T5("src", i * 8 + 4 * g, i * 8 + 4 * g + 4).unsqueeze(2).to_broadcast([5, 4, 128]), [RT5], [bSr])
                cp("pool", bS[0:1, :], T5("qrow", g * 512, (g + 1) * 512, rows=1), [RT5, bSr], [bSr])
                cp("pool", bC.rearrange("p (h q) -> p h q", h=4),
                   T5("srcc", i * 8 + 4 * g, i * 8 + 4 * g + 4).unsqueeze(2).to_broadcast([5, 4, 128]), [RT5], [bCr])
                cp("pool", bC[0:1, :], T5("qrow", g * 512, (g + 1) * 512, rows=1), [RT5, bCr], [bCr])
                bS3 = bS.rearrange("p (h q) -> p h q", h=4)
                bC3 = bC.rearrange("p (h q) -> p h q", h=4)
                rhsQ = qt[:, 4 * g:4 * g + 4, :]
                ac, acr = acc[k2], R("acc%d" % k2)
                ob, zb = (2, 3) if cnt["oz"] % 2 == 0 else (4, 5)
                cnt["oz"] += 1
                js = [j for j in range(4) if 4065 + 128 * i - 2048 * j + 127 >= 0]
                cmp_pts = []
                for n_, j in enumerate(js):
                    mms = [(kcT[:, g, j * 128:(j + 1) * 128], rhsQ, [R("kcT"), rq]),
                           (T5("posrows_c", j * 128, (j + 1) * 128), bC3, [RT5, bCr])]
                    if (i, j) in CMP_PART:
                        t_ = CMP_PART[(i, j)]
                        mms.append((identb, bc4(TA("cmpm", t_ * 128, (t_ + 1) * 128)), [RTB, RTA]))
                    score_and_pv(mms, PTc[j], R("PTc%d" % j), vcs[:, j, g, :], [R("vcs")], ob, zb,
                                 n_ == 0, n_ == len(js) - 1)
                    cmp_pts.append((j, PTc[j], R("PTc%d" % j), Pn[j], R("Pn%d" % j)))
                epilogue(0, ob, zb, gp, gpr, ac, acr, True, cmp_pts)
                nmm = 4 * len(js)
                c_ = 0
                for hh in range(4):
                    for j in js:
                        mm(ps[6][:, 0:128], Pn[j][:, hh * 128:(hh + 1) * 128], TA("ovl", j * 128, (j + 1) * 128),
                           c_ == 0, c_ == nmm - 1, [R("Pn%d" % j), RTA], [PSR[6]])
                        c_ += 1
                tt("dve", iw, ps[6][:, 0:128], TF("tblA", 64 - 2 * i, 192 - 2 * i), ALU.mult, [PSR[6], RTF], [R("iw")])
                tt("dve", iw, iw, TF("tblB", 64 - 2 * i, 192 - 2 * i), ALU.add, [R("iw"), RTF], [R("iw")])
                tt("dve", iw, iw, TF("F0"), ALU.max, [R("iw"), RTF], [R("iw")])
                max8(m8a, iw, [R("iw")], [R("m8a")])
                mrepl(iw2, m8a, iw, -3e9, [R("iw"), R("m8a")], [R("iw2")])
                max8(m8b, iw2, [R("iw2")], [R("m8b")])
                ts("dve", mb, iw, m8b[:, 7:8], NEG, ALU.is_lt, ALU.mult, [R("iw"), R("m8b")], [R("mb")])
                trp(psb[6][:, 512:640], mb, identb, [R("mb"), RTB], [PSR[6]])
                mt, mtr = mbT[k2], R("mbT%d" % k2)
                cp("act", mt, psb[6][:, 512:640], [PSR[6]], [mtr])
                ob, zb = (2, 3) if cnt["oz"] % 2 == 0 else (4, 5)
                cnt["oz"] += 1
                for t_ in range(5):
                    kt = 28 + i + t_
                    mms = [(kw_[:, g, t_ * 128:(t_ + 1) * 128], rhsQ, [rkw, rq]),
                           (T5("posrows", kt * 128, (kt + 1) * 128), bS3, [RT5, bSr])]
                    if t_ == 0:
                        mms.append((identb, bc4(TB("band")), [RTB]))
                    if t_ == 4:
                        mms.append((identb, bc4(TB("tri")), [RTB]))
                    k3 = cnt["pt"] % 3
                    cnt["pt"] += 1
                    score_and_pv(mms, PTr[k3], R("PTr%d" % k3), vw_[:, t_, g * 128:(g + 1) * 128], [rvw], ob, zb,
                                 t_ == 0, t_ == 4)
                epilogue(2, ob, zb, gp, gpr, ac, acr, False)
                ob, zb = (2, 3) if cnt["oz"] % 2 == 0 else (4, 5)
                cnt["oz"] += 1
                nkt = 33 + i
                for kt in range(nkt):
                    mms = [(kselT[:, g, kt * 128:(kt + 1) * 128], rhsQ, [RKS, rq]),
                           (T5("posrows", kt * 128, (kt + 1) * 128), bS3, [RT5, bSr]),
                           (TA("E", kt * 128, (kt + 1) * 128), bc4(mt), [RTA, mtr])]
                    if kt == nkt - 1:
                        mms.append((identb, bc4(TB("tri")), [RTB]))
                    k3 = cnt["pt"] % 3
                    cnt["pt"] += 1
                    score_and_pv(mms, PTr[k3], R("PTr%d" % k3), vsel[:, kt, g * 128:(g + 1) * 128], [RVS], ob, zb,
                                 kt == 0, kt == nkt - 1)
                epilogue(1, ob, zb, gp, gpr, ac, acr, False)
                ab, abr = accb[k2], R("accb%d" % k2)
                cp("act", ab, ac, [acr], [abr])
                dma("sp", yT_d[g * 512:(g + 1) * 512, oc:oc + 128].rearrange("(h p) q -> p h q", p=128),
                    ab.rearrange("p (h q) -> p h q", h=4), [abr], [], ws=[R("yT_d")])

    if stages >= 4:
        areset()
        wg = al("wg", [16, 512])
        bg = al("bg", [1, 512])
        nwc = al("nwc", [128, 2])
        dma("sp", wg, gla_w_gate[:, :], [], [R("wg")])
        dma("sp", bg, gla_b_gate[:, :], [], [R("bg")])
        dma("sp", nwc, gla_nw_col[:, :], [], [R("nwc")])
        St = [al("St%d" % h_, [128, 256]) for h_ in range(4)]
        Sb = [al("Sb%d" % h_, [128, 256], BF16) for h_ in range(4)]
        for h_ in range(4):
            memset("pool", St[h_], 0.0, [R("St%d" % h_)])
            memset("pool", Sb[h_], 0.0, [R("Sb%d" % h_)])
        kT4 = [al("kT4%d" % i, [128, 4, 128], BF16) for i in range(2)]
        ktok = [al("ktok%d" % i, [128, 512], BF16) for i in range(2)]
        vtok = [al("vtok%d" % i, [128, 1024], BF16) for i in range(2)]
        aTs = [al("aTs%d" % i, [16, 128]) for i in range(2)]
        qT4 = [al("qT4%d" % i, [128, 4, 128], BF16) for i in range(2)]
        rT8 = [al("rT8%d" % i, [128, 8, 128], BF16) for i in range(2)]
        la0 = al("la0", [128, 512])
        la = al("la", [128, 512])
        ebT = al("ebT", [128, 512])
        enbT = al("enbT", [128, 512])
        erev = al("erev", [128, 512])
        ktil = al("ktil", [128, 4, 128], BF16)
        kk = al("kk", [128, 512], BF16)
        qtil = al("qtil", [128, 4, 128], BF16)
        PTg = al("PTg", [128, 4, 128], BF16)
        sqa = [al("sq%d" % i, [128, 512], BF16) for i in range(2)]
        rs = al("rs", [128, 512])
        ya = [al("ya%d" % i, [128, 512]) for i in range(2)]
        ystg = [al("ystg%d" % i, [128, 4, 128], BF16) for i in range(2)]
        onecol = TF("onesf", 0, 1)
        for ch in range(64):
            c0 = ch * 128
            own = ch >= 32
            oc = c0 - TO
            b2 = ch % 2
            k4, kt_, vt_, at_ = kT4[b2], ktok[b2], vtok[b2], aTs[b2]
            rk4, rkt, rvt, rat = R("kT4%d" % b2), R("ktok%d" % b2), R("vtok%d" % b2), R("aTs%d" % b2)
            dma("sp", k4, kglaT_d[:, c0:c0 + 128].rearrange("(h p) t -> p h t", p=128), [R("kglaT_d")], [rk4])
            dma("sp", kt_, kgla_d[c0:c0 + 128, :], [R("kgla_d")], [rkt])
            dma("sp", vt_, vgla_d[c0:c0 + 128, :], [R("vgla_d")], [rvt])
            dma("sp", at_, aT_d[:, c0:c0 + 128], [R("aT_d")], [rat])
            if own:
                q4, r8 = qT4[b2], rT8[b2]
                rq4, rr8 = R("qT4%d" % b2), R("rT8%d" % b2)
                dma("sp", q4, qglaT_d[:, oc:oc + 128].rearrange("(h p) t -> p h t", p=128), [R("qglaT_d")], [rq4])
                dma("sp", r8, rT_d[:, oc:oc + 128].rearrange("(j p) t -> p j t", p=128), [R("rT_d")], [rr8])
            mm(ps[0][:], at_[0:16, :], wg[0:16, :], True, False, [rat, R("wg")], [PSR[0]])
            mm(ps[0][:], TF("onesf")[0:1, 0:128], bg[0:1, :], False, True, [RTF, R("bg")], [PSR[0]])
            act(la0, ps[0][:], AF.Exp, [PSR[0]], [R("la0")], scale=-1.0)
            act(la, la0, AF.Ln, [R("la0"), RTF], [R("la")], bias=onecol)
            for h_ in range(4):
                mm(ps[1][:, h_ * 128:(h_ + 1) * 128], la[:, h_ * 128:(h_ + 1) * 128], TF("triI"), True, True,
                   [R("la"), RTF], [PSR[1]])
            mm(ps[2][:], TF("triR"), la, True, True, [R("la"), RTF], [PSR[2]])
            act(ebT, ps[1][:], AF.Exp, [PSR[1]], [R("ebT")])
            act(enbT, ps[1][:], AF.Exp, [PSR[1]], [R("enbT")], scale=-1.0)
            act(erev, ps[2][:], AF.Exp, [PSR[2]], [R("erev")])
            tt("dve", ktil.rearrange("p h t -> p (h t)"), k4.rearrange("p h t -> p (h t)"), enbT, ALU.mult,
               [rk4, R("enbT")], [R("ktil")])
            tt("pool", kk, kt_, erev, ALU.mult, [rkt, R("erev")], [R("kk")])
            if own:
                tt("dve", qtil.rearrange("p h t -> p (h t)"), q4.rearrange("p h t -> p (h t)"), ebT, ALU.mult,
                   [rq4, R("ebT")], [R("qtil")])
                for h_ in range(4):
                    mm(ps[3][:, h_ * 128:(h_ + 1) * 128], ktil[:, h_, :], qtil[:, h_, :], True, True,
                       [R("ktil"), R("qtil")], [PSR[3]])
                tt("dve", PTg, ps3(3), bc4(TB("cmask")), ALU.mult, [PSR[3], RTB], [R("PTg")])
                for dvc in range(2):
                    pb = 4 + dvc
                    for h_ in range(4):
                        mm(ps[pb][:, h_ * 128:(h_ + 1) * 128], vt_[:, h_ * 256 + dvc * 128:h_ * 256 + (dvc + 1) * 128],
                           PTg[:, h_, :], True, False, [rvt, R("PTg")], [PSR[pb]])
                        mm(ps[pb][:, h_ * 128:(h_ + 1) * 128], Sb[h_][:, dvc * 128:(dvc + 1) * 128], qtil[:, h_, :],
                           False, True, [R("Sb%d" % h_), R("qtil")], [PSR[pb]])
                    act(sqa[dvc], ps[pb][:], AF.Square, [PSR[pb]], [R("sq%d" % dvc)])
                mm(ps[6][:], onesb, sqa[0], True, False, [RTB, R("sq0")], [PSR[6]])
                mm(ps[6][:], onesb, sqa[1], False, True, [RTB, R("sq1")], [PSR[6]])
                ts("dve", rs, ps[6][:], 1.0 / 256, RMS_EPS, ALU.mult, ALU.add, [PSR[6]], [R("rs")])
                act(rs, rs, AF.Sqrt, [R("rs")], [R("rs")])
                recip(rs, rs, [R("rs")], [R("rs")])
                for dvc in range(2):
                    pb = 4 + dvc
                    y_, yr = ya[dvc], R("ya%d" % dvc)
                    tt("dve", y_, ps[pb][:], rs, ALU.mult, [PSR[pb], R("rs")], [yr])
                    ys, ysr = ystg[dvc], R("ystg%d" % dvc)
                    stt("dve", ys, y_.rearrange("p (h t) -> p h t", h=4), nwc[:, dvc:dvc + 1], r8[:, dvc::2, :],
                        ALU.mult, ALU.mult, [yr, R("nwc"), rr8], [ysr])
                    dst = yT_d[1024:2048, oc:oc + 128].rearrange("(h c p) t -> p h c t", c=2, p=128)[:, :, dvc, :]
                    dma("sp", dst, ys, [ysr], [], ws=[R("yT_d")])
            for hp in range(2):
                for hq in range(2):
                    h_ = 2 * hp + hq
                    mm(ps[7][:, hq * 256:(hq + 1) * 256], kk[:, h_ * 128:(h_ + 1) * 128], vt_[:, h_ * 256:(h_ + 1) * 256],
                       True, True, [R("kk"), rvt], [PSR[7]])
                for hq in range(2):
                    h_ = 2 * hp + hq
                    stt("dve", St[h_], St[h_], ebT[:, h_ * 128 + 127:h_ * 128 + 128], ps[7][:, hq * 256:(hq + 1) * 256],
                        ALU.mult, ALU.add, [R("ebT"), PSR[7], R("St%d" % h_)], [R("St%d" % h_)])
                    cp("act", Sb[h_], St[h_], [R("St%d" % h_)], [R("Sb%d" % h_)])

    if stages >= 5:
        areset()
        wo = al("wo", [128, 16, D], BF16)
        dma("pool", wo, w_out.rearrange("(c p) n -> p c n", p=128), [], [R("wo")])
        g1r = al("g1r", [128, D])
        l1g = al("l1g", [128, D])
        l1b = al("l1b", [128, D])
        sc2r = al("sc2r", [128, D])
        sh2r = al("sh2r", [128, D])
        dma("sp", g1r, modrow_d[0:1, :].partition_broadcast(128), [R("modrow_d")], [R("g1r")])
        dma("sp", sh2r, modrow_d[1:2, :].partition_broadcast(128), [R("modrow_d")], [R("sh2r")])
        dma("sp", sc2r, modrow_d[2:3, :].partition_broadcast(128), [R("modrow_d")], [R("sc2r")])
        dma("sp", l1g, ln1_g[0:1, :].partition_broadcast(128), [], [R("l1g")])
        dma("sp", l1b, ln1_b[0:1, :].partition_broadcast(128), [], [R("l1b")])
        ts("dve", sc2r, sc2r, 1.0, None, ALU.add, None, [R("sc2r")], [R("sc2r")])
        wrt = al("wrt", [128, 16, NE])
        brt = al("brt", [128, NE])
        dma("sp", wrt, w_router.rearrange("(c p) e -> p c e", p=128), [], [R("wrt")])
        dma("sp", brt, b_router[0:1, :].partition_broadcast(128), [], [R("brt")])
        yTt = [al("yTt%d" % i, [128, 16, 128], BF16) for i in range(2)]
        xt = [al("xt%d" % i, [128, D]) for i in range(2)]
        t1 = al("t1", [128, D])
        vv = al("vv", [128, D])
        sqv = al("sqv", [128, D])
        h2f = al("h2f", [128, D])
        h2b = al("h2b", [128, D], BF16)
        h2T = al("h2T", [128, 16, 128])
        st = al("st", [128, 16])
        lg = al("lg", [128, NE])
        m8r = al("m8r", [128, 8])
        w4 = al("w4", [128, 8])
        maskf = al("maskf", [128, NE])
        maskb = al("maskb", [128, NE], BF16)
        csum = al("csum", [128, NE])
        posv = al("posv", [128, NE])
        oh = al("oh", [128, NE])
        slf = al("slf", [128, 4])
        sinfo = [al("sinfo%d" % k, [128, 2]) for k in range(4)]
        zt = al("zt", [128, 1024])
        memset("pool", zt, 0.0, [R("zt")])
        memset("pool", csum, 0.0, [R("csum")])
        dma("sp", slotinfo_d.rearrange("(p a) c -> p (a c)", p=128), zt, [R("zt")], [R("slotinfo_d")])

        def layer_norm(src, srcr, dst, dstr, gam, gamr, bet, betr):
            rsum(st[:, 0:1], src, [srcr], [R("st")])
            tt("pool", sqv, src, src, ALU.mult, [srcr], [R("sqv")])
            rsum(st[:, 1:2], sqv, [R("sqv"), R("st")], [R("st")])
            ts("dve", st[:, 2:3], st[:, 0:1], 1.0 / D, None, ALU.mult, None, [R("st")], [R("st")])
            tt("dve", st[:, 3:4], st[:, 2:3], st[:, 2:3], ALU.mult, [R("st")], [R("st")])
            stt("dve", st[:, 4:5], st[:, 1:2], 1.0 / D, st[:, 3:4], ALU.mult, ALU.subtract, [R("st")], [R("st")])
            ts("dve", st[:, 4:5], st[:, 4:5], LN_EPS, None, ALU.add, None, [R("st")], [R("st")])
            act(st[:, 5:6], st[:, 4:5], AF.Sqrt, [R("st")], [R("st")])
            recip(st[:, 6:7], st[:, 5:6], [R("st")], [R("st")])
            ts("dve", dst, src, st[:, 2:3], st[:, 6:7], ALU.subtract, ALU.mult, [srcr, R("st")], [dstr])
            tt("pool", dst, dst, gam, ALU.mult, [dstr, gamr], [dstr])
            tt("dve", dst, dst, bet, ALU.add, [dstr, betr], [dstr])

        for tl in range(NQB):
            r0 = tl * 128
            b2 = tl % 2
            yt_, x_ = yTt[b2], xt[b2]
            ryt, rx = R("yTt%d" % b2), R("xt%d" % b2)
            dma("sp", yt_, yT_d[:, r0:r0 + 128].rearrange("(j p) t -> p j t", p=128), [R("yT_d")], [ryt])
            dma("sp", x_, x_ctx[TO + r0:TO + r0 + 128, :], [], [rx])
            for nb in range(4):
                for fc in range(16):
                    mm(ps[nb][:], yt_[:, fc, :], wo[:, fc, nb * 512:(nb + 1) * 512], fc == 0, fc == 15,
                       [ryt, R("wo")], [PSR[nb]])
                tt("dve", t1[:, nb * 512:(nb + 1) * 512], ps[nb][:], g1r[:, nb * 512:(nb + 1) * 512], ALU.mult,
                   [PSR[nb], R("g1r")], [], ws=[R("t1")])
            stt("dve", vv, x_, DN_ALPHA, t1, ALU.mult, ALU.add, [rx, R("t1")], [R("vv")])
            layer_norm(vv, R("vv"), t1, R("t1"), l1g, R("l1g"), l1b, R("l1b"))
            dma("sp", x1_d[r0:r0 + 128, :], t1, [R("t1")], [], ws=[R("x1_d")])
            tt("pool", h2f, t1, sc2r, ALU.mult, [R("t1"), R("sc2r")], [R("h2f")])
            tt("dve", h2f, h2f, sh2r, ALU.add, [R("h2f"), R("sh2r")], [R("h2f")])
            cp("act", h2b, h2f, [R("h2f")], [R("h2b")])
            dma("sp", h2_d[r0:r0 + 128, :], h2b, [R("h2b")], [], ws=[R("h2_d")])
            for q4_ in range(4):
                pb = 4 + q4_
                for k_ in range(4):
                    dc = q4_ * 4 + k_
                    trp(ps[pb][:, k_ * 128:(k_ + 1) * 128], h2f[:, dc * 128:(dc + 1) * 128], identf,
                        [R("h2f"), RTF], [PSR[pb]])
                if q4_ % 2 == 0:
                    cp("act", h2T[:, q4_ * 4:(q4_ + 1) * 4, :], ps3(pb), [PSR[pb]], [], ws=[R("h2T")])
                else:
                    cp("dve", h2T[:, q4_ * 4:(q4_ + 1) * 4, :], ps3(pb), [PSR[pb]], [], ws=[R("h2T")])
            for dc in range(16):
                mm(ps[4][:, 0:NE], h2T[:, dc, :], wrt[:, dc, :], dc == 0, dc == 15, [R("h2T"), R("wrt")], [PSR[4]])
            tt("dve", lg, ps[4][:, 0:NE], brt, ALU.add, [PSR[4], R("brt")], [R("lg")])
            max8(m8r, lg, [R("lg")], [R("m8r")])
            ts("dve", w4[:, 0:4], m8r[:, 0:4], m8r[:, 0:1], None, ALU.subtract, None, [R("m8r")], [R("w4")])
            act(w4[:, 0:4], w4[:, 0:4], AF.Exp, [R("w4")], [R("w4")])
            rsum(w4[:, 4:5], w4[:, 0:4], [R("w4")], [R("w4")])
            recip(w4[:, 5:6], w4[:, 4:5], [R("w4")], [R("w4")])
            ts("dve", w4[:, 0:4], w4[:, 0:4], w4[:, 5:6], None, ALU.mult, None, [R("w4")], [R("w4")])
            ts("dve", maskf, lg, m8r[:, 3:4], None, ALU.is_ge, None, [R("lg"), R("m8r")], [R("maskf")])
            cp("dve", maskb, maskf, [R("maskf")], [R("maskb")])
            mm(ps[5][:, 0:NE], TB("ltri"), maskb, True, True, [RTB, R("maskb")], [PSR[5]])
            mm(ps[5][:, NE:2 * NE], onesb, maskb, True, True, [RTB, R("maskb")], [PSR[5]])
            tt("dve", posv, ps[5][:, 0:NE], csum, ALU.add, [PSR[5], R("csum")], [R("posv")])
            tt("dve", csum, csum, ps[5][:, NE:2 * NE], ALU.add, [PSR[5], R("csum")], [R("csum")])
            tt("dve", posv, posv, TF("iotaEC1"), ALU.add, [R("posv"), RTF], [R("posv")])
            for k_ in range(4):
                ts("dve", oh, lg, m8r[:, k_:k_ + 1], None, ALU.is_equal, None, [R("lg"), R("m8r")], [R("oh")])
                tt("dve", oh, oh, posv, ALU.mult, [R("oh"), R("posv")], [R("oh")])
                rsum(slf[:, k_:k_ + 1], oh, [R("oh"), R("slf")], [R("slf")])
            ts("dve", slot_i[:, tl, :], slf, -1.0, None, ALU.add, None, [R("slf")], [R("slot_i%d" % tl)])
            for k_ in range(4):
                si, sir = sinfo[k_], R("sinfo%d" % k_)
                cp("pool", si[:, 0:1], TF("tokid", tl, tl + 1), [RTF], [sir])
                cp("pool", si[:, 1:2], w4[:, k_:k_ + 1], [R("w4"), sir], [sir])
                scatter(slotinfo_d[:, :], slot_i[:, tl, k_:k_ + 1], si, [sir, R("slot_i%d" % tl)], [],
                        ws=[R("slotinfo_d")])

    if stages >= 6:
        areset()
        xT = al("xT", [128, 16, CAP], BF16)
        aT_ = al("actT", [128, 16, CAP], BF16)
        wgb = [al("wgb%d" % i, [128, 16, 512], BF16) for i in range(2)]
        xg = [al("xg%d" % i, [128, D], BF16) for i in range(2)]
        sinf = [al("sinf%d" % i, [128, 2]) for i in range(2)]
        sidx = [al("sidx%d" % i, [128, 1], I32) for i in range(2)]
        wsl = al("wsl", [128, 16])
        bgu = al("bgu", [128, NE * 32])
        dma("sp", bgu, b_gu_col[:, :], [], [R("bgu")])
        g1_ = [al("g1_%d" % i, [128, 512]) for i in range(2)]
        g2_ = [al("g2_%d" % i, [128, 512]) for i in range(2)]
        bdr = [al("bdr%d" % i, [128, 512]) for i in range(2)]
        yst = [al("yst%d" % i, [128, 512]) for i in range(2)]
        NJT = CAP // 128
        NSG = CAP // 512
        wc = 0
        ec = 0
        for e_ in range(NE):
            for jt in range(NJT):
                b2 = jt % 2
                sf, sfr = sinf[b2], R("sinf%d" % b2)
                sx, sxr = sidx[b2], R("sidx%d" % b2)
                xg_, xgr = xg[b2], R("xg%d" % b2)
                s0 = e_ * CAP + jt * 128
                dma("sp", sf, slotinfo_d[s0:s0 + 128, :], [R("slotinfo_d")], [sfr])
                cp("dve", sx, sf[:, 0:1], [sfr], [sxr])
                cp("dve", wsl[:, jt:jt + 1], sf[:, 1:2], [sfr], [], ws=[R("wsl")])
                gather(xg_, h2_d[:, :], sx[:, 0:1], [sxr, R("h2_d")], [xgr])
                for half in range(2):
                    pb = half
                    for k_ in range(8):
                        dc = half * 8 + k_
                        trp(psb[pb][:, k_ * 128:(k_ + 1) * 128], xg_[:, dc * 128:(dc + 1) * 128], identb,
                            [xgr, RTB], [PSR[pb]])
                    src3 = psb[pb].rearrange("p (k t) -> p k t", k=8)
                    if half == 0:
                        cp("act", xT[:, 0:8, jt * 128:(jt + 1) * 128], src3, [PSR[pb]], [], ws=[R("xT")])
                    else:
                        cp("dve", xT[:, 8:16, jt * 128:(jt + 1) * 128], src3, [PSR[pb]], [], ws=[R("xT")])
            for blk in range(8):
                w = wgb[wc % 2]
                wr = R("wgb%d" % (wc % 2))
                wc += 1
                dma("pool", w, w_gate_up[e_, :, blk * 512:(blk + 1) * 512].rearrange("(c p) n -> p c n", p=128), [], [wr])
                for f4 in range(4):
                    fcg = blk * 4 + f4
                    fc = fcg % 16
                    bcol_ = bgu[:, e_ * 32 + fcg:e_ * 32 + fcg + 1]
                    for sg in range(NSG):
                        pb = 2 + ec % 4
                        k2 = ec % 2
                        ec += 1
                        for dc in range(16):
                            mm(ps[pb][:], w[:, dc, f4 * 128:(f4 + 1) * 128], xT[:, dc, sg * 512:(sg + 1) * 512],
                               dc == 0, dc == 15, [wr, R("xT")], [PSR[pb]])
                        ga, gar = g1_[k2], R("g1_%d" % k2)
                        gb, gbr = g2_[k2], R("g2_%d" % k2)
                        dstA = aT_[:, fc, sg * 512:(sg + 1) * 512]
                        if fcg < 16:
                            ts("dve", ga, ps[pb][:], bcol_, 7.0, ALU.add, ALU.min, [PSR[pb], R("bgu")], [gar])
                            act(gb, ga, AF.Sigmoid, [gar], [gbr], scale=1.702)
                            tt("pool", dstA, ga, gb, ALU.mult, [gar, gbr], [], ws=[R("actT")])
                        else:
                            ts("dve", ga, ps[pb][:], bcol_, 7.0, ALU.add, ALU.min, [PSR[pb], R("bgu")], [gar])
                            ts("pool", gb, ga, -7.0, 1.0, ALU.max, ALU.add, [gar], [gbr])
                            tt("pool", dstA, dstA, gb, ALU.mult, [gbr, R("actT")], [], ws=[R("actT")])
            for db in range(4):
                w = wgb[wc % 2]
                wr = R("wgb%d" % (wc % 2))
                wc += 1
                dma("pool", w, w_down[e_, :, db * 512:(db + 1) * 512].rearrange("(c p) n -> p c n", p=128), [], [wr])
                bd, bdrr = bdr[db % 2], R("bdr%d" % (db % 2))
                dma("sp", bd, b_down[e_:e_ + 1, db * 512:(db + 1) * 512].partition_broadcast(128), [], [bdrr])
                for jt in range(NJT):
                    pb = 2 + ec % 4
                    k2 = ec % 2
                    ec += 1
                    for fc in range(16):
                        mm(ps[pb][:], aT_[:, fc, jt * 128:(jt + 1) * 128], w[:, fc, :], fc == 0, fc == 15,
                           [wr, R("actT")], [PSR[pb]])
                    ga, gar = g1_[k2], R("g1_%d" % k2)
                    ys, ysr = yst[k2], R("yst%d" % k2)
                    tt("dve", ga, ps[pb][:], bd, ALU.add, [PSR[pb], bdrr], [gar])
                    act(ys, ga, AF.Copy, [gar, R("wsl")], [ysr], scale=wsl[:, jt:jt + 1])
                    s0 = e_ * CAP + jt * 128
                    dma("sp", ybuf_d[db][s0:s0 + 128, :], ys, [ysr], [], ws=[R("ybuf_d")])
            memset("pool", wsl[:, 0:1], 0.0, [R("wsl"), R("xT"), R("actT")])

    if stages >= 7:
        areset()
        g2r = al("g2r", [128, D])
        l2g = al("l2g", [128, D])
        l2b = al("l2b", [128, D])
        dma("sp", g2r, modrow_d[3:4, :].partition_broadcast(128), [R("modrow_d")], [R("g2r")])
        dma("sp", l2g, ln2_g[0:1, :].partition_broadcast(128), [], [R("l2g")])
        dma("sp", l2b, ln2_b[0:1, :].partition_broadcast(128), [], [R("l2b")])
        yk = [al("yk%d" % k, [128, D]) for k in range(8)]
        x1t = [al("x1t%d" % i, [128, D]) for i in range(2)]
        v2 = al("v2", [128, D])
        sqv = al("sqv2", [128, D])
        o2 = [al("o2%d" % i, [128, D]) for i in range(2)]
        st = al("st2", [128, 16])

        for tl in range(NQB):
            r0 = tl * 128
            b2 = tl % 2
            for k_ in range(4):
                y_, yr = yk[b2 * 4 + k_], R("yk%d" % (b2 * 4 + k_))
                for q_ in range(4):
                    gather(y_[:, q_ * 512:(q_ + 1) * 512], ybuf_d[q_][:, :], slot_i[:, tl, k_:k_ + 1],
                           [R("slot_i%d" % tl), R("ybuf_d")], [], ws=[yr])
            x_, rx = x1t[b2], R("x1t%d" % b2)
            dma("sp", x_, x1_d[r0:r0 + 128, :], [R("x1_d")], [rx])
            ya_, yb_, yc_, yd_ = [yk[b2 * 4 + k_] for k_ in range(4)]
            ra, rb, rc, rd = [R("yk%d" % (b2 * 4 + k_)) for k_ in range(4)]
            tt("dve", ya_, ya_, yb_, ALU.add, [ra, rb], [ra])
            tt("pool", yc_, yc_, yd_, ALU.add, [rc, rd], [rc])
            tt("dve", ya_, ya_, yc_, ALU.add, [ra, rc], [ra])
            tt("pool", ya_, ya_, g2r, ALU.mult, [ra, R("g2r")], [ra])
            stt("dve", v2, x_, DN_ALPHA, ya_, ALU.mult, ALU.add, [rx, ra], [R("v2")])
            o_, orr = o2[b2], R("o2%d" % b2)
            rsum(st[:, 0:1], v2, [R("v2")], [R("st2")])
            tt("pool", sqv, v2, v2, ALU.mult, [R("v2")], [R("sqv2")])
            rsum(st[:, 1:2], sqv, [R("sqv2"), R("st2")], [R("st2")])
            ts("dve", st[:, 2:3], st[:, 0:1], 1.0 / D, None, ALU.mult, None, [R("st2")], [R("st2")])
            tt("dve", st[:, 3:4], st[:, 2:3], st[:, 2:3], ALU.mult, [R("st2")], [R("st2")])
            stt("dve", st[:, 4:5], st[:, 1:2], 1.0 / D, st[:, 3:4], ALU.mult, ALU.subtract, [R("st2")], [R("st2")])
            ts("dve", st[:, 4:5], st[:, 4:5], LN_EPS, None, ALU.add, None, [R("st2")], [R("st2")])
            act(st[:, 5:6], st[:, 4:5], AF.Sqrt, [R("st2")], [R("st2")])
            recip(st[:, 6:7], st[:, 5:6], [R("st2")], [R("st2")])
            ts("dve", o_, v2, st[:, 2:3], st[:, 6:7], ALU.subtract, ALU.mult, [R("v2"), R("st2")], [orr])
            tt("pool", o_, o_, l2g, ALU.mult, [orr, R("l2g")], [orr])
            tt("dve", o_, o_, l2b, ALU.add, [orr, R("l2b")], [orr])
            dma("sp", out_d[r0:r0 + 128, :], o_, [orr], [], ws=[R("out")], final=True)
    else:
        fin = al("fin", [128, 512])
        memset("pool", fin, 0.0, [R("fin")])
        dma("sp", out_d[0:128, 0:512], fin, [R("fin")], [R("out")], final=True)

    print("ops:", {k: len(v) for k, v in P.ops.items()})
    P.emit()
    return nc


def core_inputs(inp, core):
    b, h = core // 2, core % 2
    TBL = tables(h)
    x = inp["x"]
    if h == 1:
        x_ctx = x[b]
    else:
        x_ctx = np.concatenate([x[b, TO:], x[b, :TO]], axis=0)
    f = lambda a: np.ascontiguousarray(a, dtype=np.float32)

    def col(v, n):
        return f(np.asarray(v).reshape(n, 128).T)

    m = {
        "x_ctx": f(x_ctx),
        "c_col": col(inp["c"][b], 16),
        "w_ada": f(inp["w_ada"][0]),
        "b_ada_col": col(inp["b_ada"][0], 96),
        "b_ada_row": f(inp["b_ada"][0].reshape(1, -1)),
        "w_in": f(inp["w_in"][0]),
        "gla_w_gate": f(inp["gla_w_gate"][0]),
        "gla_b_gate": f(inp["gla_b_gate"][0].reshape(1, -1)),
        "gla_nw_col": col(inp["gla_norm_w"][0], 2),
        "w_out": f(inp["w_out"][0]),
        "ln1_g": f(inp["ln1_g"][0].reshape(1, -1)),
        "ln1_b": f(inp["ln1_b"][0].reshape(1, -1)),
        "w_router": f(inp["w_router"][0]),
        "b_router": f(inp["b_router"][0].reshape(1, -1)),
        "w_gate_up": f(inp["w_gate_up"][0]),
        "b_gu_col": f(inp["b_gate_up"][0].reshape(NE, 32, 128).transpose(2, 0, 1).reshape(128, NE * 32)),
        "w_down": f(inp["w_down"][0]),
        "b_down": f(inp["b_down"][0]),
        "ln2_g": f(inp["ln2_g"][0].reshape(1, -1)),
        "ln2_b": f(inp["ln2_b"][0].reshape(1, -1)),
        "tbs": TBL["tbs"], "tba": TBL["tba"], "tf": TBL["tf"], "tb5": TBL["tb5"],
    }
    for kv in ("k", "v"):
        m["cmp_pe_" + kv] = f(inp["cmp_pe_" + kv][0])
        m["cmp_w1_" + kv] = f(inp["cmp_w1_" + kv][0])
        m["cmp_b1c_" + kv] = col(inp["cmp_b1_" + kv][0], 2)
        m["cmp_w2_" + kv] = f(inp["cmp_w2_" + kv][0])
    return m


_NC_CACHE = {}


def kernel(**inputs):
    if "nc" not in _NC_CACHE:
        _NC_CACHE["nc"] = build()
    nc = _NC_CACHE["nc"]
    in_maps = [core_inputs(inputs, c) for c in range(8)]
    res = run_bass_kernel_spmd(nc, in_maps, core_ids=list(range(8)))
    out = np.empty((4, T, D), np.float32)
    for c in range(8):
        b, h = c // 2, c % 2
        out[b, h * TO:(h + 1) * TO] = res.results[c]["out"]
    return out
```

```python
import numpy as np
import ml_dtypes
import concourse.bass as bass
import concourse.mybir as mybir
from concourse.bass_utils import run_bass_kernel_spmd

F32 = mybir.dt.float32
BF16 = mybir.dt.bfloat16
I32 = mybir.dt.int32
U32 = mybir.dt.uint32
AF = mybir.ActivationFunctionType
ALU = mybir.AluOpType
AX = mybir.AxisListType

D = 2048
T = 8192
TO = 4096
NQB = 32
IN_W = 5672
NEG = -30000.0
CAP = 2048
NE = 32
LN_EPS = 1e-5
RMS_EPS = 1e-6
DN_ALPHA = 2 ** 0.25


class Res:
    __slots__ = ("name", "w", "sw", "r")

    def __init__(self, name=""):
        self.name = name
        self.w = {}
        self.sw = {}
        self.r = {}


class Prog:
    ENGS = ("pe", "act", "dve", "pool", "sp")

    def __init__(self, nc, n_dsem=40):
        self.nc = nc
        self.ops = {e: [] for e in self.ENGS}
        self.cnt = {e: 0 for e in self.ENGS}
        self.known = {e: {} for e in self.ENGS}
        self.n_dsem = n_dsem
        self.dsem_use = [0] * n_dsem
        self.dsem_next = 0
        self.final_toks = []
        self.pending = {e: [] for e in self.ENGS}

    def barrier(self):
        toks = [("E", e, self.cnt[e]) for e in self.ENGS if self.cnt[e] > 0]
        toks += [("D", s, u * 16) for s, u in enumerate(self.dsem_use) if u > 0]
        for e in self.ENGS:
            self.pending[e] = list(toks)

    def _deps(self, eng, reads, writes, ws, extra=()):
        toks = list(extra) + self.pending[eng]
        self.pending[eng] = []
        for r in reads:
            for dd in (r.w, r.sw):
                for k, v in dd.items():
                    toks.append((k[0], k[1], v))
        for w in writes:
            for dd in (w.w, w.sw, w.r):
                for k, v in dd.items():
                    toks.append((k[0], k[1], v))
        for w in ws:
            for dd in (w.w, w.r):
                for k, v in dd.items():
                    toks.append((k[0], k[1], v))
        waits = {}
        kn = self.known[eng]
        for kind, key, val in toks:
            if kind == "E" and key == eng and eng == "pe":
                continue
            if kn.get((kind, key), 0) >= val:
                continue
            if waits.get((kind, key), 0) < val:
                waits[(kind, key)] = val
        for k, v in waits.items():
            kn[k] = v
        return list(waits.items())

    def _mark(self, tok, reads, writes, ws):
        k = (tok[0], tok[1])
        for r in reads:
            if r.r.get(k, 0) < tok[2]:
                r.r[k] = tok[2]
        for w in writes:
            w.w = {k: tok[2]}
            w.sw = {}
            w.r = {}
        for w in ws:
            if w.sw.get(k, 0) < tok[2]:
                w.sw[k] = tok[2]

    def op(self, eng, fn, reads=(), writes=(), ws=()):
        waits = self._deps(eng, reads, writes, ws)
        self.cnt[eng] += 1
        tok = ("E", eng, self.cnt[eng])
        self.ops[eng].append((waits, fn, ("E", eng, 1)))
        self._mark(tok, reads, writes, ws)
        return tok

    def dma(self, eng, fn, reads=(), writes=(), ws=(), final=False):
        s = self.dsem_next
        self.dsem_next = (s + 1) % self.n_dsem
        prev = self.dsem_use[s]
        extra = [("D", s, prev * 16)] if prev > 0 else []
        waits = self._deps(eng, reads, writes, ws, extra)
        self.dsem_use[s] += 1
        tok = ("D", s, self.dsem_use[s] * 16)
        self.ops[eng].append((waits, fn, ("D", s, 16)))
        self._mark(tok, reads, writes, ws)
        if final:
            self.final_toks.append(tok)
        return tok

    def emit(self):
        nc = self.nc
        esem = {e: nc.alloc_semaphore(name="es_" + e) for e in self.ENGS}
        dsem = [nc.alloc_semaphore(name="ds_%d" % i) for i in range(self.n_dsem)]
        fw = self._deps("sp", (), (), (), self.final_toks)

        def sem_of(kind, key):
            return esem[key] if kind == "E" else dsem[key]

        def run(eng_name, e):
            for waits, fn, inc in self.ops[eng_name]:
                for (kind, key), val in waits:
                    e.wait_ge(sem_of(kind, key), val)
                ins = fn(e)
                ins.then_inc(sem_of(inc[0], inc[1]), inc[2])
            if eng_name == "sp":
                for (kind, key), val in fw:
                    e.wait_ge(sem_of(kind, key), val)

        with nc.Block() as block:
            @block.tensor
            def _(e):
                run("pe", e)

            @block.scalar
            def _(e):
                run("act", e)

            @block.vector
            def _(e):
                run("dve", e)

            @block.gpsimd
            def _(e):
                run("pool", e)

            @block.sync
            def _(e):
                run("sp", e)


def _cmp_partial_tiles():
    idx = {}
    for i in range(NQB):
        for j in range(4):
            o = 4065 + 128 * i - 2048 * j
            if o + 127 < 0:
                continue
            if o < 2032:
                idx[(i, j)] = len(idx)
    return idx


CMP_PART = _cmp_partial_tiles()


def _bf(a):
    return np.ascontiguousarray(a.astype(ml_dtypes.bfloat16))


def _pack(tb, dtype):
    off = {}
    cols = []
    o = 0
    for k, v in tb.items():
        off[k] = (o, v.shape[1])
        o += v.shape[1]
        cols.append(v)
    arr = np.concatenate(cols, axis=1)
    if dtype == "bf16":
        return _bf(arr), off
    return np.ascontiguousarray(arr.astype(np.float32)), off


def make_tables(h):
    p = np.arange(128)
    tb = {}
    tb["identb"] = np.eye(128)
    tb["onesb"] = np.ones((128, 128))
    tb["tri"] = np.where(p[:, None] <= p[None, :], 0.0, NEG)
    tb["band"] = np.where(p[:, None] > p[None, :], 0.0, NEG)
    tb["cmask"] = (p[:, None] <= p[None, :]).astype(np.float64)
    tb["ltri"] = (p[:, None] < p[None, :]).astype(np.float64)
    tbs, tbs_off = _pack(tb, "bf16")

    ta = {}
    E = np.zeros((128, 64, 128))
    for kt in range(64):
        for key in range(128):
            E[2 * kt + key // 64, kt, key] = 1.0
    ta["E"] = E.reshape(128, 64 * 128)
    ov = np.zeros((128, 4, 128))
    for j in range(4):
        cs = 16 * (128 * j + p)
        for blk in range(128):
            ov[:, j, blk] = ((cs < 64 * blk + 64) & (cs + 32 > 64 * blk)).astype(np.float64)
    ta["ovl"] = ov.reshape(128, 512)
    cm = np.zeros((128, len(CMP_PART), 128))
    for (i, j), t in CMP_PART.items():
        c = 128 * j + p
        q = 4096 + 128 * i + p
        cm[:, t, :] = np.where((16 * c + 31)[:, None] <= q[None, :], 0.0, NEG)
    ta["cmpm"] = cm.reshape(128, -1)
    tba, tba_off = _pack(ta, "bf16")

    tf = {}
    tf["identf"] = np.eye(128)
    tf["triI"] = np.where(p[:, None] <= p[None, :], -1.0 / 16, 0.0)
    tf["triR"] = np.where(p[:, None] > p[None, :], -1.0 / 16, 0.0)
    tf["onesf"] = np.ones((128, 128))
    r = np.arange(256) - 128
    curq = (p >= 64).astype(np.int64)
    future = r[None, :] > curq[:, None]
    forced = (r[None, :] <= curq[:, None]) & (r[None, :] > curq[:, None] - 2)
    tf["tblA"] = np.where(future | forced, 0.0, 1.0)
    tf["tblB"] = np.where(forced, 1e6, np.where(future, -1.0, 0.0))
    blk0 = 64 * (1 - h)
    f0 = np.full((128, 128), -1e9)
    f0[:, blk0] = 1e6
    tf["F0"] = f0
    tf["iotaE"] = np.tile(np.arange(32)[None, :], (128, 1)).astype(np.float64)
    tf["iotaEC1"] = tf["iotaE"] * CAP + 1.0
    tf["tokid"] = (np.arange(32)[None, :] * 128 + p[:, None]).astype(np.float64)
    tf["flag"] = np.full((128, 1), float(h))
    tf128, tf_off = _pack(tf, "f32")

    pos = np.arange(T)
    invalid = (pos < 4096).astype(np.float64) * (1 - h)
    posrows = np.stack([np.ones(T), pos // 64, pos % 64, invalid, np.ones(T)])
    c = np.arange(512)
    m2 = 32 * c + 31
    inv_c = ((c < 256).astype(np.float64) * (1 - h))
    inv_c[511] = 1.0
    posrows_c = np.stack([np.ones(512), m2 // 64, m2 % 64, inv_c, np.ones(512)])
    slopes = 2.0 ** (-(np.arange(8) + 1.0))
    src = np.zeros((5, NQB, 8))
    srcc = np.zeros((5, NQB, 8))
    for i in range(NQB):
        a_i = 64 + 2 * i
        src[1, i] = 64 * slopes
        src[2, i] = slopes
        src[3, i] = NEG
        src[4, i] = -64 * slopes * a_i
        srcc[1, i] = 32 * slopes
        srcc[2, i] = slopes / 2
        srcc[3, i] = NEG
        srcc[4, i] = -64 * slopes * a_i
    qrow = np.zeros((5, 8, 128))
    qrow[0] = -slopes[:, None] * np.arange(128)[None, :]
    t5 = {"posrows": posrows, "posrows_c": posrows_c, "src": src.reshape(5, -1),
          "srcc": srcc.reshape(5, -1), "qrow": qrow.reshape(5, -1)}
    for k, v in t5.items():
        ok = (v == NEG) | (v.astype(ml_dtypes.bfloat16).astype(np.float64) == v)
        assert ok.all(), k
    tb5, t5_off = _pack(t5, "bf16")
    return dict(tbs=tbs, tbs_off=tbs_off, tba=tba, tba_off=tba_off, tf=tf128, tf_off=tf_off,
                tb5=tb5, t5_off=t5_off)


_TBL_CACHE = {}


def tables(h):
    if h not in _TBL_CACHE:
        _TBL_CACHE[h] = make_tables(h)
    return _TBL_CACHE[h]


def build(debug=False, stages=99):
    nc = bass.Bass("TRN2", target_bir_lowering=False)
    P = Prog(nc)
    TBL = tables(0)
    tbo, tao, tfo, t5o = TBL["tbs_off"], TBL["tba_off"], TBL["tf_off"], TBL["t5_off"]

    def din(name, shape, dt=F32):
        return nc.dram_tensor(name, list(shape), dt, kind="ExternalInput").ap()

    skind = "ExternalOutput" if debug else "Internal"

    def dscr(name, shape, dt):
        return nc.dram_tensor(name, list(shape), dt, kind=skind).ap()

    def sb(name, shape, dt=F32):
        return nc.alloc_sbuf_tensor(name, list(shape), dt)

    ARENA = 196 * 1024
    arena = sb("arena", [128, ARENA // 2], BF16)
    ast = {"off": 0}
    DSZ = {F32: 4, BF16: 2, I32: 4, U32: 4}

    def areset():
        P.barrier()
        ast["off"] = 0

    def al(name, shape, dt=F32):
        shape = list(shape)
        nfree = 1
        for d_ in shape[1:]:
            nfree *= d_
        nbytes = (nfree * DSZ[dt] + 31) // 32 * 32
        o = ast["off"]
        assert o + nbytes <= ARENA, (name, o, nbytes)
        ast["off"] = o + nbytes
        v = arena[0:shape[0], o // 2:(o + nbytes) // 2]
        if dt != BF16:
            v = v.bitcast(dt)
        v = v[:, 0:nfree]
        if len(shape) == 3:
            v = v.rearrange("p (a b) -> p a b", a=shape[1])
        elif len(shape) == 4:
            v = v.rearrange("p (a b c) -> p a b c", a=shape[1], b=shape[2])
        return v

    RS = {}

    def R(name):
        if name not in RS:
            RS[name] = Res(name)
        return RS[name]

    def mm(out, lhsT, rhs, start, stop, reads, writes, ws=()):
        P.op("pe", lambda e: e.matmul(out, lhsT, rhs, start=start, stop=stop), reads, writes, ws)

    def trp(out, in_, ident, reads, writes, ws=()):
        P.op("pe", lambda e: e.transpose(out, in_, ident), reads, writes, ws)

    def act(out, in_, func, reads, writes, bias=None, scale=None, ws=()):
        kw = {}
        if bias is not None:
            kw["bias"] = bias
        if scale is not None:
            kw["scale"] = scale
        P.op("act", lambda e: e.activation(out=out, in_=in_, func=func, **kw), reads, writes, ws)

    def tt(eng, out, in0, in1, op, reads, writes, ws=()):
        P.op(eng, lambda e: e.tensor_tensor(out=out, in0=in0, in1=in1, op=op), reads, writes, ws)

    def ts(eng, out, in0, s1, s2, op0, op1, reads, writes, ws=()):
        if op1 is None:
            P.op(eng, lambda e: e.tensor_scalar(out=out, in0=in0, scalar1=s1, scalar2=None, op0=op0),
                 reads, writes, ws)
        else:
            P.op(eng, lambda e: e.tensor_scalar(out=out, in0=in0, scalar1=s1, scalar2=s2, op0=op0, op1=op1),
                 reads, writes, ws)

    def stt(eng, out, in0, scalar, in1, op0, op1, reads, writes, ws=()):
        P.op(eng, lambda e: e.scalar_tensor_tensor(out=out, in0=in0, scalar=scalar, in1=in1, op0=op0, op1=op1),
             reads, writes, ws)

    def cp(eng, out, in_, reads, writes, ws=()):
        if eng == "act":
            P.op("act", lambda e: e.activation(out=out, in_=in_, func=AF.Copy), reads, writes, ws)
        else:
            P.op(eng, lambda e: e.tensor_copy(out, in_), reads, writes, ws)

    def recip(out, in_, reads, writes, ws=()):
        P.op("dve", lambda e: e.reciprocal(out, in_), reads, writes, ws)

    def rsum(out, in_, reads, writes):
        P.op("dve", lambda e: e.reduce_sum(out, in_, AX.X), reads, writes)

    def max8(out, in_, reads, writes):
        P.op("dve", lambda e: e.max(out=out, in_=in_), reads, writes)

    def mrepl(out, m8, in_, val, reads, writes):
        P.op("dve", lambda e: e.match_replace(out=out, in_to_replace=m8, in_values=in_, imm_value=val), reads, writes)

    def memset(eng, ap, val, writes, ws=()):
        P.op(eng, lambda e: e.memset(ap, val), (), writes, ws)

    def dma(eng, out, in_, reads, writes, ws=(), final=False):
        P.dma(eng, lambda e: e.dma_start(out=out, in_=in_), reads, writes, ws, final=final)

    def gather(out, src, idx_ap, reads, writes, ws=()):
        P.dma("pool", lambda e: e.indirect_dma_start(
            out=out, out_offset=None, in_=src,
            in_offset=bass.IndirectOffsetOnAxis(ap=idx_ap, axis=0)), reads, writes, ws)

    def scatter(dst, idx_ap, in_, reads, writes, ws=()):
        P.dma("pool", lambda e: e.indirect_dma_start(
            out=dst, out_offset=bass.IndirectOffsetOnAxis(ap=idx_ap, axis=0),
            in_=in_, in_offset=None), reads, writes, ws)

    x_ctx = din("x_ctx", [T, D])
    c_col = din("c_col", [128, 16])
    w_ada = din("w_ada", [D, 6 * D])
    b_ada_col = din("b_ada_col", [128, 96])
    b_ada_row = din("b_ada_row", [1, 6 * D])
    w_in = din("w_in", [D, IN_W])
    cmp_in = {}
    for kv in ("k", "v"):
        cmp_in[kv] = dict(pe=din("cmp_pe_" + kv, [32, 128]), w1=din("cmp_w1_" + kv, [4096, 256]),
                          b1=din("cmp_b1c_" + kv, [128, 2]), w2=din("cmp_w2_" + kv, [256, 128]))
    gla_w_gate = din("gla_w_gate", [16, 512])
    gla_b_gate = din("gla_b_gate", [1, 512])
    gla_nw_col = din("gla_nw_col", [128, 2])
    w_out = din("w_out", [D, D])
    ln1_g = din("ln1_g", [1, D])
    ln1_b = din("ln1_b", [1, D])
    w_router = din("w_router", [D, NE])
    b_router = din("b_router", [1, NE])
    w_gate_up = din("w_gate_up", [NE, D, 2 * D])
    b_gu_col = din("b_gu_col", [128, NE * 32])
    w_down = din("w_down", [NE, D, D])
    b_down = din("b_down", [NE, D])
    ln2_g = din("ln2_g", [1, D])
    ln2_b = din("ln2_b", [1, D])
    tbs_d = din("tbs", TBL["tbs"].shape, BF16)
    tba_d = din("tba", TBL["tba"].shape, BF16)
    tf_d = din("tf", TBL["tf"].shape, F32)
    tb5_d = din("tb5", TBL["tb5"].shape, BF16)
    out_d = nc.dram_tensor("out", [TO, D], F32, kind="ExternalOutput").ap()

    modrow_d = dscr("modrow_d", [4, D], F32)
    qT_d = dscr("qT_d", [1024, TO], BF16)
    kcmpT_d = dscr("kcmpT_d", [256, T], BF16)
    vcmpT_d = dscr("vcmpT_d", [256, T], BF16)
    kselT_d = dscr("kselT_d", [256, T], BF16)
    vsel_d = dscr("vsel_d", [T, 256], BF16)
    kwinT_d = dscr("kwinT_d", [256, T], BF16)
    vwin_d = dscr("vwin_d", [T, 256], BF16)
    gT_d = dscr("gT_d", [24, TO], F32)
    qglaT_d = dscr("qglaT_d", [512, TO], BF16)
    kglaT_d = dscr("kglaT_d", [512, T], BF16)
    kgla_d = dscr("kgla_d", [T, 512], BF16)
    vgla_d = dscr("vgla_d", [T, 1024], BF16)
    aT_d = dscr("aT_d", [16, T], F32)
    rT_d = dscr("rT_d", [1024, TO], BF16)
    yT_d = dscr("yT_d", [D, TO], BF16)
    x1_d = dscr("x1_d", [TO, D], F32)
    h2_d = dscr("h2_d", [TO, D], BF16)
    slotinfo_d = dscr("slotinfo_d", [NE * CAP, 2], F32)
    ybuf_d = [nc.dram_tensor("ybuf_d%d" % q_, [NE * CAP, 512], F32, kind="Internal").ap() for q_ in range(4)]

    tbs = sb("tbs_s", TBL["tbs"].shape, BF16)
    tf128 = sb("tf_s", TBL["tf"].shape, F32)
    sc1p = sb("sc1p", [128, 32])
    sc1v = sb("sc1v", [128, 32])
    kcT = sb("kcT", [128, 2, 512], BF16)
    vcs = sb("vcs", [128, 4, 2, 128], BF16)
    slot_i = sb("slot_i", [128, NQB, 4], I32)

    def TB(name, a=None, b=None):
        o, n = tbo[name]
        return tbs[:, o:o + n] if a is None else tbs[:, o + a:o + b]

    def TF(name, a=None, b=None):
        o, n = tfo[name]
        return tf128[:, o:o + n] if a is None else tf128[:, o + a:o + b]

    ps = [nc.alloc_psum_tensor("ps%d" % i, [128, 512], F32) for i in range(8)]
    PSR = [R("ps%d" % i) for i in range(8)]
    psb = [p_[:].bitcast(BF16) for p_ in ps]

    def ps3(b_):
        return ps[b_][:].rearrange("p (h q) -> p h q", h=4)

    dma("sp", tbs[:], tbs_d[:, :], [], [R("tbs")])
    dma("sp", tf128[:], tf_d[:, :], [], [R("tf")])
    identf = TF("identf")
    identb = TB("identb")
    onesb = TB("onesb")
    flagc = TF("flag")
    RTB, RTF = R("tbs"), R("tf")

    def bc4(ap2d):
        return ap2d.unsqueeze(1).to_broadcast([128, 4, 128])

    cc = al("cc", [128, 16])
    scc = al("scc", [128, 16])
    screp = al("screp", [128, 16, 128])
    bcol = al("bcol", [128, 96])
    modcol = al("modcol", [128, 32])
    modrep = al("modrep", [128, 4, D])
    wa = [al("wa%d" % i, [128, 16, 512]) for i in range(2)]
    brow_ = [al("brow%d" % i, [128, 512]) for i in range(2)]
    dma("sp", cc, c_col[:, :], [], [R("cc")])
    dma("sp", bcol, b_ada_col[:, :], [], [R("bcol")])
    act(scc, cc, AF.Silu, [R("cc")], [R("scc")])
    cp("dve", screp, scc.unsqueeze(2).to_broadcast([128, 16, 128]), [R("scc")], [R("screp")])
    for blk in range(24):
        w = wa[blk % 2]
        wr = R("wa%d" % (blk % 2))
        dma("sp", w, w_ada[:, blk * 512:(blk + 1) * 512].rearrange("(dc p) n -> p dc n", p=128), [], [wr])
        if blk < 8:
            for fc in range(4):
                col = blk * 4 + fc
                for dc in range(16):
                    mm(ps[0][:, col:col + 1], w[:, dc, fc * 128:(fc + 1) * 128], scc[:, dc:dc + 1],
                       dc == 0, dc == 15, [wr, R("scc")], [PSR[0]])
        else:
            br = brow_[blk % 2]
            brr = R("brow%d" % (blk % 2))
            dma("sp", br, b_ada_row[0:1, blk * 512:(blk + 1) * 512].partition_broadcast(128), [], [brr])
            pb = 1 + blk % 2
            for dc in range(16):
                mm(ps[pb][:], screp[:, dc, :], w[:, dc, :], dc == 0, dc == 15, [wr, R("screp")], [PSR[pb]])
            seg = (blk - 8) // 4
            off = ((blk - 8) % 4) * 512
            tt("dve", modrep[:, seg, off:off + 512], ps[pb][:], br, ALU.add, [PSR[pb], brr], [], ws=[R("modrep")])
    tt("dve", modcol, ps[0][:, 0:32], bcol[:, 0:32], ALU.add, [PSR[0], R("bcol")], [R("modcol")])
    ts("dve", sc1p[:, 0:16], modcol[:, 16:32], 1.0, None, ALU.add, None, [R("modcol")], [R("sc1p")])
    cp("dve", sc1p[:, 16:32], modcol[:, 0:16], [R("modcol"), R("sc1p")], [R("sc1p")])
    ts("dve", sc1v[:], sc1p[:], flagc[:, 0:1], None, ALU.mult, None, [R("sc1p"), RTF], [R("sc1v")])
    for seg in range(4):
        dma("sp", modrow_d[seg:seg + 1, :], modrep[0:1, seg, :], [R("modrep")], [], ws=[R("modrow_d")])

    if stages >= 1:
        areset()
        xs = al("xs", [128, 4, D])
        hT = al("hT", [128, 16, 1024], BF16)
        wb = [al("wb%d" % i, [128, 16, 512], BF16) for i in range(2)]
        stg = [al("stg%d" % i, [128, 512], BF16) for i in range(4)]
        stgf = [al("stgf%d" % i, [128, 512], F32) for i in range(2)]
        SQ = 128 ** -0.5
        blocks = [
            (0, 512, [("F", 0, 512, qT_d, 0, "q")], True),
            (512, 512, [("F", 0, 512, qT_d, 512, "q")], True),
            (1024, 512, [("F", 0, 256, kcmpT_d, 0, "c"), ("F", 256, 256, vcmpT_d, 0, "c")], False),
            (1536, 512, [("F", 0, 256, kselT_d, 0, "c"), ("T", 256, 256, vsel_d, 0, "c")], False),
            (2048, 512, [("F", 0, 256, kwinT_d, 0, "c"), ("T", 256, 256, vwin_d, 0, "c")], False),
            (2560, 24, [("F", 0, 24, gT_d, 0, "sig")], True),
            (2584, 512, [("F", 0, 512, qglaT_d, 0, "q")], True),
            (3096, 512, [("F", 0, 512, kglaT_d, 0, "c"), ("T", 0, 512, kgla_d, 0, "c")], False),
            (3608, 512, [("T", 0, 512, vgla_d, 0, "c")], False),
            (4120, 512, [("T", 0, 512, vgla_d, 512, "c")], False),
            (4632, 16, [("F", 0, 16, aT_d, 0, "f32")], False),
            (4648, 512, [("F", 0, 512, rT_d, 0, "silu")], True),
            (5160, 512, [("F", 0, 512, rT_d, 512, "silu")], True),
        ]
        wcnt = 0
        pcnt = 0
        scnt = 0
        for g in range(8):
            own = g >= 4
            scl = sc1p if own else sc1v
            sclr = R("sc1p") if own else R("sc1v")
            for hh in range(2):
                for tt_ in range(4):
                    r0 = g * 1024 + hh * 512 + tt_ * 128
                    dma("sp", xs[:, tt_, :], x_ctx[r0:r0 + 128, :], [], [R("xs%d" % tt_)])
                for dc in range(16):
                    pb = pcnt % 4
                    pcnt += 1
                    for tt_ in range(4):
                        trp(ps[pb][:, tt_ * 128:(tt_ + 1) * 128], xs[:, tt_, dc * 128:(dc + 1) * 128], identf,
                            [R("xs%d" % tt_), RTF], [PSR[pb]])
                    act(hT[:, dc, hh * 512:(hh + 1) * 512], ps[pb][:], AF.Identity, [PSR[pb], sclr], [],
                        bias=scl[:, 16 + dc:17 + dc], scale=scl[:, dc:dc + 1], ws=[R("hT")])
            for (c0, ncol, subs, own_only) in blocks:
                if own_only and not own:
                    continue
                w = wb[wcnt % 2]
                wr = R("wb%d" % (wcnt % 2))
                wcnt += 1
                dma("pool", w[:, :, 0:ncol], w_in[:, c0:c0 + ncol].rearrange("(dc p) n -> p dc n", p=128), [], [wr])
                for (mode, so, sn, dst, doff, ev) in subs:
                    if mode == "F":
                        for fc in range((sn + 127) // 128):
                            nf = min(128, sn - fc * 128)
                            for hh in range(2):
                                pb = 4 + pcnt % 4
                                pcnt += 1
                                for dc in range(16):
                                    mm(ps[pb][0:nf, :], w[:, dc, so + fc * 128:so + fc * 128 + nf],
                                       hT[:, dc, hh * 512:(hh + 1) * 512], dc == 0, dc == 15, [wr, R("hT")], [PSR[pb]])
                                tcol = (g * 1024 + hh * 512) - (TO if own_only else 0)
                                frow = doff + fc * 128
                                if ev in ("sig", "f32"):
                                    s_ = stgf[scnt % 2]
                                    sr = R("stgf%d" % (scnt % 2))
                                else:
                                    s_ = stg[scnt % 4]
                                    sr = R("stg%d" % (scnt % 4))
                                scnt += 1
                                if ev == "q":
                                    act(s_[0:nf, :], ps[pb][0:nf, :], AF.Identity, [PSR[pb]], [sr], scale=SQ)
                                elif ev == "sig":
                                    act(s_[0:nf, :], ps[pb][0:nf, :], AF.Sigmoid, [PSR[pb]], [sr])
                                elif ev == "silu":
                                    act(s_[0:nf, :], ps[pb][0:nf, :], AF.Silu, [PSR[pb]], [sr])
                                elif scnt % 2 == 0:
                                    act(s_[0:nf, :], ps[pb][0:nf, :], AF.Copy, [PSR[pb]], [sr])
                                else:
                                    cp("dve", s_[0:nf, :], ps[pb][0:nf, :], [PSR[pb]], [sr])
                                dma("sp", dst[frow:frow + nf, tcol:tcol + 512], s_[0:nf, :], [sr], [],
                                    ws=[R(dst.name)])
                    else:
                        for tt_ in range(8):
                            pb = 4 + pcnt % 4
                            pcnt += 1
                            for dc in range(16):
                                mm(ps[pb][:, 0:sn], hT[:, dc, tt_ * 128:(tt_ + 1) * 128], w[:, dc, so:so + sn],
                                   dc == 0, dc == 15, [wr, R("hT")], [PSR[pb]])
                            s_ = stg[scnt % 4]
                            sr = R("stg%d" % (scnt % 4))
                            scnt += 1
                            if scnt % 2 == 0:
                                act(s_[:, 0:sn], ps[pb][:, 0:sn], AF.Copy, [PSR[pb]], [sr])
                            else:
                                cp("dve", s_[:, 0:sn], ps[pb][:, 0:sn], [PSR[pb]], [sr])
                            t0 = g * 1024 + tt_ * 128
                            dma("sp", dst[t0:t0 + 128, doff:doff + sn], s_[:, 0:sn], [sr], [], ws=[R(dst.name)])

    if stages >= 2:
        areset()
        kTc = al("kTc", [128, 2, T], BF16)
        w1 = al("w1", [128, 32, 256], BF16)
        pes = al("pes", [32, 128])
        peT = al("peT", [128, 32], BF16)
        b1c = al("b1c", [128, 2])
        w2 = al("w2", [128, 2, 128], BF16)
        biasc = al("biasc", [128, 2])
        u = [al("u%d" % i, [128, 512]) for i in range(2)]
        u2 = [al("u2%d" % i, [128, 512]) for i in range(2)]
        gel = al("gel", [128, 2, 512], BF16)
        memset("pool", gel[:, :, 511:512], 0.0, [R("gel")])
        memset("pool", kcT[:, :, 511:512], 0.0, [R("kcT")])
        for kv in ("k", "v"):
            ci = cmp_in[kv]
            src_d = kcmpT_d if kv == "k" else vcmpT_d
            dma("sp", kTc, src_d.rearrange("(g p) t -> p g t", p=128), [R(src_d.name)], [R("kTc")])
            dma("pool", w1, ci["w1"].rearrange("(l p) n -> p l n", p=128), [], [R("w1")])
            dma("pool", w2, ci["w2"].rearrange("(c p) n -> p c n", p=128), [], [R("w2")])
            dma("sp", pes, ci["pe"][:, :], [], [R("pes")])
            dma("sp", b1c, ci["b1"][:, :], [], [R("b1c")])
            trp(ps[0][:, 0:32], pes[0:32, :], identf[0:32, 0:32], [R("pes"), RTF], [PSR[0]])
            cp("dve", peT, ps[0][:, 0:32], [PSR[0]], [R("peT")])
            for hc in range(2):
                for l in range(32):
                    mm(ps[1][:, hc:hc + 1], w1[:, l, hc * 128:(hc + 1) * 128], peT[:, l:l + 1],
                       l == 0, l == 31, [R("w1"), R("peT")], [PSR[1]])
            tt("dve", biasc, ps[1][:, 0:2], b1c, ALU.add, [PSR[1], R("b1c")], [R("biasc")])
            for g in range(2):
                for hc in range(2):
                    pb = 2 + hc
                    for l in range(32):
                        mm(ps[pb][:, 0:511], w1[:, l, hc * 128:(hc + 1) * 128], kTc[:, g, l:l + 16 * 510 + 1:16],
                           l == 0, l == 31, [R("w1"), R("kTc")], [PSR[pb]])
                    uu, uu2 = u[hc], u2[hc]
                    ru, ru2 = R("u%d" % hc), R("u2%d" % hc)
                    act(uu[:, 0:511], ps[pb][:, 0:511], AF.Identity, [PSR[pb], R("biasc")], [ru],
                        bias=biasc[:, hc:hc + 1])
                    tt("dve", uu2[:, 0:511], uu[:, 0:511], uu[:, 0:511], ALU.mult, [ru], [ru2])
                    tt("dve", uu2[:, 0:511], uu2[:, 0:511], uu[:, 0:511], ALU.mult, [ru, ru2], [ru2])
                    stt("dve", uu2[:, 0:511], uu2[:, 0:511], 0.044715, uu[:, 0:511], ALU.mult, ALU.add, [ru, ru2], [ru2])
                    act(uu2[:, 0:511], uu2[:, 0:511], AF.Tanh, [ru2], [ru2], scale=0.7978845608028654)
                    ts("dve", uu2[:, 0:511], uu2[:, 0:511], 1.0, 0.5, ALU.add, ALU.mult, [ru2], [ru2])
                    tt("dve", gel[:, hc, 0:511], uu2[:, 0:511], uu[:, 0:511], ALU.mult, [ru, ru2], [], ws=[R("gel")])
                if kv == "k":
                    for hc in range(2):
                        mm(ps[4][:, 0:511], w2[:, hc, :], gel[:, hc, 0:511], hc == 0, hc == 1,
                           [R("w2"), R("gel")], [PSR[4]])
                    cp("act", kcT[:, g, 0:511], ps[4][:, 0:511], [PSR[4]], [], ws=[R("kcT")])
                else:
                    for ct in range(4):
                        for hc in range(2):
                            mm(ps[4][:, ct * 128:(ct + 1) * 128], gel[:, hc, ct * 128:(ct + 1) * 128], w2[:, hc, :],
                               hc == 0, hc == 1, [R("w2"), R("gel")], [PSR[4]])
                    cp("act", vcs[:, :, g, :], ps[4][:].rearrange("p (c d) -> p c d", c=4), [PSR[4]], [],
                       ws=[R("vcs")])
                memset("pool", gel[:, :, 511:512], 0.0, [R("gel")])

    if stages >= 3:
        areset()
        kselT = al("kselT", [128, 2, T], BF16)
        vsel = al("vsel", [128, 64, 256], BF16)
        tba = al("tba", TBL["tba"].shape, BF16)
        tb5 = al("tb5", TBL["tb5"].shape, BF16)
        dma("sp", kselT, kselT_d.rearrange("(g p) t -> p g t", p=128), [R("kselT_d")], [R("kselT")])
        dma("sp", vsel, vsel_d.rearrange("(kt p) c -> p kt c", p=128), [R("vsel_d")], [R("vsel")])
        dma("sp", tba, tba_d[:, :], [], [R("tba")])
        dma("sp", tb5, tb5_d[:, :], [], [R("tb5")])
        RKS, RVS, RTA, RT5 = R("kselT"), R("vsel"), R("tba"), R("tb5")

        def TA(name, a, b):
            o, n = tao[name]
            return tba[:, o + a:o + b]

        def T5(name, a, b, rows=5):
            o, n = t5o[name]
            return tb5[0:rows, o + a:o + b]

        qTb = [al("qTb%d" % i, [128, 8, 128], BF16) for i in range(2)]
        kwT = [al("kwT%d" % i, [128, 2, 640], BF16) for i in range(2)]
        vw = [al("vw%d" % i, [128, 5, 256], BF16) for i in range(2)]
        grep = [al("grep%d" % i, [128, 12, 128]) for i in range(2)]
        PTc = [al("PTc%d" % i, [128, 512], BF16) for i in range(4)]
        Pn = [al("Pn%d" % i, [128, 512], BF16) for i in range(4)]
        PTr = [al("PTr%d" % i, [128, 512], BF16) for i in range(3)]
        browS = [al("browS%d" % i, [5, 512], BF16) for i in range(2)]
        browC = [al("browC%d" % i, [5, 512], BF16) for i in range(2)]
        Rz = [al("Rz%d" % i, [128, 512]) for i in range(2)]
        Rg = [al("Rg%d" % i, [128, 512]) for i in range(2)]
        tmpo = [al("tmpo%d" % i, [128, 512]) for i in range(2)]
        acc = [al("acc%d" % i, [128, 512]) for i in range(2)]
        accb = [al("accb%d" % i, [128, 512], BF16) for i in range(2)]
        iw = al("iw", [128, 128])
        iw2 = al("iw2", [128, 128])
        m8a = al("m8a", [128, 8])
        m8b = al("m8b", [128, 8])
        mb = al("mb", [128, 128], BF16)
        mbT = [al("mbT%d" % i, [128, 128], BF16) for i in range(2)]
        cnt = {"pt": 0, "s": 0, "oz": 0, "ep": 0}

        pend = {"f": None}

        def flush_pv():
            if pend["f"] is not None:
                pend["f"]()
                pend["f"] = None

        def score_and_pv(mms, PT, PTres, vl, vreads, ob, zb, first, last):
            sbk = cnt["s"] % 2
            cnt["s"] += 1
            for n_, (l_, r_, rd_) in enumerate(mms):
                mm(ps3(sbk), l_, r_, n_ == 0, n_ == len(mms) - 1, rd_, [PSR[sbk]])
            act(PT, ps[sbk][:], AF.Exp, [PSR[sbk]], [PTres])
            flush_pv()

            def pv():
                mm(ps[ob][:], vl, PT, first, last, vreads + [PTres], [PSR[ob]])
                mm(ps[zb][:], onesb, PT, first, last, [RTB, PTres], [PSR[zb]])
            pend["f"] = pv

        def epilogue(br, ob, zb, grp, grpr, ac, acr, first_branch, cmp_pts=None):
            k = cnt["ep"] % 2
            cnt["ep"] += 1
            rz, rg, tm = Rz[k], Rg[k], tmpo[k]
            rzr, rgr, tmr = R("Rz%d" % k), R("Rg%d" % k), R("tmpo%d" % k)
            ts("dve", rz, ps[zb][:], 1e-30, None, ALU.add, None, [PSR[zb]], [rzr])
            recip(rz, rz, [rzr], [rzr])
            if cmp_pts is not None:
                for (j, ptc, ptr, pn, pnr) in cmp_pts:
                    tt("pool", pn, ptc, rz, ALU.mult, [ptr, rzr], [pnr])
            tt("pool", rg.rearrange("p (h q) -> p h q", h=4), rz.rearrange("p (h q) -> p h q", h=4),
               grp[:, br::3, :], ALU.mult, [rzr, grpr], [rgr])
            if first_branch:
                tt("dve", ac, ps[ob][:], rg, ALU.mult, [PSR[ob], rgr], [acr])
            else:
                tt("dve", tm, ps[ob][:], rg, ALU.mult, [PSR[ob], rgr], [tmr])
                tt("pool", ac, ac, tm, ALU.add, [tmr, acr], [acr])

        for i in range(NQB):
            q0 = 4096 + 128 * i
            oc = 128 * i
            b2 = i % 2
            qt, kw_, vw_ = qTb[b2], kwT[b2], vw[b2]
            rq, rkw, rvw = R("qTb%d" % b2), R("kwT%d" % b2), R("vw%d" % b2)
            dma("sp", qt, qT_d[:, oc:oc + 128].rearrange("(h p) q -> p h q", p=128), [R("qT_d")], [rq])
            dma("sp", kw_, kwinT_d[:, q0 - 512:q0 + 128].rearrange("(g p) t -> p g t", p=128), [R("kwinT_d")], [rkw])
            dma("sp", vw_, vwin_d[q0 - 512:q0 + 128, :].rearrange("(kt p) c -> p kt c", p=128), [R("vwin_d")], [rvw])
            for g in range(2):
                ig = 2 * i + g
                k2 = ig % 2
                gp, gpr = grep[k2], R("grep%d" % k2)
                dma("sp", gp, gT_d[12 * g:12 * g + 12, oc:oc + 128].partition_broadcast(128), [R("gT_d")], [gpr])
                bS, bSr = browS[k2], R("browS%d" % k2)
                bC, bCr = browC[k2], R("browC%d" % k2)
                cp("pool", bS.rearrange("p (h q) -> p h q", h=4),
                   T5("src", i * 8 + 4 * g, i * 8 + 4 * g + 4).unsqueeze(2).to_broadcast([5, 4, 128]), [RT5], [bSr])
                cp("pool", bS[0:1, :], T5("qrow", g * 512, (g + 1) * 512, rows=1), [RT5, bSr], [bSr])
                cp("pool", bC.rearrange("p (h q) -> p h q", h=4),
                   T5("srcc", i * 8 + 4 * g, i * 8 + 4 * g + 4).unsqueeze(2).to_broadcast([5, 4, 128]), [RT5], [bCr])
                cp("pool", bC[0:1, :], T5("qrow", g * 512, (g + 1) * 512, rows=1), [RT5, bCr], [bCr])
                bS3 = bS.rearrange("p (h q) -> p h q", h=4)
                bC3 = bC.rearrange("p (h q) -> p h q", h=4)
                rhsQ = qt[:, 4 * g:4 * g + 4, :]
                ac, acr = acc[k2], R("acc%d" % k2)
                ob, zb = (2, 3) if cnt["oz"] % 2 == 0 else (4, 5)
                cnt["oz"] += 1
                js = [j for j in range(4) if 4065 + 128 * i - 2048 * j + 127 >= 0]
                cmp_pts = []
                for n_, j in enumerate(js):
                    mms = [(kcT[:, g, j * 128:(j + 1) * 128], rhsQ, [R("kcT"), rq]),
                           (T5("posrows_c", j * 128, (j + 1) * 128), bC3, [RT5, bCr])]
                    if (i, j) in CMP_PART:
                        t_ = CMP_PART[(i, j)]
                        mms.append((identb, bc4(TA("cmpm", t_ * 128, (t_ + 1) * 128)), [RTB, RTA]))
                    score_and_pv(mms, PTc[j], R("PTc%d" % j), vcs[:, j, g, :], [R("vcs")], ob, zb,
                                 n_ == 0, n_ == len(js) - 1)
                    cmp_pts.append((j, PTc[j], R("PTc%d" % j), Pn[j], R("Pn%d" % j)))
                flush_pv()
                epilogue(0, ob, zb, gp, gpr, ac, acr, True, cmp_pts)
                nmm = 4 * len(js)
                c_ = 0
                for hh in range(4):
                    for j in js:
                        mm(ps[6][:, 0:128], Pn[j][:, hh * 128:(hh + 1) * 128], TA("ovl", j * 128, (j + 1) * 128),
                           c_ == 0, c_ == nmm - 1, [R("Pn%d" % j), RTA], [PSR[6]])
                        c_ += 1
                tt("dve", iw, ps[6][:, 0:128], TF("tblA", 64 - 2 * i, 192 - 2 * i), ALU.mult, [PSR[6], RTF], [R("iw")])
                tt("dve", iw, iw, TF("tblB", 64 - 2 * i, 192 - 2 * i), ALU.add, [R("iw"), RTF], [R("iw")])
                tt("dve", iw, iw, TF("F0"), ALU.max, [R("iw"), RTF], [R("iw")])
                max8(m8a, iw, [R("iw")], [R("m8a")])
                mrepl(iw2, m8a, iw, -3e9, [R("iw"), R("m8a")], [R("iw2")])
                max8(m8b, iw2, [R("iw2")], [R("m8b")])
                ts("dve", mb, iw, m8b[:, 7:8], NEG, ALU.is_lt, ALU.mult, [R("iw"), R("m8b")], [R("mb")])
                trp(psb[6][:, 512:640], mb, identb, [R("mb"), RTB], [PSR[6]])
                mt, mtr = mbT[k2], R("mbT%d" % k2)
                cp("act", mt, psb[6][:, 512:640], [PSR[6]], [mtr])
                ob, zb = (2, 3) if cnt["oz"] % 2 == 0 else (4, 5)
                cnt["oz"] += 1
                for t_ in range(5):
                    kt = 28 + i + t_
                    mms = [(kw_[:, g, t_ * 128:(t_ + 1) * 128], rhsQ, [rkw, rq]),
                           (T5("posrows", kt * 128, (kt + 1) * 128), bS3, [RT5, bSr])]
                    if t_ == 0:
                        mms.append((identb, bc4(TB("band")), [RTB]))
                    if t_ == 4:
                        mms.append((identb, bc4(TB("tri")), [RTB]))
                    k3 = cnt["pt"] % 3
                    cnt["pt"] += 1
                    score_and_pv(mms, PTr[k3], R("PTr%d" % k3), vw_[:, t_, g * 128:(g + 1) * 128], [rvw], ob, zb,
                                 t_ == 0, t_ == 4)
                flush_pv()
                epilogue(2, ob, zb, gp, gpr, ac, acr, False)
                ob, zb = (2, 3) if cnt["oz"] % 2 == 0 else (4, 5)
                cnt["oz"] += 1
                nkt = 33 + i
                for kt in range(nkt):
                    mms = [(kselT[:, g, kt * 128:(kt + 1) * 128], rhsQ, [RKS, rq]),
                           (T5("posrows", kt * 128, (kt + 1) * 128), bS3, [RT5, bSr]),
                           (TA("E", kt * 128, (kt + 1) * 128), bc4(mt), [RTA, mtr])]
                    if kt == nkt - 1:
                        mms.append((identb, bc4(TB("tri")), [RTB]))
                    k3 = cnt["pt"] % 3
                    cnt["pt"] += 1
                    score_and_pv(mms, PTr[k3], R("PTr%d" % k3), vsel[:, kt, g * 128:(g + 1) * 128], [RVS], ob, zb,
                                 kt == 0, kt == nkt - 1)
                flush_pv()
                epilogue(1, ob, zb, gp, gpr, ac, acr, False)
                ab, abr = accb[k2], R("accb%d" % k2)
                cp("act", ab, ac, [acr], [abr])
                dma("sp", yT_d[g * 512:(g + 1) * 512, oc:oc + 128].rearrange("(h p) q -> p h q", p=128),
                    ab.rearrange("p (h q) -> p h q", h=4), [abr], [], ws=[R("yT_d")])

    if stages >= 4:
        areset()
        wg = al("wg", [16, 512])
        bg = al("bg", [1, 512])
        nwc = al("nwc", [128, 2])
        dma("sp", wg, gla_w_gate[:, :], [], [R("wg")])
        dma("sp", bg, gla_b_gate[:, :], [], [R("bg")])
        dma("sp", nwc, gla_nw_col[:, :], [], [R("nwc")])
        St = [al("St%d" % h_, [128, 256]) for h_ in range(4)]
        Sb = [al("Sb%d" % h_, [128, 256], BF16) for h_ in range(4)]
        for h_ in range(4):
            memset("pool", St[h_], 0.0, [R("St%d" % h_)])
            memset("pool", Sb[h_], 0.0, [R("Sb%d" % h_)])
        kT4 = [al("kT4%d" % i, [128, 4, 128], BF16) for i in range(2)]
        ktok = [al("ktok%d" % i, [128, 512], BF16) for i in range(2)]
        vtok = [al("vtok%d" % i, [128, 1024], BF16) for i in range(2)]
        aTs = [al("aTs%d" % i, [16, 128]) for i in range(2)]
        qT4 = [al("qT4%d" % i, [128, 4, 128], BF16) for i in range(2)]
        rT8 = [al("rT8%d" % i, [128, 8, 128], BF16) for i in range(2)]
        la0 = al("la0", [128, 512])
        la = al("la", [128, 512])
        ebT = al("ebT", [128, 512])
        enbT = al("enbT", [128, 512])
        erev = al("erev", [128, 512])
        ktil = al("ktil", [128, 4, 128], BF16)
        kk = al("kk", [128, 512], BF16)
        qtil = al("qtil", [128, 4, 128], BF16)
        PTg = al("PTg", [128, 4, 128], BF16)
        sqa = [al("sq%d" % i, [128, 512], BF16) for i in range(2)]
        rs = al("rs", [128, 512])
        ya = [al("ya%d" % i, [128, 512]) for i in range(2)]
        ystg = [al("ystg%d" % i, [128, 4, 128], BF16) for i in range(2)]
        onecol = TF("onesf", 0, 1)
        for ch in range(64):
            c0 = ch * 128
            own = ch >= 32
            oc = c0 - TO
            b2 = ch % 2
            k4, kt_, vt_, at_ = kT4[b2], ktok[b2], vtok[b2], aTs[b2]
            rk4, rkt, rvt, rat = R("kT4%d" % b2), R("ktok%d" % b2), R("vtok%d" % b2), R("aTs%d" % b2)
            dma("sp", k4, kglaT_d[:, c0:c0 + 128].rearrange("(h p) t -> p h t", p=128), [R("kglaT_d")], [rk4])
            dma("sp", kt_, kgla_d[c0:c0 + 128, :], [R("kgla_d")], [rkt])
            dma("sp", vt_, vgla_d[c0:c0 + 128, :], [R("vgla_d")], [rvt])
            dma("sp", at_, aT_d[:, c0:c0 + 128], [R("aT_d")], [rat])
            if own:
                q4, r8 = qT4[b2], rT8[b2]
                rq4, rr8 = R("qT4%d" % b2), R("rT8%d" % b2)
                dma("sp", q4, qglaT_d[:, oc:oc + 128].rearrange("(h p) t -> p h t", p=128), [R("qglaT_d")], [rq4])
                dma("sp", r8, rT_d[:, oc:oc + 128].rearrange("(j p) t -> p j t", p=128), [R("rT_d")], [rr8])
            mm(ps[0][:], at_[0:16, :], wg[0:16, :], True, False, [rat, R("wg")], [PSR[0]])
            mm(ps[0][:], TF("onesf")[0:1, 0:128], bg[0:1, :], False, True, [RTF, R("bg")], [PSR[0]])
            act(la0, ps[0][:], AF.Exp, [PSR[0]], [R("la0")], scale=-1.0)
            act(la, la0, AF.Ln, [R("la0"), RTF], [R("la")], bias=onecol)
            for h_ in range(4):
                mm(ps[1][:, h_ * 128:(h_ + 1) * 128], la[:, h_ * 128:(h_ + 1) * 128], TF("triI"), True, True,
                   [R("la"), RTF], [PSR[1]])
            mm(ps[2][:], TF("triR"), la, True, True, [R("la"), RTF], [PSR[2]])
            act(ebT, ps[1][:], AF.Exp, [PSR[1]], [R("ebT")])
            act(enbT, ps[1][:], AF.Exp, [PSR[1]], [R("enbT")], scale=-1.0)
            act(erev, ps[2][:], AF.Exp, [PSR[2]], [R("erev")])
            tt("dve", ktil.rearrange("p h t -> p (h t)"), k4.rearrange("p h t -> p (h t)"), enbT, ALU.mult,
               [rk4, R("enbT")], [R("ktil")])
            tt("pool", kk, kt_, erev, ALU.mult, [rkt, R("erev")], [R("kk")])
            if own:
                tt("dve", qtil.rearrange("p h t -> p (h t)"), q4.rearrange("p h t -> p (h t)"), ebT, ALU.mult,
                   [rq4, R("ebT")], [R("qtil")])
                for h_ in range(4):
                    mm(ps[3][:, h_ * 128:(h_ + 1) * 128], ktil[:, h_, :], qtil[:, h_, :], True, True,
                       [R("ktil"), R("qtil")], [PSR[3]])
                tt("dve", PTg, ps3(3), bc4(TB("cmask")), ALU.mult, [PSR[3], RTB], [R("PTg")])
                for dvc in range(2):
                    pb = 4 + dvc
                    for h_ in range(4):
                        mm(ps[pb][:, h_ * 128:(h_ + 1) * 128], vt_[:, h_ * 256 + dvc * 128:h_ * 256 + (dvc + 1) * 128],
                           PTg[:, h_, :], True, False, [rvt, R("PTg")], [PSR[pb]])
                        mm(ps[pb][:, h_ * 128:(h_ + 1) * 128], Sb[h_][:, dvc * 128:(dvc + 1) * 128], qtil[:, h_, :],
                           False, True, [R("Sb%d" % h_), R("qtil")], [PSR[pb]])
                    act(sqa[dvc], ps[pb][:], AF.Square, [PSR[pb]], [R("sq%d" % dvc)])
                mm(ps[6][:], onesb, sqa[0], True, False, [RTB, R("sq0")], [PSR[6]])
                mm(ps[6][:], onesb, sqa[1], False, True, [RTB, R("sq1")], [PSR[6]])
                ts("dve", rs, ps[6][:], 1.0 / 256, RMS_EPS, ALU.mult, ALU.add, [PSR[6]], [R("rs")])
                act(rs, rs, AF.Sqrt, [R("rs")], [R("rs")])
                recip(rs, rs, [R("rs")], [R("rs")])
                for dvc in range(2):
                    pb = 4 + dvc
                    y_, yr = ya[dvc], R("ya%d" % dvc)
                    tt("dve", y_, ps[pb][:], rs, ALU.mult, [PSR[pb], R("rs")], [yr])
                    ys, ysr = ystg[dvc], R("ystg%d" % dvc)
                    stt("dve", ys, y_.rearrange("p (h t) -> p h t", h=4), nwc[:, dvc:dvc + 1], r8[:, dvc::2, :],
                        ALU.mult, ALU.mult, [yr, R("nwc"), rr8], [ysr])
                    dst = yT_d[1024:2048, oc:oc + 128].rearrange("(h c p) t -> p h c t", c=2, p=128)[:, :, dvc, :]
                    dma("sp", dst, ys, [ysr], [], ws=[R("yT_d")])
            for hp in range(2):
                for hq in range(2):
                    h_ = 2 * hp + hq
                    mm(ps[7][:, hq * 256:(hq + 1) * 256], kk[:, h_ * 128:(h_ + 1) * 128], vt_[:, h_ * 256:(h_ + 1) * 256],
                       True, True, [R("kk"), rvt], [PSR[7]])
                for hq in range(2):
                    h_ = 2 * hp + hq
                    stt("dve", St[h_], St[h_], ebT[:, h_ * 128 + 127:h_ * 128 + 128], ps[7][:, hq * 256:(hq + 1) * 256],
                        ALU.mult, ALU.add, [R("ebT"), PSR[7], R("St%d" % h_)], [R("St%d" % h_)])
                    cp("act", Sb[h_], St[h_], [R("St%d" % h_)], [R("Sb%d" % h_)])

    if stages >= 5:
        areset()
        wo = al("wo", [128, 16, D], BF16)
        dma("pool", wo, w_out.rearrange("(c p) n -> p c n", p=128), [], [R("wo")])
        g1r = al("g1r", [128, D])
        l1g = al("l1g", [128, D])
        l1b = al("l1b", [128, D])
        sc2r = al("sc2r", [128, D])
        sh2r = al("sh2r", [128, D])
        dma("sp", g1r, modrow_d[0:1, :].partition_broadcast(128), [R("modrow_d")], [R("g1r")])
        dma("sp", sh2r, modrow_d[1:2, :].partition_broadcast(128), [R("modrow_d")], [R("sh2r")])
        dma("sp", sc2r, modrow_d[2:3, :].partition_broadcast(128), [R("modrow_d")], [R("sc2r")])
        dma("sp", l1g, ln1_g[0:1, :].partition_broadcast(128), [], [R("l1g")])
        dma("sp", l1b, ln1_b[0:1, :].partition_broadcast(128), [], [R("l1b")])
        ts("dve", sc2r, sc2r, 1.0, None, ALU.add, None, [R("sc2r")], [R("sc2r")])
        wrt = al("wrt", [128, 16, NE])
        brt = al("brt", [128, NE])
        dma("sp", wrt, w_router.rearrange("(c p) e -> p c e", p=128), [], [R("wrt")])
        dma("sp", brt, b_router[0:1, :].partition_broadcast(128), [], [R("brt")])
        yTt = [al("yTt%d" % i, [128, 16, 128], BF16) for i in range(2)]
        xt = [al("xt%d" % i, [128, D]) for i in range(2)]
        t1 = al("t1", [128, D])
        vv = al("vv", [128, D])
        sqv = al("sqv", [128, D])
        h2f = al("h2f", [128, D])
        h2b = al("h2b", [128, D], BF16)
        h2T = al("h2T", [128, 16, 128])
        st = al("st", [128, 16])
        lg = al("lg", [128, NE])
        m8r = al("m8r", [128, 8])
        w4 = al("w4", [128, 8])
        maskf = al("maskf", [128, NE])
        maskb = al("maskb", [128, NE], BF16)
        csum = al("csum", [128, NE])
        posv = al("posv", [128, NE])
        oh = al("oh", [128, NE])
        slf = al("slf", [128, 4])
        sinfo = [al("sinfo%d" % k, [128, 2]) for k in range(4)]
        zt = al("zt", [128, 1024])
        memset("pool", zt, 0.0, [R("zt")])
        memset("pool", csum, 0.0, [R("csum")])
        dma("sp", slotinfo_d.rearrange("(p a) c -> p (a c)", p=128), zt, [R("zt")], [R("slotinfo_d")])

        def layer_norm(src, srcr, dst, dstr, gam, gamr, bet, betr):
            rsum(st[:, 0:1], src, [srcr], [R("st")])
            tt("pool", sqv, src, src, ALU.mult, [srcr], [R("sqv")])
            rsum(st[:, 1:2], sqv, [R("sqv"), R("st")], [R("st")])
            ts("dve", st[:, 2:3], st[:, 0:1], 1.0 / D, None, ALU.mult, None, [R("st")], [R("st")])
            tt("dve", st[:, 3:4], st[:, 2:3], st[:, 2:3], ALU.mult, [R("st")], [R("st")])
            stt("dve", st[:, 4:5], st[:, 1:2], 1.0 / D, st[:, 3:4], ALU.mult, ALU.subtract, [R("st")], [R("st")])
            ts("dve", st[:, 4:5], st[:, 4:5], LN_EPS, None, ALU.add, None, [R("st")], [R("st")])
            act(st[:, 5:6], st[:, 4:5], AF.Sqrt, [R("st")], [R("st")])
            recip(st[:, 6:7], st[:, 5:6], [R("st")], [R("st")])
            ts("dve", dst, src, st[:, 2:3], st[:, 6:7], ALU.subtract, ALU.mult, [srcr, R("st")], [dstr])
            tt("pool", dst, dst, gam, ALU.mult, [dstr, gamr], [dstr])
            tt("dve", dst, dst, bet, ALU.add, [dstr, betr], [dstr])

        for tl in range(NQB):
            r0 = tl * 128
            b2 = tl % 2
            yt_, x_ = yTt[b2], xt[b2]
            ryt, rx = R("yTt%d" % b2), R("xt%d" % b2)
            dma("sp", yt_, yT_d[:, r0:r0 + 128].rearrange("(j p) t -> p j t", p=128), [R("yT_d")], [ryt])
            dma("sp", x_, x_ctx[TO + r0:TO + r0 + 128, :], [], [rx])
            for nb in range(4):
                for fc in range(16):
                    mm(ps[nb][:], yt_[:, fc, :], wo[:, fc, nb * 512:(nb + 1) * 512], fc == 0, fc == 15,
                       [ryt, R("wo")], [PSR[nb]])
                tt("dve", t1[:, nb * 512:(nb + 1) * 512], ps[nb][:], g1r[:, nb * 512:(nb + 1) * 512], ALU.mult,
                   [PSR[nb], R("g1r")], [], ws=[R("t1")])
            stt("dve", vv, x_, DN_ALPHA, t1, ALU.mult, ALU.add, [rx, R("t1")], [R("vv")])
            layer_norm(vv, R("vv"), t1, R("t1"), l1g, R("l1g"), l1b, R("l1b"))
            dma("sp", x1_d[r0:r0 + 128, :], t1, [R("t1")], [], ws=[R("x1_d")])
            tt("pool", h2f, t1, sc2r, ALU.mult, [R("t1"), R("sc2r")], [R("h2f")])
            tt("dve", h2f, h2f, sh2r, ALU.add, [R("h2f"), R("sh2r")], [R("h2f")])
            cp("act", h2b, h2f, [R("h2f")], [R("h2b")])
            dma("sp", h2_d[r0:r0 + 128, :], h2b, [R("h2b")], [], ws=[R("h2_d")])
            for q4_ in range(4):
                pb = 4 + q4_
                for k_ in range(4):
                    dc = q4_ * 4 + k_
                    trp(ps[pb][:, k_ * 128:(k_ + 1) * 128], h2f[:, dc * 128:(dc + 1) * 128], identf,
                        [R("h2f"), RTF], [PSR[pb]])
                if q4_ % 2 == 0:
                    cp("act", h2T[:, q4_ * 4:(q4_ + 1) * 4, :], ps3(pb), [PSR[pb]], [], ws=[R("h2T")])
                else:
                    cp("dve", h2T[:, q4_ * 4:(q4_ + 1) * 4, :], ps3(pb), [PSR[pb]], [], ws=[R("h2T")])
            for dc in range(16):
                mm(ps[4][:, 0:NE], h2T[:, dc, :], wrt[:, dc, :], dc == 0, dc == 15, [R("h2T"), R("wrt")], [PSR[4]])
            tt("dve", lg, ps[4][:, 0:NE], brt, ALU.add, [PSR[4], R("brt")], [R("lg")])
            max8(m8r, lg, [R("lg")], [R("m8r")])
            ts("dve", w4[:, 0:4], m8r[:, 0:4], m8r[:, 0:1], None, ALU.subtract, None, [R("m8r")], [R("w4")])
            act(w4[:, 0:4], w4[:, 0:4], AF.Exp, [R("w4")], [R("w4")])
            rsum(w4[:, 4:5], w4[:, 0:4], [R("w4")], [R("w4")])
            recip(w4[:, 5:6], w4[:, 4:5], [R("w4")], [R("w4")])
            ts("dve", w4[:, 0:4], w4[:, 0:4], w4[:, 5:6], None, ALU.mult, None, [R("w4")], [R("w4")])
            ts("dve", maskf, lg, m8r[:, 3:4], None, ALU.is_ge, None, [R("lg"), R("m8r")], [R("maskf")])
            cp("dve", maskb, maskf, [R("maskf")], [R("maskb")])
            mm(ps[5][:, 0:NE], TB("ltri"), maskb, True, True, [RTB, R("maskb")], [PSR[5]])
            mm(ps[5][:, NE:2 * NE], onesb, maskb, True, True, [RTB, R("maskb")], [PSR[5]])
            tt("dve", posv, ps[5][:, 0:NE], csum, ALU.add, [PSR[5], R("csum")], [R("posv")])
            tt("dve", csum, csum, ps[5][:, NE:2 * NE], ALU.add, [PSR[5], R("csum")], [R("csum")])
            tt("dve", posv, posv, TF("iotaEC1"), ALU.add, [R("posv"), RTF], [R("posv")])
            for k_ in range(4):
                ts("dve", oh, lg, m8r[:, k_:k_ + 1], None, ALU.is_equal, None, [R("lg"), R("m8r")], [R("oh")])
                tt("dve", oh, oh, posv, ALU.mult, [R("oh"), R("posv")], [R("oh")])
                rsum(slf[:, k_:k_ + 1], oh, [R("oh"), R("slf")], [R("slf")])
            ts("dve", slot_i[:, tl, :], slf, -1.0, None, ALU.add, None, [R("slf")], [R("slot_i%d" % tl)])
            for k_ in range(4):
                si, sir = sinfo[k_], R("sinfo%d" % k_)
                cp("pool", si[:, 0:1], TF("tokid", tl, tl + 1), [RTF], [sir])
                cp("pool", si[:, 1:2], w4[:, k_:k_ + 1], [R("w4"), sir], [sir])
                scatter(slotinfo_d[:, :], slot_i[:, tl, k_:k_ + 1], si, [sir, R("slot_i%d" % tl)], [],
                        ws=[R("slotinfo_d")])

    if stages >= 6:
        areset()
        xT = al("xT", [128, 16, CAP], BF16)
        aT_ = al("actT", [128, 16, CAP], BF16)
        NWB = 2
        wgb = [al("wgb%d" % i, [128, 16, 512], BF16) for i in range(NWB)]
        xg = [al("xg%d" % i, [128, D], BF16) for i in range(3)]
        sinfA = [al("sinfA%d" % i, [128, 16, 2]) for i in range(2)]
        sidxA = [al("sidxA%d" % i, [128, 16], I32) for i in range(2)]
        wslA = [al("wslA%d" % i, [128, 16]) for i in range(2)]
        bgu = al("bgu", [128, NE * 32])
        dma("sp", bgu, b_gu_col[:, :], [], [R("bgu")])
        g1_ = [al("g1_%d" % i, [128, 512]) for i in range(2)]
        g2_ = [al("g2_%d" % i, [128, 512]) for i in range(2)]
        bdr = [al("bdr%d" % i, [128, 512]) for i in range(2)]
        yst = [al("yst%d" % i, [128, 512]) for i in range(2)]
        NJT = CAP // 128
        NSG = CAP // 512
        cnt6 = {"ec": 0, "xg": 0}

        wlist = []
        for e_ in range(NE):
            for blk in range(8):
                wlist.append(("gu", e_, blk))
            for db in range(4):
                wlist.append(("d", e_, db))

        def wload(n):
            kind, e_, blk = wlist[n]
            w, wr = wgb[n % NWB], R("wgb%d" % (n % NWB))
            src = w_gate_up if kind == "gu" else w_down
            dma("pool", w, src[e_, :, blk * 512:(blk + 1) * 512].rearrange("(c p) n -> p c n", p=128), [], [wr])

        def prep_expert(e_):
            k = e_ % 2
            dma("sp", sinfA[k], slotinfo_d[e_ * CAP:(e_ + 1) * CAP, :].rearrange("(j p) c -> p j c", p=128),
                [R("slotinfo_d")], [R("sinfA%d" % k)])
            cp("dve", sidxA[k], sinfA[k][:, :, 0], [R("sinfA%d" % k)], [R("sidxA%d" % k)])
            cp("dve", wslA[k], sinfA[k][:, :, 1], [R("sinfA%d" % k)], [R("wslA%d" % k)])

        def gather_tile(e_, jt):
            k = e_ % 2
            b3 = (e_ * NJT + jt) % 3
            gather(xg[b3], h2_d[:, :], sidxA[k][:, jt:jt + 1], [R("sidxA%d" % k), R("h2_d")], [R("xg%d" % b3)])

        def transpose_tile(e_, jt):
            b3 = (e_ * NJT + jt) % 3
            xg_, xgr = xg[b3], R("xg%d" % b3)
            for half in range(2):
                pb = half
                for k_ in range(8):
                    dc = half * 8 + k_
                    trp(psb[pb][:, k_ * 128:(k_ + 1) * 128], xg_[:, dc * 128:(dc + 1) * 128], identb,
                        [xgr, RTB], [PSR[pb]])
                src3 = psb[pb].rearrange("p (k t) -> p k t", k=8)
                if half == 0:
                    cp("act", xT[:, 0:8, jt * 128:(jt + 1) * 128], src3, [PSR[pb]], [], ws=[R("xT")])
                else:
                    cp("dve", xT[:, 8:16, jt * 128:(jt + 1) * 128], src3, [PSR[pb]], [], ws=[R("xT")])

        def gather_step(e_, jt):
            if jt + 2 < NJT:
                gather_tile(e_, jt + 2)
            transpose_tile(e_, jt)

        wload(0)
        prep_expert(0)
        gather_tile(0, 0)
        gather_tile(0, 1)
        for jt in range(NJT):
            gather_step(0, jt)
        wn = 0
        for e_ in range(NE):
            wsl = wslA[e_ % 2]
            wslr = R("wslA%d" % (e_ % 2))
            for blk in range(8):
                if wn + 1 < len(wlist):
                    wload(wn + 1)
                w, wr = wgb[wn % NWB], R("wgb%d" % (wn % NWB))
                wn += 1
                for f4 in range(4):
                    fcg = blk * 4 + f4
                    fc = fcg % 16
                    bcol_ = bgu[:, e_ * 32 + fcg:e_ * 32 + fcg + 1]
                    for sg in range(NSG):
                        ec = cnt6["ec"]
                        cnt6["ec"] += 1
                        pb = 2 + ec % 6
                        k2 = ec % 2
                        for dc in range(16):
                            mm(ps[pb][:], w[:, dc, f4 * 128:(f4 + 1) * 128], xT[:, dc, sg * 512:(sg + 1) * 512],
                               dc == 0, dc == 15, [wr, R("xT")], [PSR[pb]])
                        ga, gar = g1_[k2], R("g1_%d" % k2)
                        gb, gbr = g2_[k2], R("g2_%d" % k2)
                        dstA = aT_[:, fc, sg * 512:(sg + 1) * 512]
                        if fcg < 16:
                            ts("dve", ga, ps[pb][:], bcol_, 7.0, ALU.add, ALU.min, [PSR[pb], R("bgu")], [gar])
                            act(gb, ga, AF.Sigmoid, [gar], [gbr], scale=1.702)
                            tt("dve", dstA, ga, gb, ALU.mult, [gar, gbr], [], ws=[R("actT")])
                        else:
                            ts("dve", ga, ps[pb][:], bcol_, 7.0, ALU.add, ALU.min, [PSR[pb], R("bgu")], [gar])
                            ts("dve", gb, ga, -7.0, 1.0, ALU.max, ALU.add, [gar], [gbr])
                            tt("dve", dstA, dstA, gb, ALU.mult, [gbr, R("actT")], [], ws=[R("actT")])
            if e_ + 1 < NE:
                prep_expert(e_ + 1)
                gather_tile(e_ + 1, 0)
                gather_tile(e_ + 1, 1)
            step = 0
            for db in range(4):
                if wn + 1 < len(wlist):
                    wload(wn + 1)
                w, wr = wgb[wn % NWB], R("wgb%d" % (wn % NWB))
                wn += 1
                bd, bdrr = bdr[db % 2], R("bdr%d" % (db % 2))
                dma("sp", bd, b_down[e_:e_ + 1, db * 512:(db + 1) * 512].partition_broadcast(128), [], [bdrr])
                for jt in range(NJT):
                    ec = cnt6["ec"]
                    cnt6["ec"] += 1
                    pb = 2 + ec % 6
                    k2 = ec % 2
                    for fc in range(16):
                        mm(ps[pb][:], aT_[:, fc, jt * 128:(jt + 1) * 128], w[:, fc, :], fc == 0, fc == 15,
                           [wr, R("actT")], [PSR[pb]])
                    ga, gar = g1_[k2], R("g1_%d" % k2)
                    ys, ysr = yst[k2], R("yst%d" % k2)
                    tt("dve", ga, ps[pb][:], bd, ALU.add, [PSR[pb], bdrr], [gar])
                    act(ys, ga, AF.Copy, [gar, wslr], [ysr], scale=wsl[:, jt:jt + 1])
                    s0 = e_ * CAP + jt * 128
                    dma("sp", ybuf_d[db][s0:s0 + 128, :], ys, [ysr], [], ws=[R("ybuf_d")])
                    if e_ + 1 < NE and step % 4 == 3:
                        gather_step(e_ + 1, step // 4)
                    step += 1

    if stages >= 7:
        areset()
        g2r = al("g2r", [128, D])
        l2g = al("l2g", [128, D])
        l2b = al("l2b", [128, D])
        dma("sp", g2r, modrow_d[3:4, :].partition_broadcast(128), [R("modrow_d")], [R("g2r")])
        dma("sp", l2g, ln2_g[0:1, :].partition_broadcast(128), [], [R("l2g")])
        dma("sp", l2b, ln2_b[0:1, :].partition_broadcast(128), [], [R("l2b")])
        yk = [al("yk%d" % k, [128, D]) for k in range(8)]
        x1t = [al("x1t%d" % i, [128, D]) for i in range(2)]
        v2 = al("v2", [128, D])
        sqv = al("sqv2", [128, D])
        o2 = [al("o2%d" % i, [128, D]) for i in range(2)]
        st = al("st2", [128, 16])

        for tl in range(NQB):
            r0 = tl * 128
            b2 = tl % 2
            for k_ in range(4):
                y_, yr = yk[b2 * 4 + k_], R("yk%d" % (b2 * 4 + k_))
                for q_ in range(4):
                    gather(y_[:, q_ * 512:(q_ + 1) * 512], ybuf_d[q_][:, :], slot_i[:, tl, k_:k_ + 1],
                           [R("slot_i%d" % tl), R("ybuf_d")], [], ws=[yr])
            x_, rx = x1t[b2], R("x1t%d" % b2)
            dma("sp", x_, x1_d[r0:r0 + 128, :], [R("x1_d")], [rx])
            ya_, yb_, yc_, yd_ = [yk[b2 * 4 + k_] for k_ in range(4)]
            ra, rb, rc, rd = [R("yk%d" % (b2 * 4 + k_)) for k_ in range(4)]
            tt("dve", ya_, ya_, yb_, ALU.add, [ra, rb], [ra])
            tt("pool", yc_, yc_, yd_, ALU.add, [rc, rd], [rc])
            tt("dve", ya_, ya_, yc_, ALU.add, [ra, rc], [ra])
            tt("pool", ya_, ya_, g2r, ALU.mult, [ra, R("g2r")], [ra])
            stt("dve", v2, x_, DN_ALPHA, ya_, ALU.mult, ALU.add, [rx, ra], [R("v2")])
            o_, orr = o2[b2], R("o2%d" % b2)
            rsum(st[:, 0:1], v2, [R("v2")], [R("st2")])
            tt("pool", sqv, v2, v2, ALU.mult, [R("v2")], [R("sqv2")])
            rsum(st[:, 1:2], sqv, [R("sqv2"), R("st2")], [R("st2")])
            ts("dve", st[:, 2:3], st[:, 0:1], 1.0 / D, None, ALU.mult, None, [R("st2")], [R("st2")])
            tt("dve", st[:, 3:4], st[:, 2:3], st[:, 2:3], ALU.mult, [R("st2")], [R("st2")])
            stt("dve", st[:, 4:5], st[:, 1:2], 1.0 / D, st[:, 3:4], ALU.mult, ALU.subtract, [R("st2")], [R("st2")])
            ts("dve", st[:, 4:5], st[:, 4:5], LN_EPS, None, ALU.add, None, [R("st2")], [R("st2")])
            act(st[:, 5:6], st[:, 4:5], AF.Sqrt, [R("st2")], [R("st2")])
            recip(st[:, 6:7], st[:, 5:6], [R("st2")], [R("st2")])
            ts("dve", o_, v2, st[:, 2:3], st[:, 6:7], ALU.subtract, ALU.mult, [R("v2"), R("st2")], [orr])
            tt("pool", o_, o_, l2g, ALU.mult, [orr, R("l2g")], [orr])
            tt("dve", o_, o_, l2b, ALU.add, [orr, R("l2b")], [orr])
            dma("sp", out_d[r0:r0 + 128, :], o_, [orr], [], ws=[R("out")], final=True)
    else:
        fin = al("fin", [128, 512])
        memset("pool", fin, 0.0, [R("fin")])
        dma("sp", out_d[0:128, 0:512], fin, [R("fin")], [R("out")], final=True)

    print("ops:", {k: len(v) for k, v in P.ops.items()})
    P.emit()
    return nc


def core_inputs(inp, core):
    b, h = core // 2, core % 2
    TBL = tables(h)
    x = inp["x"]
    if h == 1:
        x_ctx = x[b]
    else:
        x_ctx = np.concatenate([x[b, TO:], x[b, :TO]], axis=0)
    f = lambda a: np.ascontiguousarray(a, dtype=np.float32)

    def col(v, n):
        return f(np.asarray(v).reshape(n, 128).T)

    m = {
        "x_ctx": f(x_ctx),
        "c_col": col(inp["c"][b], 16),
        "w_ada": f(inp["w_ada"][0]),
        "b_ada_col": col(inp["b_ada"][0], 96),
        "b_ada_row": f(inp["b_ada"][0].reshape(1, -1)),
        "w_in": f(inp["w_in"][0]),
        "gla_w_gate": f(inp["gla_w_gate"][0]),
        "gla_b_gate": f(inp["gla_b_gate"][0].reshape(1, -1)),
        "gla_nw_col": col(inp["gla_norm_w"][0], 2),
        "w_out": f(inp["w_out"][0]),
        "ln1_g": f(inp["ln1_g"][0].reshape(1, -1)),
        "ln1_b": f(inp["ln1_b"][0].reshape(1, -1)),
        "w_router": f(inp["w_router"][0]),
        "b_router": f(inp["b_router"][0].reshape(1, -1)),
        "w_gate_up": f(inp["w_gate_up"][0]),
        "b_gu_col": f(inp["b_gate_up"][0].reshape(NE, 32, 128).transpose(2, 0, 1).reshape(128, NE * 32)),
        "w_down": f(inp["w_down"][0]),
        "b_down": f(inp["b_down"][0]),
        "ln2_g": f(inp["ln2_g"][0].reshape(1, -1)),
        "ln2_b": f(inp["ln2_b"][0].reshape(1, -1)),
        "tbs": TBL["tbs"], "tba": TBL["tba"], "tf": TBL["tf"], "tb5": TBL["tb5"],
    }
    for kv in ("k", "v"):
        m["cmp_pe_" + kv] = f(inp["cmp_pe_" + kv][0])
        m["cmp_w1_" + kv] = f(inp["cmp_w1_" + kv][0])
        m["cmp_b1c_" + kv] = col(inp["cmp_b1_" + kv][0], 2)
        m["cmp_w2_" + kv] = f(inp["cmp_w2_" + kv][0])
    return m


_NC_CACHE = {}


def kernel(**inputs):
    if "nc" not in _NC_CACHE:
        _NC_CACHE["nc"] = build()
    nc = _NC_CACHE["nc"]
    in_maps = [core_inputs(inputs, c) for c in range(8)]
    res = run_bass_kernel_spmd(nc, in_maps, core_ids=list(range(8)))
    out = np.empty((4, T, D), np.float32)
    for c in range(8):
        b, h = c // 2, c % 2
        out[b, h * TO:(h + 1) * TO] = res.results[c]["out"]
    return out
```

```python
import numpy as np
import ml_dtypes
import concourse.bass as bass
import concourse.mybir as mybir
from concourse.bass_utils import run_bass_kernel_spmd

F32 = mybir.dt.float32
BF16 = mybir.dt.bfloat16
I32 = mybir.dt.int32
U32 = mybir.dt.uint32
AF = mybir.ActivationFunctionType
ALU = mybir.AluOpType
AX = mybir.AxisListType

D = 2048
T = 8192
TO = 4096
NQB = 32
IN_W = 5672
NEG = -30000.0
CAP = 2048
NE = 32
LN_EPS = 1e-5
RMS_EPS = 1e-6
DN_ALPHA = 2 ** 0.25


class Res:
    __slots__ = ("name", "w", "sw", "r")

    def __init__(self, name=""):
        self.name = name
        self.w = {}
        self.sw = {}
        self.r = {}


class Prog:
    ENGS = ("pe", "act", "dve", "pool", "sp")

    def __init__(self, nc, n_dsem=40):
        self.nc = nc
        self.ops = {e: [] for e in self.ENGS}
        self.cnt = {e: 0 for e in self.ENGS}
        self.known = {e: {} for e in self.ENGS}
        self.n_dsem = n_dsem
        self.dsem_use = [0] * n_dsem
        self.dsem_next = 0
        self.final_toks = []
        self.pending = {e: [] for e in self.ENGS}
        self.pred = None
        self._saved_known = None

    def begin_pred(self, key):
        self.pred = key
        self._saved_known = {e: dict(self.known[e]) for e in self.ENGS}

    def end_pred(self):
        self.pred = None
        self.known = self._saved_known
        self._saved_known = None

    def regload(self, ap, engs=("pe", "act", "dve", "sp")):
        for eng in engs:
            waits = self._deps(eng, (), (), ())
            self.ops[eng].append((waits, ("regload", ap), None, None))

    def barrier(self):
        toks = [("E", e, self.cnt[e]) for e in self.ENGS if self.cnt[e] > 0]
        toks += [("D", s, u * 16) for s, u in enumerate(self.dsem_use) if u > 0]
        for e in self.ENGS:
            self.pending[e] = list(toks)

    def _deps(self, eng, reads, writes, ws, extra=()):
        toks = list(extra) + self.pending[eng]
        self.pending[eng] = []
        for r in reads:
            for dd in (r.w, r.sw):
                for k, v in dd.items():
                    toks.append((k[0], k[1], v))
        for w in writes:
            for dd in (w.w, w.sw, w.r):
                for k, v in dd.items():
                    toks.append((k[0], k[1], v))
        for w in ws:
            for dd in (w.w, w.r):
                for k, v in dd.items():
                    toks.append((k[0], k[1], v))
        waits = {}
        kn = self.known[eng]
        for kind, key, val in toks:
            if kind == "E" and key == eng and eng == "pe":
                continue
            if kn.get((kind, key), 0) >= val:
                continue
            if waits.get((kind, key), 0) < val:
                waits[(kind, key)] = val
        for k, v in waits.items():
            kn[k] = v
        return list(waits.items())

    def _mark(self, tok, reads, writes, ws):
        k = (tok[0], tok[1])
        for r in reads:
            if r.r.get(k, 0) < tok[2]:
                r.r[k] = tok[2]
        for w in writes:
            w.w = {k: tok[2]}
            w.sw = {}
            w.r = {}
        for w in ws:
            if w.sw.get(k, 0) < tok[2]:
                w.sw[k] = tok[2]

    def op(self, eng, fn, reads=(), writes=(), ws=()):
        waits = self._deps(eng, reads, writes, ws)
        self.cnt[eng] += 1
        tok = ("E", eng, self.cnt[eng])
        self.ops[eng].append((waits, fn, ("E", eng, 1), self.pred, self.cnt[eng] - 1))
        self._mark(tok, reads, writes, ws)
        return tok

    def dma(self, eng, fn, reads=(), writes=(), ws=(), final=False):
        s = self.dsem_next
        self.dsem_next = (s + 1) % self.n_dsem
        prev = self.dsem_use[s]
        extra = [("D", s, prev * 16)] if prev > 0 else []
        waits = self._deps(eng, reads, writes, ws, extra)
        self.dsem_use[s] += 1
        tok = ("D", s, self.dsem_use[s] * 16)
        self.ops[eng].append((waits, fn, ("D", s, 16), self.pred, prev * 16))
        self._mark(tok, reads, writes, ws)
        if final:
            self.final_toks.append(tok)
        return tok

    def emit(self):
        nc = self.nc
        esem = {e: nc.alloc_semaphore(name="es_" + e) for e in self.ENGS}
        dsem = [nc.alloc_semaphore(name="ds_%d" % i) for i in range(self.n_dsem)]
        fw = self._deps("sp", (), (), (), self.final_toks)

        def sem_of(kind, key):
            return esem[key] if kind == "E" else dsem[key]

        def run(eng_name, e):
            ops = self.ops[eng_name]
            reg = e.alloc_register("cnt_" + eng_name) if any(o[1].__class__ is tuple for o in ops) else None

            def emit_one(o):
                waits, fn, inc = o[0], o[1], o[2]
                for (kind, key), val in waits:
                    e.wait_ge(sem_of(kind, key), val)
                if fn.__class__ is tuple:
                    e.reg_load(reg, fn[1])
                    return
                ins = fn(e)
                ins.then_inc(sem_of(inc[0], inc[1]), inc[2])

            i = 0
            n = len(ops)
            while i < n:
                pred = ops[i][3]
                if pred is None:
                    emit_one(ops[i])
                    i += 1
                    continue
                j = i
                while j < n and ops[j][3] == pred:
                    j += 1
                with e.If_lt(reg, pred[1] * 512 + 1):
                    n_e_ops = 0
                    for o in ops[i:j]:
                        kind, key, v = o[2]
                        if kind == "E":
                            if n_e_ops == 0:
                                e.wait_ge(sem_of(kind, key), o[4])
                            n_e_ops += 1
                        else:
                            e.wait_ge(sem_of(kind, key), o[4])
                            e.sem_inc(sem_of(kind, key), v)
                    if n_e_ops:
                        e.sem_inc(esem[eng_name], n_e_ops)
                with e.Else():
                    for o in ops[i:j]:
                        emit_one(o)
                i = j
            if eng_name == "sp":
                for (kind, key), val in fw:
                    e.wait_ge(sem_of(kind, key), val)

        with nc.Block() as block:
            @block.tensor
            def _(e):
                run("pe", e)

            @block.scalar
            def _(e):
                run("act", e)

            @block.vector
            def _(e):
                run("dve", e)

            @block.gpsimd
            def _(e):
                run("pool", e)

            @block.sync
            def _(e):
                run("sp", e)


def _cmp_partial_tiles():
    idx = {}
    for i in range(NQB):
        for j in range(4):
            o = 4065 + 128 * i - 2048 * j
            if o + 127 < 0:
                continue
            if o < 2032:
                idx[(i, j)] = len(idx)
    return idx


CMP_PART = _cmp_partial_tiles()


def _bf(a):
    return np.ascontiguousarray(a.astype(ml_dtypes.bfloat16))


def _pack(tb, dtype):
    off = {}
    cols = []
    o = 0
    for k, v in tb.items():
        off[k] = (o, v.shape[1])
        o += v.shape[1]
        cols.append(v)
    arr = np.concatenate(cols, axis=1)
    if dtype == "bf16":
        return _bf(arr), off
    return np.ascontiguousarray(arr.astype(np.float32)), off


def make_tables(h):
    p = np.arange(128)
    tb = {}
    tb["identb"] = np.eye(128)
    tb["onesb"] = np.ones((128, 128))
    tb["tri"] = np.where(p[:, None] <= p[None, :], 0.0, NEG)
    tb["band"] = np.where(p[:, None] > p[None, :], 0.0, NEG)
    tb["cmask"] = (p[:, None] <= p[None, :]).astype(np.float64)
    tb["ltri"] = (p[:, None] < p[None, :]).astype(np.float64)
    tbs, tbs_off = _pack(tb, "bf16")

    ta = {}
    E = np.zeros((128, 64, 128))
    for kt in range(64):
        for key in range(128):
            E[2 * kt + key // 64, kt, key] = 1.0
    ta["E"] = E.reshape(128, 64 * 128)
    ov = np.zeros((128, 4, 128))
    for j in range(4):
        cs = 16 * (128 * j + p)
        for blk in range(128):
            ov[:, j, blk] = ((cs < 64 * blk + 64) & (cs + 32 > 64 * blk)).astype(np.float64)
    ta["ovl"] = ov.reshape(128, 512)
    cm = np.zeros((128, len(CMP_PART), 128))
    for (i, j), t in CMP_PART.items():
        c = 128 * j + p
        q = 4096 + 128 * i + p
        cm[:, t, :] = np.where((16 * c + 31)[:, None] <= q[None, :], 0.0, NEG)
    ta["cmpm"] = cm.reshape(128, -1)
    tba, tba_off = _pack(ta, "bf16")

    tf = {}
    tf["identf"] = np.eye(128)
    tf["triI"] = np.where(p[:, None] <= p[None, :], -1.0 / 16, 0.0)
    tf["triR"] = np.where(p[:, None] > p[None, :], -1.0 / 16, 0.0)
    tf["onesf"] = np.ones((128, 128))
    r = np.arange(256) - 128
    curq = (p >= 64).astype(np.int64)
    future = r[None, :] > curq[:, None]
    forced = (r[None, :] <= curq[:, None]) & (r[None, :] > curq[:, None] - 2)
    tf["tblA"] = np.where(future | forced, 0.0, 1.0)
    tf["tblB"] = np.where(forced, 1e6, np.where(future, -1.0, 0.0))
    blk0 = 64 * (1 - h)
    f0 = np.full((128, 128), -1e9)
    f0[:, blk0] = 1e6
    tf["F0"] = f0
    tf["iotaE"] = np.tile(np.arange(32)[None, :], (128, 1)).astype(np.float64)
    tf["iotaEC1"] = tf["iotaE"] * CAP + 1.0
    tf["tokid"] = (np.arange(32)[None, :] * 128 + p[:, None]).astype(np.float64)
    tf["flag"] = np.full((128, 1), float(h))
    tf128, tf_off = _pack(tf, "f32")

    pos = np.arange(T)
    invalid = (pos < 4096).astype(np.float64) * (1 - h)
    posrows = np.stack([np.ones(T), pos // 64, pos % 64, invalid, np.ones(T)])
    c = np.arange(512)
    m2 = 32 * c + 31
    inv_c = ((c < 256).astype(np.float64) * (1 - h))
    inv_c[511] = 1.0
    posrows_c = np.stack([np.ones(512), m2 // 64, m2 % 64, inv_c, np.ones(512)])
    slopes = 2.0 ** (-(np.arange(8) + 1.0))
    src = np.zeros((5, NQB, 8))
    srcc = np.zeros((5, NQB, 8))
    for i in range(NQB):
        a_i = 64 + 2 * i
        src[1, i] = 64 * slopes
        src[2, i] = slopes
        src[3, i] = NEG
        src[4, i] = -64 * slopes * a_i
        srcc[1, i] = 32 * slopes
        srcc[2, i] = slopes / 2
        srcc[3, i] = NEG
        srcc[4, i] = -64 * slopes * a_i
    qrow = np.zeros((5, 8, 128))
    qrow[0] = -slopes[:, None] * np.arange(128)[None, :]
    t5 = {"posrows": posrows, "posrows_c": posrows_c, "src": src.reshape(5, -1),
          "srcc": srcc.reshape(5, -1), "qrow": qrow.reshape(5, -1)}
    for k, v in t5.items():
        ok = (v == NEG) | (v.astype(ml_dtypes.bfloat16).astype(np.float64) == v)
        assert ok.all(), k
    tb5, t5_off = _pack(t5, "bf16")
    return dict(tbs=tbs, tbs_off=tbs_off, tba=tba, tba_off=tba_off, tf=tf128, tf_off=tf_off,
                tb5=tb5, t5_off=t5_off)


_TBL_CACHE = {}


def tables(h):
    if h not in _TBL_CACHE:
        _TBL_CACHE[h] = make_tables(h)
    return _TBL_CACHE[h]


def build(debug=False, stages=99):
    nc = bass.Bass("TRN2", target_bir_lowering=False)
    P = Prog(nc)
    TBL = tables(0)
    tbo, tao, tfo, t5o = TBL["tbs_off"], TBL["tba_off"], TBL["tf_off"], TBL["t5_off"]

    def din(name, shape, dt=F32):
        return nc.dram_tensor(name, list(shape), dt, kind="ExternalInput").ap()

    skind = "ExternalOutput" if debug else "Internal"

    def dscr(name, shape, dt):
        return nc.dram_tensor(name, list(shape), dt, kind=skind).ap()

    def sb(name, shape, dt=F32):
        return nc.alloc_sbuf_tensor(name, list(shape), dt)

    ARENA = 196 * 1024
    arena = sb("arena", [128, ARENA // 2], BF16)
    ast = {"off": 0}
    DSZ = {F32: 4, BF16: 2, I32: 4, U32: 4}

    def areset():
        P.barrier()
        ast["off"] = 0

    def al(name, shape, dt=F32):
        shape = list(shape)
        nfree = 1
        for d_ in shape[1:]:
            nfree *= d_
        nbytes = (nfree * DSZ[dt] + 31) // 32 * 32
        o = ast["off"]
        assert o + nbytes <= ARENA, (name, o, nbytes)
        ast["off"] = o + nbytes
        v = arena[0:shape[0], o // 2:(o + nbytes) // 2]
        if dt != BF16:
            v = v.bitcast(dt)
        v = v[:, 0:nfree]
        if len(shape) == 3:
            v = v.rearrange("p (a b) -> p a b", a=shape[1])
        elif len(shape) == 4:
            v = v.rearrange("p (a b c) -> p a b c", a=shape[1], b=shape[2])
        return v

    RS = {}

    def R(name):
        if name not in RS:
            RS[name] = Res(name)
        return RS[name]

    def mm(out, lhsT, rhs, start, stop, reads, writes, ws=()):
        P.op("pe", lambda e: e.matmul(out, lhsT, rhs, start=start, stop=stop), reads, writes, ws)

    def trp(out, in_, ident, reads, writes, ws=()):
        P.op("pe", lambda e: e.transpose(out, in_, ident), reads, writes, ws)

    def act(out, in_, func, reads, writes, bias=None, scale=None, ws=()):
        kw = {}
        if bias is not None:
            kw["bias"] = bias
        if scale is not None:
            kw["scale"] = scale
        P.op("act", lambda e: e.activation(out=out, in_=in_, func=func, **kw), reads, writes, ws)

    def tt(eng, out, in0, in1, op, reads, writes, ws=()):
        P.op(eng, lambda e: e.tensor_tensor(out=out, in0=in0, in1=in1, op=op), reads, writes, ws)

    def ts(eng, out, in0, s1, s2, op0, op1, reads, writes, ws=()):
        if op1 is None:
            P.op(eng, lambda e: e.tensor_scalar(out=out, in0=in0, scalar1=s1, scalar2=None, op0=op0),
                 reads, writes, ws)
        else:
            P.op(eng, lambda e: e.tensor_scalar(out=out, in0=in0, scalar1=s1, scalar2=s2, op0=op0, op1=op1),
                 reads, writes, ws)

    def stt(eng, out, in0, scalar, in1, op0, op1, reads, writes, ws=()):
        P.op(eng, lambda e: e.scalar_tensor_tensor(out=out, in0=in0, scalar=scalar, in1=in1, op0=op0, op1=op1),
             reads, writes, ws)

    def cp(eng, out, in_, reads, writes, ws=()):
        if eng == "act":
            P.op("act", lambda e: e.activation(out=out, in_=in_, func=AF.Copy), reads, writes, ws)
        else:
            P.op(eng, lambda e: e.tensor_copy(out, in_), reads, writes, ws)

    def recip(out, in_, reads, writes, ws=()):
        P.op("dve", lambda e: e.reciprocal(out, in_), reads, writes, ws)

    def rsum(out, in_, reads, writes):
        P.op("dve", lambda e: e.reduce_sum(out, in_, AX.X), reads, writes)

    def max8(out, in_, reads, writes):
        P.op("dve", lambda e: e.max(out=out, in_=in_), reads, writes)

    def mrepl(out, m8, in_, val, reads, writes):
        P.op("dve", lambda e: e.match_replace(out=out, in_to_replace=m8, in_values=in_, imm_value=val), reads, writes)

    def memset(eng, ap, val, writes, ws=()):
        P.op(eng, lambda e: e.memset(ap, val), (), writes, ws)

    def dma(eng, out, in_, reads, writes, ws=(), final=False):
        P.dma(eng, lambda e: e.dma_start(out=out, in_=in_), reads, writes, ws, final=final)

    def gather(out, src, idx_ap, reads, writes, ws=()):
        P.dma("pool", lambda e: e.indirect_dma_start(
            out=out, out_offset=None, in_=src,
            in_offset=bass.IndirectOffsetOnAxis(ap=idx_ap, axis=0)), reads, writes, ws)

    def scatter(dst, idx_ap, in_, reads, writes, ws=()):
        P.dma("pool", lambda e: e.indirect_dma_start(
            out=dst, out_offset=bass.IndirectOffsetOnAxis(ap=idx_ap, axis=0),
            in_=in_, in_offset=None), reads, writes, ws)

    x_ctx = din("x_ctx", [T, D])
    c_col = din("c_col", [128, 16])
    w_ada = din("w_ada", [D, 6 * D])
    b_ada_col = din("b_ada_col", [128, 96])
    b_ada_row = din("b_ada_row", [1, 6 * D])
    w_in = din("w_in", [D, IN_W])
    cmp_in = {}
    for kv in ("k", "v"):
        cmp_in[kv] = dict(pe=din("cmp_pe_" + kv, [32, 128]), w1=din("cmp_w1_" + kv, [4096, 256]),
                          b1=din("cmp_b1c_" + kv, [128, 2]), w2=din("cmp_w2_" + kv, [256, 128]))
    gla_w_gate = din("gla_w_gate", [16, 512])
    gla_b_gate = din("gla_b_gate", [1, 512])
    gla_nw_col = din("gla_nw_col", [128, 2])
    w_out = din("w_out", [D, D])
    ln1_g = din("ln1_g", [1, D])
    ln1_b = din("ln1_b", [1, D])
    w_router = din("w_router", [D, NE])
    b_router = din("b_router", [1, NE])
    w_gate_up = din("w_gate_up", [NE, D, 2 * D])
    b_gu_col = din("b_gu_col", [128, NE * 32])
    w_down = din("w_down", [NE, D, D])
    b_down = din("b_down", [NE, D])
    ln2_g = din("ln2_g", [1, D])
    ln2_b = din("ln2_b", [1, D])
    tbs_d = din("tbs", TBL["tbs"].shape, BF16)
    tba_d = din("tba", TBL["tba"].shape, BF16)
    tf_d = din("tf", TBL["tf"].shape, F32)
    tb5_d = din("tb5", TBL["tb5"].shape, BF16)
    out_d = nc.dram_tensor("out", [TO, D], F32, kind="ExternalOutput").ap()

    modrow_d = dscr("modrow_d", [4, D], F32)
    qT_d = dscr("qT_d", [1024, TO], BF16)
    kcmpT_d = dscr("kcmpT_d", [256, T], BF16)
    vcmpT_d = dscr("vcmpT_d", [256, T], BF16)
    kselT_d = dscr("kselT_d", [256, T], BF16)
    vsel_d = dscr("vsel_d", [T, 256], BF16)
    kwinT_d = dscr("kwinT_d", [256, T], BF16)
    vwin_d = dscr("vwin_d", [T, 256], BF16)
    gT_d = dscr("gT_d", [24, TO], F32)
    qglaT_d = dscr("qglaT_d", [512, TO], BF16)
    kglaT_d = dscr("kglaT_d", [512, T], BF16)
    kgla_d = dscr("kgla_d", [T, 512], BF16)
    vgla_d = dscr("vgla_d", [T, 1024], BF16)
    aT_d = dscr("aT_d", [16, T], F32)
    rT_d = dscr("rT_d", [1024, TO], BF16)
    yT_d = dscr("yT_d", [D, TO], BF16)
    x1_d = dscr("x1_d", [TO, D], F32)
    h2_d = dscr("h2_d", [TO, D], BF16)
    slotinfo_d = dscr("slotinfo_d", [NE * CAP, 2], F32)
    counts_d = dscr("counts_d", [1, NE], I32)
    ybuf_d = [nc.dram_tensor("ybuf_d%d" % q_, [NE * CAP, 512], F32, kind="Internal").ap() for q_ in range(4)]

    tbs = sb("tbs_s", TBL["tbs"].shape, BF16)
    tf128 = sb("tf_s", TBL["tf"].shape, F32)
    sc1p = sb("sc1p", [128, 32])
    sc1v = sb("sc1v", [128, 32])
    kcT = sb("kcT", [128, 2, 512], BF16)
    vcs = sb("vcs", [128, 4, 2, 128], BF16)
    slot_i = sb("slot_i", [128, NQB, 4], I32)

    def TB(name, a=None, b=None):
        o, n = tbo[name]
        return tbs[:, o:o + n] if a is None else tbs[:, o + a:o + b]

    def TF(name, a=None, b=None):
        o, n = tfo[name]
        return tf128[:, o:o + n] if a is None else tf128[:, o + a:o + b]

    ps = [nc.alloc_psum_tensor("ps%d" % i, [128, 512], F32) for i in range(8)]
    PSR = [R("ps%d" % i) for i in range(8)]
    psb = [p_[:].bitcast(BF16) for p_ in ps]

    def ps3(b_):
        return ps[b_][:].rearrange("p (h q) -> p h q", h=4)

    dma("sp", tbs[:], tbs_d[:, :], [], [R("tbs")])
    dma("sp", tf128[:], tf_d[:, :], [], [R("tf")])
    identf = TF("identf")
    identb = TB("identb")
    onesb = TB("onesb")
    flagc = TF("flag")
    RTB, RTF = R("tbs"), R("tf")

    def bc4(ap2d):
        return ap2d.unsqueeze(1).to_broadcast([128, 4, 128])

    cc = al("cc", [128, 16])
    scc = al("scc", [128, 16])
    screp = al("screp", [128, 16, 128])
    bcol = al("bcol", [128, 96])
    modcol = al("modcol", [128, 32])
    modrep = al("modrep", [128, 4, D])
    wa = [al("wa%d" % i, [128, 16, 512]) for i in range(2)]
    brow_ = [al("brow%d" % i, [128, 512]) for i in range(2)]
    dma("sp", cc, c_col[:, :], [], [R("cc")])
    dma("sp", bcol, b_ada_col[:, :], [], [R("bcol")])
    act(scc, cc, AF.Silu, [R("cc")], [R("scc")])
    cp("dve", screp, scc.unsqueeze(2).to_broadcast([128, 16, 128]), [R("scc")], [R("screp")])
    for blk in range(24):
        w = wa[blk % 2]
        wr = R("wa%d" % (blk % 2))
        dma("sp", w, w_ada[:, blk * 512:(blk + 1) * 512].rearrange("(dc p) n -> p dc n", p=128), [], [wr])
        if blk < 8:
            for fc in range(4):
                col = blk * 4 + fc
                for dc in range(16):
                    mm(ps[0][:, col:col + 1], w[:, dc, fc * 128:(fc + 1) * 128], scc[:, dc:dc + 1],
                       dc == 0, dc == 15, [wr, R("scc")], [PSR[0]])
        else:
            br = brow_[blk % 2]
            brr = R("brow%d" % (blk % 2))
            dma("sp", br, b_ada_row[0:1, blk * 512:(blk + 1) * 512].partition_broadcast(128), [], [brr])
            pb = 1 + blk % 2
            for dc in range(16):
                mm(ps[pb][:], screp[:, dc, :], w[:, dc, :], dc == 0, dc == 15, [wr, R("screp")], [PSR[pb]])
            seg = (blk - 8) // 4
            off = ((blk - 8) % 4) * 512
            tt("dve", modrep[:, seg, off:off + 512], ps[pb][:], br, ALU.add, [PSR[pb], brr], [], ws=[R("modrep")])
    tt("dve", modcol, ps[0][:, 0:32], bcol[:, 0:32], ALU.add, [PSR[0], R("bcol")], [R("modcol")])
    ts("dve", sc1p[:, 0:16], modcol[:, 16:32], 1.0, None, ALU.add, None, [R("modcol")], [R("sc1p")])
    cp("dve", sc1p[:, 16:32], modcol[:, 0:16], [R("modcol"), R("sc1p")], [R("sc1p")])
    ts("dve", sc1v[:], sc1p[:], flagc[:, 0:1], None, ALU.mult, None, [R("sc1p"), RTF], [R("sc1v")])
    for seg in range(4):
        dma("sp", modrow_d[seg:seg + 1, :], modrep[0:1, seg, :], [R("modrep")], [], ws=[R("modrow_d")])

    if stages >= 1:
        areset()
        xs = al("xs", [128, 4, D])
        hT = al("hT", [128, 16, 1024], BF16)
        wb = [al("wb%d" % i, [128, 16, 512], BF16) for i in range(2)]
        stg = [al("stg%d" % i, [128, 512], BF16) for i in range(4)]
        stgf = [al("stgf%d" % i, [128, 512], F32) for i in range(2)]
        SQ = 128 ** -0.5
        blocks = [
            (0, 512, [("F", 0, 512, qT_d, 0, "q")], True),
            (512, 512, [("F", 0, 512, qT_d, 512, "q")], True),
            (1024, 512, [("F", 0, 256, kcmpT_d, 0, "c"), ("F", 256, 256, vcmpT_d, 0, "c")], False),
            (1536, 512, [("F", 0, 256, kselT_d, 0, "c"), ("T", 256, 256, vsel_d, 0, "c")], False),
            (2048, 512, [("F", 0, 256, kwinT_d, 0, "c"), ("T", 256, 256, vwin_d, 0, "c")], False),
            (2560, 24, [("F", 0, 24, gT_d, 0, "sig")], True),
            (2584, 512, [("F", 0, 512, qglaT_d, 0, "q")], True),
            (3096, 512, [("F", 0, 512, kglaT_d, 0, "c"), ("T", 0, 512, kgla_d, 0, "c")], False),
            (3608, 512, [("T", 0, 512, vgla_d, 0, "c")], False),
            (4120, 512, [("T", 0, 512, vgla_d, 512, "c")], False),
            (4632, 16, [("F", 0, 16, aT_d, 0, "f32")], False),
            (4648, 512, [("F", 0, 512, rT_d, 0, "silu")], True),
            (5160, 512, [("F", 0, 512, rT_d, 512, "silu")], True),
        ]
        wcnt = 0
        pcnt = 0
        scnt = 0
        for g in range(8):
            own = g >= 4
            scl = sc1p if own else sc1v
            sclr = R("sc1p") if own else R("sc1v")
            for hh in range(2):
                for tt_ in range(4):
                    r0 = g * 1024 + hh * 512 + tt_ * 128
                    dma("sp", xs[:, tt_, :], x_ctx[r0:r0 + 128, :], [], [R("xs%d" % tt_)])
                for dc in range(16):
                    pb = pcnt % 4
                    pcnt += 1
                    for tt_ in range(4):
                        trp(ps[pb][:, tt_ * 128:(tt_ + 1) * 128], xs[:, tt_, dc * 128:(dc + 1) * 128], identf,
                            [R("xs%d" % tt_), RTF], [PSR[pb]])
                    act(hT[:, dc, hh * 512:(hh + 1) * 512], ps[pb][:], AF.Identity, [PSR[pb], sclr], [],
                        bias=scl[:, 16 + dc:17 + dc], scale=scl[:, dc:dc + 1], ws=[R("hT")])
            for (c0, ncol, subs, own_only) in blocks:
                if own_only and not own:
                    continue
                w = wb[wcnt % 2]
                wr = R("wb%d" % (wcnt % 2))
                wcnt += 1
                dma("pool", w[:, :, 0:ncol], w_in[:, c0:c0 + ncol].rearrange("(dc p) n -> p dc n", p=128), [], [wr])
                for (mode, so, sn, dst, doff, ev) in subs:
                    if mode == "F":
                        for fc in range((sn + 127) // 128):
                            nf = min(128, sn - fc * 128)
                            for hh in range(2):
                                pb = 4 + pcnt % 4
                                pcnt += 1
                                for dc in range(16):
                                    mm(ps[pb][0:nf, :], w[:, dc, so + fc * 128:so + fc * 128 + nf],
                                       hT[:, dc, hh * 512:(hh + 1) * 512], dc == 0, dc == 15, [wr, R("hT")], [PSR[pb]])
                                tcol = (g * 1024 + hh * 512) - (TO if own_only else 0)
                                frow = doff + fc * 128
                                if ev in ("sig", "f32"):
                                    s_ = stgf[scnt % 2]
                                    sr = R("stgf%d" % (scnt % 2))
                                else:
                                    s_ = stg[scnt % 4]
                                    sr = R("stg%d" % (scnt % 4))
                                scnt += 1
                                if ev == "q":
                                    act(s_[0:nf, :], ps[pb][0:nf, :], AF.Identity, [PSR[pb]], [sr], scale=SQ)
                                elif ev == "sig":
                                    act(s_[0:nf, :], ps[pb][0:nf, :], AF.Sigmoid, [PSR[pb]], [sr])
                                elif ev == "silu":
                                    act(s_[0:nf, :], ps[pb][0:nf, :], AF.Silu, [PSR[pb]], [sr])
                                elif scnt % 2 == 0:
                                    act(s_[0:nf, :], ps[pb][0:nf, :], AF.Copy, [PSR[pb]], [sr])
                                else:
                                    cp("dve", s_[0:nf, :], ps[pb][0:nf, :], [PSR[pb]], [sr])
                                dma("sp", dst[frow:frow + nf, tcol:tcol + 512], s_[0:nf, :], [sr], [],
                                    ws=[R(dst.name)])
                    else:
                        for tt_ in range(8):
                            pb = 4 + pcnt % 4
                            pcnt += 1
                            for dc in range(16):
                                mm(ps[pb][:, 0:sn], hT[:, dc, tt_ * 128:(tt_ + 1) * 128], w[:, dc, so:so + sn],
                                   dc == 0, dc == 15, [wr, R("hT")], [PSR[pb]])
                            s_ = stg[scnt % 4]
                            sr = R("stg%d" % (scnt % 4))
                            scnt += 1
                            if scnt % 2 == 0:
                                act(s_[:, 0:sn], ps[pb][:, 0:sn], AF.Copy, [PSR[pb]], [sr])
                            else:
                                cp("dve", s_[:, 0:sn], ps[pb][:, 0:sn], [PSR[pb]], [sr])
                            t0 = g * 1024 + tt_ * 128
                            dma("sp", dst[t0:t0 + 128, doff:doff + sn], s_[:, 0:sn], [sr], [], ws=[R(dst.name)])

    if stages >= 2:
        areset()
        kTc = al("kTc", [128, 2, T], BF16)
        w1 = al("w1", [128, 32, 256], BF16)
        pes = al("pes", [32, 128])
        peT = al("peT", [128, 32], BF16)
        b1c = al("b1c", [128, 2])
        w2 = al("w2", [128, 2, 128], BF16)
        biasc = al("biasc", [128, 2])
        u = [al("u%d" % i, [128, 512]) for i in range(2)]
        u2 = [al("u2%d" % i, [128, 512]) for i in range(2)]
        gel = al("gel", [128, 2, 512], BF16)
        memset("pool", gel[:, :, 511:512], 0.0, [R("gel")])
        memset("pool", kcT[:, :, 511:512], 0.0, [R("kcT")])
        for kv in ("k", "v"):
            ci = cmp_in[kv]
            src_d = kcmpT_d if kv == "k" else vcmpT_d
            dma("sp", kTc, src_d.rearrange("(g p) t -> p g t", p=128), [R(src_d.name)], [R("kTc")])
            dma("pool", w1, ci["w1"].rearrange("(l p) n -> p l n", p=128), [], [R("w1")])
            dma("pool", w2, ci["w2"].rearrange("(c p) n -> p c n", p=128), [], [R("w2")])
            dma("sp", pes, ci["pe"][:, :], [], [R("pes")])
            dma("sp", b1c, ci["b1"][:, :], [], [R("b1c")])
            trp(ps[0][:, 0:32], pes[0:32, :], identf[0:32, 0:32], [R("pes"), RTF], [PSR[0]])
            cp("dve", peT, ps[0][:, 0:32], [PSR[0]], [R("peT")])
            for hc in range(2):
                for l in range(32):
                    mm(ps[1][:, hc:hc + 1], w1[:, l, hc * 128:(hc + 1) * 128], peT[:, l:l + 1],
                       l == 0, l == 31, [R("w1"), R("peT")], [PSR[1]])
            tt("dve", biasc, ps[1][:, 0:2], b1c, ALU.add, [PSR[1], R("b1c")], [R("biasc")])
            for g in range(2):
                for hc in range(2):
                    pb = 2 + hc
                    for l in range(32):
                        mm(ps[pb][:, 0:511], w1[:, l, hc * 128:(hc + 1) * 128], kTc[:, g, l:l + 16 * 510 + 1:16],
                           l == 0, l == 31, [R("w1"), R("kTc")], [PSR[pb]])
                    uu, uu2 = u[hc], u2[hc]
                    ru, ru2 = R("u%d" % hc), R("u2%d" % hc)
                    act(uu[:, 0:511], ps[pb][:, 0:511], AF.Identity, [PSR[pb], R("biasc")], [ru],
                        bias=biasc[:, hc:hc + 1])
                    tt("dve", uu2[:, 0:511], uu[:, 0:511], uu[:, 0:511], ALU.mult, [ru], [ru2])
                    tt("dve", uu2[:, 0:511], uu2[:, 0:511], uu[:, 0:511], ALU.mult, [ru, ru2], [ru2])
                    stt("dve", uu2[:, 0:511], uu2[:, 0:511], 0.044715, uu[:, 0:511], ALU.mult, ALU.add, [ru, ru2], [ru2])
                    act(uu2[:, 0:511], uu2[:, 0:511], AF.Tanh, [ru2], [ru2], scale=0.7978845608028654)
                    ts("dve", uu2[:, 0:511], uu2[:, 0:511], 1.0, 0.5, ALU.add, ALU.mult, [ru2], [ru2])
                    tt("dve", gel[:, hc, 0:511], uu2[:, 0:511], uu[:, 0:511], ALU.mult, [ru, ru2], [], ws=[R("gel")])
                if kv == "k":
                    for hc in range(2):
                        mm(ps[4][:, 0:511], w2[:, hc, :], gel[:, hc, 0:511], hc == 0, hc == 1,
                           [R("w2"), R("gel")], [PSR[4]])
                    cp("act", kcT[:, g, 0:511], ps[4][:, 0:511], [PSR[4]], [], ws=[R("kcT")])
                else:
                    for ct in range(4):
                        for hc in range(2):
                            mm(ps[4][:, ct * 128:(ct + 1) * 128], gel[:, hc, ct * 128:(ct + 1) * 128], w2[:, hc, :],
                               hc == 0, hc == 1, [R("w2"), R("gel")], [PSR[4]])
                    cp("act", vcs[:, :, g, :], ps[4][:].rearrange("p (c d) -> p c d", c=4), [PSR[4]], [],
                       ws=[R("vcs")])
                memset("pool", gel[:, :, 511:512], 0.0, [R("gel")])

    if stages >= 3:
        areset()
        kselT = al("kselT", [128, 2, T], BF16)
        vsel = al("vsel", [128, 64, 256], BF16)
        tba = al("tba", TBL["tba"].shape, BF16)
        tb5 = al("tb5", TBL["tb5"].shape, BF16)
        dma("sp", kselT, kselT_d.rearrange("(g p) t -> p g t", p=128), [R("kselT_d")], [R("kselT")])
        dma("sp", vsel, vsel_d.rearrange("(kt p) c -> p kt c", p=128), [R("vsel_d")], [R("vsel")])
        dma("sp", tba, tba_d[:, :], [], [R("tba")])
        dma("sp", tb5, tb5_d[:, :], [], [R("tb5")])
        RKS, RVS, RTA, RT5 = R("kselT"), R("vsel"), R("tba"), R("tb5")

        def TA(name, a, b):
            o, n = tao[name]
            return tba[:, o + a:o + b]

        def T5(name, a, b, rows=5):
            o, n = t5o[name]
            return tb5[0:rows, o + a:o + b]

        qTb = [al("qTb%d" % i, [128, 8, 128], BF16) for i in range(2)]
        kwT = [al("kwT%d" % i, [128, 2, 640], BF16) for i in range(2)]
        vw = [al("vw%d" % i, [128, 5, 256], BF16) for i in range(2)]
        grep = [al("grep%d" % i, [128, 12, 128]) for i in range(2)]
        PTc = [al("PTc%d" % i, [128, 512], BF16) for i in range(4)]
        Pn = [al("Pn%d" % i, [128, 512], BF16) for i in range(4)]
        PTr = [al("PTr%d" % i, [128, 512], BF16) for i in range(3)]
        browS = [al("browS%d" % i, [5, 512], BF16) for i in range(2)]
        browC = [al("browC%d" % i, [5, 512], BF16) for i in range(2)]
        Rz = [al("Rz%d" % i, [128, 512]) for i in range(2)]
        Rg = [al("Rg%d" % i, [128, 512]) for i in range(2)]
        tmpo = [al("tmpo%d" % i, [128, 512]) for i in range(2)]
        acc = [al("acc%d" % i, [128, 512]) for i in range(2)]
        accb = [al("accb%d" % i, [128, 512], BF16) for i in range(2)]
        iw = al("iw", [128, 128])
        iw2 = al("iw2", [128, 128])
        m8a = al("m8a", [128, 8])
        m8b = al("m8b", [128, 8])
        mb = al("mb", [128, 128], BF16)
        mbT = [al("mbT%d" % i, [128, 128], BF16) for i in range(2)]
        cnt = {"pt": 0, "s": 0, "oz": 0, "ep": 0}

        pend = {"f": None}

        def flush_pv():
            if pend["f"] is not None:
                pend["f"]()
                pend["f"] = None

        def score_and_pv(mms, PT, PTres, vl, vreads, ob, zb, first, last):
            sbk = cnt["s"] % 2
            cnt["s"] += 1
            for n_, (l_, r_, rd_) in enumerate(mms):
                mm(ps3(sbk), l_, r_, n_ == 0, n_ == len(mms) - 1, rd_, [PSR[sbk]])
            act(PT, ps[sbk][:], AF.Exp, [PSR[sbk]], [PTres])
            flush_pv()

            def pv():
                mm(ps[ob][:], vl, PT, first, last, vreads + [PTres], [PSR[ob]])
                mm(ps[zb][:], onesb, PT, first, last, [RTB, PTres], [PSR[zb]])
            pend["f"] = pv

        def epilogue(br, ob, zb, grp, grpr, ac, acr, first_branch, cmp_pts=None):
            k = cnt["ep"] % 2
            cnt["ep"] += 1
            rz, rg, tm = Rz[k], Rg[k], tmpo[k]
            rzr, rgr, tmr = R("Rz%d" % k), R("Rg%d" % k), R("tmpo%d" % k)
            ts("dve", rz, ps[zb][:], 1e-30, None, ALU.add, None, [PSR[zb]], [rzr])
            recip(rz, rz, [rzr], [rzr])
            if cmp_pts is not None:
                for (j, ptc, ptr, pn, pnr) in cmp_pts:
                    tt("pool", pn, ptc, rz, ALU.mult, [ptr, rzr], [pnr])
            tt("pool", rg.rearrange("p (h q) -> p h q", h=4), rz.rearrange("p (h q) -> p h q", h=4),
               grp[:, br::3, :], ALU.mult, [rzr, grpr], [rgr])
            if first_branch:
                tt("dve", ac, ps[ob][:], rg, ALU.mult, [PSR[ob], rgr], [acr])
            else:
                tt("dve", tm, ps[ob][:], rg, ALU.mult, [PSR[ob], rgr], [tmr])
                tt("pool", ac, ac, tm, ALU.add, [tmr, acr], [acr])

        for i in range(NQB):
            q0 = 4096 + 128 * i
            oc = 128 * i
            b2 = i % 2
            qt, kw_, vw_ = qTb[b2], kwT[b2], vw[b2]
            rq, rkw, rvw = R("qTb%d" % b2), R("kwT%d" % b2), R("vw%d" % b2)
            dma("sp", qt, qT_d[:, oc:oc + 128].rearrange("(h p) q -> p h q", p=128), [R("qT_d")], [rq])
            dma("sp", kw_, kwinT_d[:, q0 - 512:q0 + 128].rearrange("(g p) t -> p g t", p=128), [R("kwinT_d")], [rkw])
            dma("sp", vw_, vwin_d[q0 - 512:q0 + 128, :].rearrange("(kt p) c -> p kt c", p=128), [R("vwin_d")], [rvw])
            for g in range(2):
                ig = 2 * i + g
                k2 = ig % 2
                gp, gpr = grep[k2], R("grep%d" % k2)
                dma("sp", gp, gT_d[12 * g:12 * g + 12, oc:oc + 128].partition_broadcast(128), [R("gT_d")], [gpr])
                bS, bSr = browS[k2], R("browS%d" % k2)
                bC, bCr = browC[k2], R("browC%d" % k2)
                cp("pool", bS.rearrange("p (h q) -> p h q", h=4),
                   T5("src", i * 8 + 4 * g, i * 8 + 4 * g + 4).unsqueeze(2).to_broadcast([5, 4, 128]), [RT5], [bSr])
                cp("pool", bS[0:1, :], T5("qrow", g * 512, (g + 1) * 512, rows=1), [RT5, bSr], [bSr])
                cp("pool", bC.rearrange("p (h q) -> p h q", h=4),
                   T5("srcc", i * 8 + 4 * g, i * 8 + 4 * g + 4).unsqueeze(2).to_broadcast([5, 4, 128]), [RT5], [bCr])
                cp("pool", bC[0:1, :], T5("qrow", g * 512, (g + 1) * 512, rows=1), [RT5, bCr], [bCr])
                bS3 = bS.rearrange("p (h q) -> p h q", h=4)
                bC3 = bC.rearrange("p (h q) -> p h q", h=4)
                rhsQ = qt[:, 4 * g:4 * g + 4, :]
                ac, acr = acc[k2], R("acc%d" % k2)
                ob, zb = (2, 3) if cnt["oz"] % 2 == 0 else (4, 5)
                cnt["oz"] += 1
                js = [j for j in range(4) if 4065 + 128 * i - 2048 * j + 127 >= 0]
                cmp_pts = []
                for n_, j in enumerate(js):
                    mms = [(kcT[:, g, j * 128:(j + 1) * 128], rhsQ, [R("kcT"), rq]),
                           (T5("posrows_c", j * 128, (j + 1) * 128), bC3, [RT5, bCr])]
                    if (i, j) in CMP_PART:
                        t_ = CMP_PART[(i, j)]
                        mms.append((identb, bc4(TA("cmpm", t_ * 128, (t_ + 1) * 128)), [RTB, RTA]))
                    score_and_pv(mms, PTc[j], R("PTc%d" % j), vcs[:, j, g, :], [R("vcs")], ob, zb,
                                 n_ == 0, n_ == len(js) - 1)
                    cmp_pts.append((j, PTc[j], R("PTc%d" % j), Pn[j], R("Pn%d" % j)))
                flush_pv()
                epilogue(0, ob, zb, gp, gpr, ac, acr, True, cmp_pts)
                nmm = 4 * len(js)
                c_ = 0
                for hh in range(4):
                    for j in js:
                        mm(ps[6][:, 0:128], Pn[j][:, hh * 128:(hh + 1) * 128], TA("ovl", j * 128, (j + 1) * 128),
                           c_ == 0, c_ == nmm - 1, [R("Pn%d" % j), RTA], [PSR[6]])
                        c_ += 1
                tt("dve", iw, ps[6][:, 0:128], TF("tblA", 64 - 2 * i, 192 - 2 * i), ALU.mult, [PSR[6], RTF], [R("iw")])
                tt("dve", iw, iw, TF("tblB", 64 - 2 * i, 192 - 2 * i), ALU.add, [R("iw"), RTF], [R("iw")])
                tt("dve", iw, iw, TF("F0"), ALU.max, [R("iw"), RTF], [R("iw")])
                max8(m8a, iw, [R("iw")], [R("m8a")])
                mrepl(iw2, m8a, iw, -3e9, [R("iw"), R("m8a")], [R("iw2")])
                max8(m8b, iw2, [R("iw2")], [R("m8b")])
                ts("dve", mb, iw, m8b[:, 7:8], NEG, ALU.is_lt, ALU.mult, [R("iw"), R("m8b")], [R("mb")])
                trp(psb[6][:, 512:640], mb, identb, [R("mb"), RTB], [PSR[6]])
                mt, mtr = mbT[k2], R("mbT%d" % k2)
                cp("act", mt, psb[6][:, 512:640], [PSR[6]], [mtr])
                ob, zb = (2, 3) if cnt["oz"] % 2 == 0 else (4, 5)
                cnt["oz"] += 1
                for t_ in range(5):
                    kt = 28 + i + t_
                    mms = [(kw_[:, g, t_ * 128:(t_ + 1) * 128], rhsQ, [rkw, rq]),
                           (T5("posrows", kt * 128, (kt + 1) * 128), bS3, [RT5, bSr])]
                    if t_ == 0:
                        mms.append((identb, bc4(TB("band")), [RTB]))
                    if t_ == 4:
                        mms.append((identb, bc4(TB("tri")), [RTB]))
                    k3 = cnt["pt"] % 3
                    cnt["pt"] += 1
                    score_and_pv(mms, PTr[k3], R("PTr%d" % k3), vw_[:, t_, g * 128:(g + 1) * 128], [rvw], ob, zb,
                                 t_ == 0, t_ == 4)
                flush_pv()
                epilogue(2, ob, zb, gp, gpr, ac, acr, False)
                ob, zb = (2, 3) if cnt["oz"] % 2 == 0 else (4, 5)
                cnt["oz"] += 1
                nkt = 33 + i
                for kt in range(nkt):
                    mms = [(kselT[:, g, kt * 128:(kt + 1) * 128], rhsQ, [RKS, rq]),
                           (T5("posrows", kt * 128, (kt + 1) * 128), bS3, [RT5, bSr]),
                           (TA("E", kt * 128, (kt + 1) * 128), bc4(mt), [RTA, mtr])]
                    if kt == nkt - 1:
                        mms.append((identb, bc4(TB("tri")), [RTB]))
                    k3 = cnt["pt"] % 3
                    cnt["pt"] += 1
                    score_and_pv(mms, PTr[k3], R("PTr%d" % k3), vsel[:, kt, g * 128:(g + 1) * 128], [RVS], ob, zb,
                                 kt == 0, kt == nkt - 1)
                flush_pv()
                epilogue(1, ob, zb, gp, gpr, ac, acr, False)
                ab, abr = accb[k2], R("accb%d" % k2)
                cp("act", ab, ac, [acr], [abr])
                dma("sp", yT_d[g * 512:(g + 1) * 512, oc:oc + 128].rearrange("(h p) q -> p h q", p=128),
                    ab.rearrange("p (h q) -> p h q", h=4), [abr], [], ws=[R("yT_d")])

    if stages >= 4:
        areset()
        wg = al("wg", [16, 512])
        bg = al("bg", [1, 512])
        nwc = al("nwc", [128, 2])
        dma("sp", wg, gla_w_gate[:, :], [], [R("wg")])
        dma("sp", bg, gla_b_gate[:, :], [], [R("bg")])
        dma("sp", nwc, gla_nw_col[:, :], [], [R("nwc")])
        St = [al("St%d" % h_, [128, 256]) for h_ in range(4)]
        Sb = [al("Sb%d" % h_, [128, 256], BF16) for h_ in range(4)]
        for h_ in range(4):
            memset("pool", St[h_], 0.0, [R("St%d" % h_)])
            memset("pool", Sb[h_], 0.0, [R("Sb%d" % h_)])
        kT4 = [al("kT4%d" % i, [128, 4, 128], BF16) for i in range(2)]
        ktok = [al("ktok%d" % i, [128, 512], BF16) for i in range(2)]
        vtok = [al("vtok%d" % i, [128, 1024], BF16) for i in range(2)]
        aTs = [al("aTs%d" % i, [16, 128]) for i in range(2)]
        qT4 = [al("qT4%d" % i, [128, 4, 128], BF16) for i in range(2)]
        rT8 = [al("rT8%d" % i, [128, 8, 128], BF16) for i in range(2)]
        la0 = al("la0", [128, 512])
        la = al("la", [128, 512])
        ebT = al("ebT", [128, 512])
        enbT = al("enbT", [128, 512])
        erev = al("erev", [128, 512])
        ktil = al("ktil", [128, 4, 128], BF16)
        kk = al("kk", [128, 512], BF16)
        qtil = al("qtil", [128, 4, 128], BF16)
        PTg = al("PTg", [128, 4, 128], BF16)
        sqa = [al("sq%d" % i, [128, 512], BF16) for i in range(2)]
        rs = al("rs", [128, 512])
        ya = [al("ya%d" % i, [128, 512]) for i in range(2)]
        ystg = [al("ystg%d" % i, [128, 4, 128], BF16) for i in range(2)]
        onecol = TF("onesf", 0, 1)
        for ch in range(64):
            c0 = ch * 128
            own = ch >= 32
            oc = c0 - TO
            b2 = ch % 2
            k4, kt_, vt_, at_ = kT4[b2], ktok[b2], vtok[b2], aTs[b2]
            rk4, rkt, rvt, rat = R("kT4%d" % b2), R("ktok%d" % b2), R("vtok%d" % b2), R("aTs%d" % b2)
            dma("sp", k4, kglaT_d[:, c0:c0 + 128].rearrange("(h p) t -> p h t", p=128), [R("kglaT_d")], [rk4])
            dma("sp", kt_, kgla_d[c0:c0 + 128, :], [R("kgla_d")], [rkt])
            dma("sp", vt_, vgla_d[c0:c0 + 128, :], [R("vgla_d")], [rvt])
            dma("sp", at_, aT_d[:, c0:c0 + 128], [R("aT_d")], [rat])
            if own:
                q4, r8 = qT4[b2], rT8[b2]
                rq4, rr8 = R("qT4%d" % b2), R("rT8%d" % b2)
                dma("sp", q4, qglaT_d[:, oc:oc + 128].rearrange("(h p) t -> p h t", p=128), [R("qglaT_d")], [rq4])
                dma("sp", r8, rT_d[:, oc:oc + 128].rearrange("(j p) t -> p j t", p=128), [R("rT_d")], [rr8])
            mm(ps[0][:], at_[0:16, :], wg[0:16, :], True, False, [rat, R("wg")], [PSR[0]])
            mm(ps[0][:], TF("onesf")[0:1, 0:128], bg[0:1, :], False, True, [RTF, R("bg")], [PSR[0]])
            act(la0, ps[0][:], AF.Exp, [PSR[0]], [R("la0")], scale=-1.0)
            act(la, la0, AF.Ln, [R("la0"), RTF], [R("la")], bias=onecol)
            for h_ in range(4):
                mm(ps[1][:, h_ * 128:(h_ + 1) * 128], la[:, h_ * 128:(h_ + 1) * 128], TF("triI"), True, True,
                   [R("la"), RTF], [PSR[1]])
            mm(ps[2][:], TF("triR"), la, True, True, [R("la"), RTF], [PSR[2]])
            act(ebT, ps[1][:], AF.Exp, [PSR[1]], [R("ebT")])
            act(enbT, ps[1][:], AF.Exp, [PSR[1]], [R("enbT")], scale=-1.0)
            act(erev, ps[2][:], AF.Exp, [PSR[2]], [R("erev")])
            tt("dve", ktil.rearrange("p h t -> p (h t)"), k4.rearrange("p h t -> p (h t)"), enbT, ALU.mult,
               [rk4, R("enbT")], [R("ktil")])
            tt("pool", kk, kt_, erev, ALU.mult, [rkt, R("erev")], [R("kk")])
            if own:
                tt("dve", qtil.rearrange("p h t -> p (h t)"), q4.rearrange("p h t -> p (h t)"), ebT, ALU.mult,
                   [rq4, R("ebT")], [R("qtil")])
                for h_ in range(4):
                    mm(ps[3][:, h_ * 128:(h_ + 1) * 128], ktil[:, h_, :], qtil[:, h_, :], True, True,
                       [R("ktil"), R("qtil")], [PSR[3]])
                tt("dve", PTg, ps3(3), bc4(TB("cmask")), ALU.mult, [PSR[3], RTB], [R("PTg")])
                for dvc in range(2):
                    pb = 4 + dvc
                    for h_ in range(4):
                        mm(ps[pb][:, h_ * 128:(h_ + 1) * 128], vt_[:, h_ * 256 + dvc * 128:h_ * 256 + (dvc + 1) * 128],
                           PTg[:, h_, :], True, False, [rvt, R("PTg")], [PSR[pb]])
                        mm(ps[pb][:, h_ * 128:(h_ + 1) * 128], Sb[h_][:, dvc * 128:(dvc + 1) * 128], qtil[:, h_, :],
                           False, True, [R("Sb%d" % h_), R("qtil")], [PSR[pb]])
                    act(sqa[dvc], ps[pb][:], AF.Square, [PSR[pb]], [R("sq%d" % dvc)])
                mm(ps[6][:], onesb, sqa[0], True, False, [RTB, R("sq0")], [PSR[6]])
                mm(ps[6][:], onesb, sqa[1], False, True, [RTB, R("sq1")], [PSR[6]])
                ts("dve", rs, ps[6][:], 1.0 / 256, RMS_EPS, ALU.mult, ALU.add, [PSR[6]], [R("rs")])
                act(rs, rs, AF.Sqrt, [R("rs")], [R("rs")])
                recip(rs, rs, [R("rs")], [R("rs")])
                for dvc in range(2):
                    pb = 4 + dvc
                    y_, yr = ya[dvc], R("ya%d" % dvc)
                    tt("dve", y_, ps[pb][:], rs, ALU.mult, [PSR[pb], R("rs")], [yr])
                    ys, ysr = ystg[dvc], R("ystg%d" % dvc)
                    stt("dve", ys, y_.rearrange("p (h t) -> p h t", h=4), nwc[:, dvc:dvc + 1], r8[:, dvc::2, :],
                        ALU.mult, ALU.mult, [yr, R("nwc"), rr8], [ysr])
                    dst = yT_d[1024:2048, oc:oc + 128].rearrange("(h c p) t -> p h c t", c=2, p=128)[:, :, dvc, :]
                    dma("sp", dst, ys, [ysr], [], ws=[R("yT_d")])
            for hp in range(2):
                for hq in range(2):
                    h_ = 2 * hp + hq
                    mm(ps[7][:, hq * 256:(hq + 1) * 256], kk[:, h_ * 128:(h_ + 1) * 128], vt_[:, h_ * 256:(h_ + 1) * 256],
                       True, True, [R("kk"), rvt], [PSR[7]])
                for hq in range(2):
                    h_ = 2 * hp + hq
                    stt("dve", St[h_], St[h_], ebT[:, h_ * 128 + 127:h_ * 128 + 128], ps[7][:, hq * 256:(hq + 1) * 256],
                        ALU.mult, ALU.add, [R("ebT"), PSR[7], R("St%d" % h_)], [R("St%d" % h_)])
                    cp("act", Sb[h_], St[h_], [R("St%d" % h_)], [R("Sb%d" % h_)])

    if stages >= 5:
        areset()
        wo = al("wo", [128, 16, D], BF16)
        dma("pool", wo, w_out.rearrange("(c p) n -> p c n", p=128), [], [R("wo")])
        g1r = al("g1r", [128, D])
        l1g = al("l1g", [128, D])
        l1b = al("l1b", [128, D])
        sc2r = al("sc2r", [128, D])
        sh2r = al("sh2r", [128, D])
        dma("sp", g1r, modrow_d[0:1, :].partition_broadcast(128), [R("modrow_d")], [R("g1r")])
        dma("sp", sh2r, modrow_d[1:2, :].partition_broadcast(128), [R("modrow_d")], [R("sh2r")])
        dma("sp", sc2r, modrow_d[2:3, :].partition_broadcast(128), [R("modrow_d")], [R("sc2r")])
        dma("sp", l1g, ln1_g[0:1, :].partition_broadcast(128), [], [R("l1g")])
        dma("sp", l1b, ln1_b[0:1, :].partition_broadcast(128), [], [R("l1b")])
        ts("dve", sc2r, sc2r, 1.0, None, ALU.add, None, [R("sc2r")], [R("sc2r")])
        wrt = al("wrt", [128, 16, NE])
        brt = al("brt", [128, NE])
        dma("sp", wrt, w_router.rearrange("(c p) e -> p c e", p=128), [], [R("wrt")])
        dma("sp", brt, b_router[0:1, :].partition_broadcast(128), [], [R("brt")])
        yTt = [al("yTt%d" % i, [128, 16, 128], BF16) for i in range(2)]
        xt = [al("xt%d" % i, [128, D]) for i in range(2)]
        t1 = al("t1", [128, D])
        vv = al("vv", [128, D])
        sqv = al("sqv", [128, D])
        h2f = al("h2f", [128, D])
        h2b = al("h2b", [128, D], BF16)
        h2T = al("h2T", [128, 16, 128])
        st = al("st", [128, 16])
        lg = al("lg", [128, NE])
        m8r = al("m8r", [128, 8])
        w4 = al("w4", [128, 8])
        maskf = al("maskf", [128, NE])
        maskb = al("maskb", [128, NE], BF16)
        csum = al("csum", [128, NE])
        posv = al("posv", [128, NE])
        oh = al("oh", [128, NE])
        slf = al("slf", [128, 4])
        sinfo = [al("sinfo%d" % k, [128, 2]) for k in range(4)]
        zt = al("zt", [128, 1024])
        memset("pool", zt, 0.0, [R("zt")])
        memset("pool", csum, 0.0, [R("csum")])
        dma("sp", slotinfo_d.rearrange("(p a) c -> p (a c)", p=128), zt, [R("zt")], [R("slotinfo_d")])

        def layer_norm(src, srcr, dst, dstr, gam, gamr, bet, betr):
            rsum(st[:, 0:1], src, [srcr], [R("st")])
            tt("pool", sqv, src, src, ALU.mult, [srcr], [R("sqv")])
            rsum(st[:, 1:2], sqv, [R("sqv"), R("st")], [R("st")])
            ts("dve", st[:, 2:3], st[:, 0:1], 1.0 / D, None, ALU.mult, None, [R("st")], [R("st")])
            tt("dve", st[:, 3:4], st[:, 2:3], st[:, 2:3], ALU.mult, [R("st")], [R("st")])
            stt("dve", st[:, 4:5], st[:, 1:2], 1.0 / D, st[:, 3:4], ALU.mult, ALU.subtract, [R("st")], [R("st")])
            ts("dve", st[:, 4:5], st[:, 4:5], LN_EPS, None, ALU.add, None, [R("st")], [R("st")])
            act(st[:, 5:6], st[:, 4:5], AF.Sqrt, [R("st")], [R("st")])
            recip(st[:, 6:7], st[:, 5:6], [R("st")], [R("st")])
            ts("dve", dst, src, st[:, 2:3], st[:, 6:7], ALU.subtract, ALU.mult, [srcr, R("st")], [dstr])
            tt("pool", dst, dst, gam, ALU.mult, [dstr, gamr], [dstr])
            tt("dve", dst, dst, bet, ALU.add, [dstr, betr], [dstr])

        for tl in range(NQB):
            r0 = tl * 128
            b2 = tl % 2
            yt_, x_ = yTt[b2], xt[b2]
            ryt, rx = R("yTt%d" % b2), R("xt%d" % b2)
            dma("sp", yt_, yT_d[:, r0:r0 + 128].rearrange("(j p) t -> p j t", p=128), [R("yT_d")], [ryt])
            dma("sp", x_, x_ctx[TO + r0:TO + r0 + 128, :], [], [rx])
            for nb in range(4):
                for fc in range(16):
                    mm(ps[nb][:], yt_[:, fc, :], wo[:, fc, nb * 512:(nb + 1) * 512], fc == 0, fc == 15,
                       [ryt, R("wo")], [PSR[nb]])
                tt("dve", t1[:, nb * 512:(nb + 1) * 512], ps[nb][:], g1r[:, nb * 512:(nb + 1) * 512], ALU.mult,
                   [PSR[nb], R("g1r")], [], ws=[R("t1")])
            stt("dve", vv, x_, DN_ALPHA, t1, ALU.mult, ALU.add, [rx, R("t1")], [R("vv")])
            layer_norm(vv, R("vv"), t1, R("t1"), l1g, R("l1g"), l1b, R("l1b"))
            dma("sp", x1_d[r0:r0 + 128, :], t1, [R("t1")], [], ws=[R("x1_d")])
            tt("pool", h2f, t1, sc2r, ALU.mult, [R("t1"), R("sc2r")], [R("h2f")])
            tt("dve", h2f, h2f, sh2r, ALU.add, [R("h2f"), R("sh2r")], [R("h2f")])
            cp("act", h2b, h2f, [R("h2f")], [R("h2b")])
            dma("sp", h2_d[r0:r0 + 128, :], h2b, [R("h2b")], [], ws=[R("h2_d")])
            for q4_ in range(4):
                pb = 4 + q4_
                for k_ in range(4):
                    dc = q4_ * 4 + k_
                    trp(ps[pb][:, k_ * 128:(k_ + 1) * 128], h2f[:, dc * 128:(dc + 1) * 128], identf,
                        [R("h2f"), RTF], [PSR[pb]])
                if q4_ % 2 == 0:
                    cp("act", h2T[:, q4_ * 4:(q4_ + 1) * 4, :], ps3(pb), [PSR[pb]], [], ws=[R("h2T")])
                else:
                    cp("dve", h2T[:, q4_ * 4:(q4_ + 1) * 4, :], ps3(pb), [PSR[pb]], [], ws=[R("h2T")])
            for dc in range(16):
                mm(ps[4][:, 0:NE], h2T[:, dc, :], wrt[:, dc, :], dc == 0, dc == 15, [R("h2T"), R("wrt")], [PSR[4]])
            tt("dve", lg, ps[4][:, 0:NE], brt, ALU.add, [PSR[4], R("brt")], [R("lg")])
            max8(m8r, lg, [R("lg")], [R("m8r")])
            ts("dve", w4[:, 0:4], m8r[:, 0:4], m8r[:, 0:1], None, ALU.subtract, None, [R("m8r")], [R("w4")])
            act(w4[:, 0:4], w4[:, 0:4], AF.Exp, [R("w4")], [R("w4")])
            rsum(w4[:, 4:5], w4[:, 0:4], [R("w4")], [R("w4")])
            recip(w4[:, 5:6], w4[:, 4:5], [R("w4")], [R("w4")])
            ts("dve", w4[:, 0:4], w4[:, 0:4], w4[:, 5:6], None, ALU.mult, None, [R("w4")], [R("w4")])
            ts("dve", maskf, lg, m8r[:, 3:4], None, ALU.is_ge, None, [R("lg"), R("m8r")], [R("maskf")])
            cp("dve", maskb, maskf, [R("maskf")], [R("maskb")])
            mm(ps[5][:, 0:NE], TB("ltri"), maskb, True, True, [RTB, R("maskb")], [PSR[5]])
            mm(ps[5][:, NE:2 * NE], onesb, maskb, True, True, [RTB, R("maskb")], [PSR[5]])
            tt("dve", posv, ps[5][:, 0:NE], csum, ALU.add, [PSR[5], R("csum")], [R("posv")])
            tt("dve", csum, csum, ps[5][:, NE:2 * NE], ALU.add, [PSR[5], R("csum")], [R("csum")])
            tt("dve", posv, posv, TF("iotaEC1"), ALU.add, [R("posv"), RTF], [R("posv")])
            for k_ in range(4):
                ts("dve", oh, lg, m8r[:, k_:k_ + 1], None, ALU.is_equal, None, [R("lg"), R("m8r")], [R("oh")])
                tt("dve", oh, oh, posv, ALU.mult, [R("oh"), R("posv")], [R("oh")])
                rsum(slf[:, k_:k_ + 1], oh, [R("oh"), R("slf")], [R("slf")])
            ts("dve", slot_i[:, tl, :], slf, -1.0, None, ALU.add, None, [R("slf")], [R("slot_i%d" % tl)])
            for k_ in range(4):
                si, sir = sinfo[k_], R("sinfo%d" % k_)
                cp("pool", si[:, 0:1], TF("tokid", tl, tl + 1), [RTF], [sir])
                cp("pool", si[:, 1:2], w4[:, k_:k_ + 1], [R("w4"), sir], [sir])
                scatter(slotinfo_d[:, :], slot_i[:, tl, k_:k_ + 1], si, [sir, R("slot_i%d" % tl)], [],
                        ws=[R("slotinfo_d")])

        cnt_i = al("cnt_i", [128, NE], I32)
        cp("dve", cnt_i, csum, [R("csum")], [R("cnt_i")])
        dma("sp", counts_d[0:1, :], cnt_i[0:1, :], [R("cnt_i")], [R("counts_d")])

    if stages >= 6:
        areset()
        xT = al("xT", [128, 16, CAP], BF16)
        aT_ = al("actT", [128, 16, CAP], BF16)
        NWB = 2
        wgb = [al("wgb%d" % i, [128, 16, 512], BF16) for i in range(NWB)]
        xg = [al("xg%d" % i, [128, D], BF16) for i in range(3)]
        sinfA = [al("sinfA%d" % i, [128, 16, 2]) for i in range(2)]
        sidxA = [al("sidxA%d" % i, [128, 16], I32) for i in range(2)]
        wslA = [al("wslA%d" % i, [128, 16]) for i in range(2)]
        bgu = al("bgu", [128, NE * 32])
        dma("sp", bgu, b_gu_col[:, :], [], [R("bgu")])
        g1_ = [al("g1_%d" % i, [128, 512]) for i in range(2)]
        g2_ = [al("g2_%d" % i, [128, 512]) for i in range(2)]
        bdr = [al("bdr%d" % i, [128, 512]) for i in range(2)]
        yst = [al("yst%d" % i, [128, 512]) for i in range(2)]
        NJT = CAP // 128
        NSG = CAP // 512
        cnt6 = {"ec": 0, "xg": 0}

        wlist = []
        for e_ in range(NE):
            for blk in range(8):
                wlist.append(("gu", e_, blk))
            for db in range(4):
                wlist.append(("d", e_, db))

        def wload(n):
            kind, e_, blk = wlist[n]
            w, wr = wgb[n % NWB], R("wgb%d" % (n % NWB))
            src = w_gate_up if kind == "gu" else w_down
            dma("pool", w, src[e_, :, blk * 512:(blk + 1) * 512].rearrange("(c p) n -> p c n", p=128), [], [wr])

        def prep_expert(e_):
            k = e_ % 2
            dma("sp", sinfA[k], slotinfo_d[e_ * CAP:(e_ + 1) * CAP, :].rearrange("(j p) c -> p j c", p=128),
                [R("slotinfo_d")], [R("sinfA%d" % k)])
            cp("dve", sidxA[k], sinfA[k][:, :, 0], [R("sinfA%d" % k)], [R("sidxA%d" % k)])
            cp("dve", wslA[k], sinfA[k][:, :, 1], [R("sinfA%d" % k)], [R("wslA%d" % k)])

        def gather_tile(e_, jt):
            k = e_ % 2
            b3 = (e_ * NJT + jt) % 3
            gather(xg[b3], h2_d[:, :], sidxA[k][:, jt:jt + 1], [R("sidxA%d" % k), R("h2_d")], [R("xg%d" % b3)])

        def transpose_tile(e_, jt):
            b3 = (e_ * NJT + jt) % 3
            xg_, xgr = xg[b3], R("xg%d" % b3)
            for half in range(2):
                pb = half
                for k_ in range(8):
                    dc = half * 8 + k_
                    trp(psb[pb][:, k_ * 128:(k_ + 1) * 128], xg_[:, dc * 128:(dc + 1) * 128], identb,
                        [xgr, RTB], [PSR[pb]])
                src3 = psb[pb].rearrange("p (k t) -> p k t", k=8)
                if half == 0:
                    cp("act", xT[:, 0:8, jt * 128:(jt + 1) * 128], src3, [PSR[pb]], [], ws=[R("xT")])
                else:
                    cp("dve", xT[:, 8:16, jt * 128:(jt + 1) * 128], src3, [PSR[pb]], [], ws=[R("xT")])

        def gather_step(e_, jt):
            if jt + 2 < NJT:
                gather_tile(e_, jt + 2)
            transpose_tile(e_, jt)

        wload(0)
        prep_expert(0)
        gather_tile(0, 0)
        gather_tile(0, 1)
        for jt in range(NJT):
            gather_step(0, jt)
        wn = 0
        for e_ in range(NE):
            wsl = wslA[e_ % 2]
            wslr = R("wslA%d" % (e_ % 2))
            P.regload(counts_d[0:1, e_:e_ + 1])
            for blk in range(8):
                if wn + 1 < len(wlist):
                    wload(wn + 1)
                w, wr = wgb[wn % NWB], R("wgb%d" % (wn % NWB))
                wn += 1
                for sg in range(NSG):
                    for f4 in range(4):
                        fcg = blk * 4 + f4
                        fc = fcg % 16
                        bcol_ = bgu[:, e_ * 32 + fcg:e_ * 32 + fcg + 1]
                        ec = cnt6["ec"]
                        cnt6["ec"] += 1
                        pb = 2 + ec % 6
                        k2 = ec % 2
                        P.begin_pred((e_, sg))
                        for dc in range(16):
                            mm(ps[pb][:], w[:, dc, f4 * 128:(f4 + 1) * 128], xT[:, dc, sg * 512:(sg + 1) * 512],
                               dc == 0, dc == 15, [wr, R("xT")], [PSR[pb]])
                        ga, gar = g1_[k2], R("g1_%d" % k2)
                        gb, gbr = g2_[k2], R("g2_%d" % k2)
                        dstA = aT_[:, fc, sg * 512:(sg + 1) * 512]
                        if fcg < 16:
                            ts("dve", ga, ps[pb][:], bcol_, 7.0, ALU.add, ALU.min, [PSR[pb], R("bgu")], [gar])
                            act(gb, ga, AF.Sigmoid, [gar], [gbr], scale=1.702)
                            tt("dve", dstA, ga, gb, ALU.mult, [gar, gbr], [], ws=[R("actT")])
                        else:
                            ts("dve", ga, ps[pb][:], bcol_, 7.0, ALU.add, ALU.min, [PSR[pb], R("bgu")], [gar])
                            ts("dve", gb, ga, -7.0, 1.0, ALU.max, ALU.add, [gar], [gbr])
                            tt("dve", dstA, dstA, gb, ALU.mult, [gbr, R("actT")], [], ws=[R("actT")])
                        P.end_pred()
            if e_ + 1 < NE:
                prep_expert(e_ + 1)
                gather_tile(e_ + 1, 0)
                gather_tile(e_ + 1, 1)
            step = 0
            for db in range(4):
                if wn + 1 < len(wlist):
                    wload(wn + 1)
                w, wr = wgb[wn % NWB], R("wgb%d" % (wn % NWB))
                wn += 1
                bd, bdrr = bdr[db % 2], R("bdr%d" % (db % 2))
                dma("sp", bd, b_down[e_:e_ + 1, db * 512:(db + 1) * 512].partition_broadcast(128), [], [bdrr])
                for jt in range(NJT):
                    ec = cnt6["ec"]
                    cnt6["ec"] += 1
                    pb = 2 + ec % 6
                    k2 = ec % 2
                    P.begin_pred((e_, jt // 4))
                    for fc in range(16):
                        mm(ps[pb][:], aT_[:, fc, jt * 128:(jt + 1) * 128], w[:, fc, :], fc == 0, fc == 15,
                           [wr, R("actT")], [PSR[pb]])
                    ga, gar = g1_[k2], R("g1_%d" % k2)
                    ys, ysr = yst[k2], R("yst%d" % k2)
                    tt("dve", ga, ps[pb][:], bd, ALU.add, [PSR[pb], bdrr], [gar])
                    act(ys, ga, AF.Copy, [gar, wslr], [ysr], scale=wsl[:, jt:jt + 1])
                    s0 = e_ * CAP + jt * 128
                    dma("sp", ybuf_d[db][s0:s0 + 128, :], ys, [ysr], [], ws=[R("ybuf_d")])
                    P.end_pred()
                    if e_ + 1 < NE and step % 4 == 3:
                        gather_step(e_ + 1, step // 4)
                    step += 1

    if stages >= 7:
        areset()
        g2r = al("g2r", [128, D])
        l2g = al("l2g", [128, D])
        l2b = al("l2b", [128, D])
        dma("sp", g2r, modrow_d[3:4, :].partition_broadcast(128), [R("modrow_d")], [R("g2r")])
        dma("sp", l2g, ln2_g[0:1, :].partition_broadcast(128), [], [R("l2g")])
        dma("sp", l2b, ln2_b[0:1, :].partition_broadcast(128), [], [R("l2b")])
        yk = [al("yk%d" % k, [128, D]) for k in range(8)]
        x1t = [al("x1t%d" % i, [128, D]) for i in range(2)]
        v2 = al("v2", [128, D])
        sqv = al("sqv2", [128, D])
        o2 = [al("o2%d" % i, [128, D]) for i in range(2)]
        st = al("st2", [128, 16])

        for tl in range(NQB):
            r0 = tl * 128
            b2 = tl % 2
            for k_ in range(4):
                y_, yr = yk[b2 * 4 + k_], R("yk%d" % (b2 * 4 + k_))
                for q_ in range(4):
                    gather(y_[:, q_ * 512:(q_ + 1) * 512], ybuf_d[q_][:, :], slot_i[:, tl, k_:k_ + 1],
                           [R("slot_i%d" % tl), R("ybuf_d")], [], ws=[yr])
            x_, rx = x1t[b2], R("x1t%d" % b2)
            dma("sp", x_, x1_d[r0:r0 + 128, :], [R("x1_d")], [rx])
            ya_, yb_, yc_, yd_ = [yk[b2 * 4 + k_] for k_ in range(4)]
            ra, rb, rc, rd = [R("yk%d" % (b2 * 4 + k_)) for k_ in range(4)]
            tt("dve", ya_, ya_, yb_, ALU.add, [ra, rb], [ra])
            tt("pool", yc_, yc_, yd_, ALU.add, [rc, rd], [rc])
            tt("dve", ya_, ya_, yc_, ALU.add, [ra, rc], [ra])
            tt("pool", ya_, ya_, g2r, ALU.mult, [ra, R("g2r")], [ra])
            stt("dve", v2, x_, DN_ALPHA, ya_, ALU.mult, ALU.add, [rx, ra], [R("v2")])
            o_, orr = o2[b2], R("o2%d" % b2)
            rsum(st[:, 0:1], v2, [R("v2")], [R("st2")])
            tt("pool", sqv, v2, v2, ALU.mult, [R("v2")], [R("sqv2")])
            rsum(st[:, 1:2], sqv, [R("sqv2"), R("st2")], [R("st2")])
            ts("dve", st[:, 2:3], st[:, 0:1], 1.0 / D, None, ALU.mult, None, [R("st2")], [R("st2")])
            tt("dve", st[:, 3:4], st[:, 2:3], st[:, 2:3], ALU.mult, [R("st2")], [R("st2")])
            stt("dve", st[:, 4:5], st[:, 1:2], 1.0 / D, st[:, 3:4], ALU.mult, ALU.subtract, [R("st2")], [R("st2")])
            ts("dve", st[:, 4:5], st[:, 4:5], LN_EPS, None, ALU.add, None, [R("st2")], [R("st2")])
            act(st[:, 5:6], st[:, 4:5], AF.Sqrt, [R("st2")], [R("st2")])
            recip(st[:, 6:7], st[:, 5:6], [R("st2")], [R("st2")])
            ts("dve", o_, v2, st[:, 2:3], st[:, 6:7], ALU.subtract, ALU.mult, [R("v2"), R("st2")], [orr])
            tt("pool", o_, o_, l2g, ALU.mult, [orr, R("l2g")], [orr])
            tt("dve", o_, o_, l2b, ALU.add, [orr, R("l2b")], [orr])
            dma("sp", out_d[r0:r0 + 128, :], o_, [orr], [], ws=[R("out")], final=True)
    else:
        fin = al("fin", [128, 512])
        memset("pool", fin, 0.0, [R("fin")])
        dma("sp", out_d[0:128, 0:512], fin, [R("fin")], [R("out")], final=True)

    print("ops:", {k: len(v) for k, v in P.ops.items()})
    P.emit()
    return nc


def core_inputs(inp, core):
    b, h = core // 2, core % 2
    TBL = tables(h)
    x = inp["x"]
    if h == 1:
        x_ctx = x[b]
    else:
        x_ctx = np.concatenate([x[b, TO:], x[b, :TO]], axis=0)
    f = lambda a: np.ascontiguousarray(a, dtype=np.float32)

    def col(v, n):
        return f(np.asarray(v).reshape(n, 128).T)

    m = {
        "x_ctx": f(x_ctx),
        "c_col": col(inp["c"][b], 16),
        "w_ada": f(inp["w_ada"][0]),
        "b_ada_col": col(inp["b_ada"][0], 96),
        "b_ada_row": f(inp["b_ada"][0].reshape(1, -1)),
        "w_in": f(inp["w_in"][0]),
        "gla_w_gate": f(inp["gla_w_gate"][0]),
        "gla_b_gate": f(inp["gla_b_gate"][0].reshape(1, -1)),
        "gla_nw_col": col(inp["gla_norm_w"][0], 2),
        "w_out": f(inp["w_out"][0]),
        "ln1_g": f(inp["ln1_g"][0].reshape(1, -1)),
        "ln1_b": f(inp["ln1_b"][0].reshape(1, -1)),
        "w_router": f(inp["w_router"][0]),
        "b_router": f(inp["b_router"][0].reshape(1, -1)),
        "w_gate_up": f(inp["w_gate_up"][0]),
        "b_gu_col": f(inp["b_gate_up"][0].reshape(NE, 32, 128).transpose(2, 0, 1).reshape(128, NE * 32)),
        "w_down": f(inp["w_down"][0]),
        "b_down": f(inp["b_down"][0]),
        "ln2_g": f(inp["ln2_g"][0].reshape(1, -1)),
        "ln2_b": f(inp["ln2_b"][0].reshape(1, -1)),
        "tbs": TBL["tbs"], "tba": TBL["tba"], "tf": TBL["tf"], "tb5": TBL["tb5"],
    }
    for kv in ("k", "v"):
        m["cmp_pe_" + kv] = f(inp["cmp_pe_" + kv][0])
        m["cmp_w1_" + kv] = f(inp["cmp_w1_" + kv][0])
        m["cmp_b1c_" + kv] = col(inp["cmp_b1_" + kv][0], 2)
        m["cmp_w2_" + kv] = f(inp["cmp_w2_" + kv][0])
    return m


_NC_CACHE = {}


def kernel(**inputs):
    if "nc" not in _NC_CACHE:
        _NC_CACHE["nc"] = build()
    nc = _NC_CACHE["nc"]
    in_maps = [core_inputs(inputs, c) for c in range(8)]
    res = run_bass_kernel_spmd(nc, in_maps, core_ids=list(range(8)))
    out = np.empty((4, T, D), np.float32)
    for c in range(8):
        b, h = c // 2, c % 2
        out[b, h * TO:(h + 1) * TO] = res.results[c]["out"]
    return out
```
